# Optimizing a Trainium2 kernel written in Bass

```python
import math
import jax, jax.numpy as jnp
from jax import lax
import numpy as np

D_MODEL = 1024
BATCH = 4
SEQ = 4096
DEPTH = 2

N_MIXERS = 2
SSM_EXPAND = 2
SSM_D_INNER = SSM_EXPAND * D_MODEL
SSM_HEADDIM = 64
SSM_N_HEADS = SSM_D_INNER // SSM_HEADDIM
SSM_N_GROUPS = 8
SSM_HEADS_PER_GROUP = SSM_N_HEADS // SSM_N_GROUPS
SSM_D_STATE = 128
SSM_CONV_K = 4
SSM_CHUNK = 128
SSM_CONV_DIM = SSM_D_INNER + 2 * SSM_N_GROUPS * SSM_D_STATE
SSM_IN_DIM = SSM_D_INNER + SSM_CONV_DIM + SSM_N_HEADS
ATTN_HEAD_DIM = 64
ATTN_N_HEADS = D_MODEL // (2 * ATTN_HEAD_DIM)
ATTN_V_DIM = 2 * ATTN_HEAD_DIM
ATTN_QKV_DIM = 3 * D_MODEL
ROT_DIM = ATTN_HEAD_DIM // 4
ROPE_THETA = 500000.0
Q_BLOCK = 128
MOE_GROUPS = 4
MOE_EXPERTS_PER_GROUP = 8
MOE_N_EXPERTS = MOE_GROUPS * MOE_EXPERTS_PER_GROUP
MOE_TOP_K = 2
MOE_D_FF = 512
MOE_BLOCK = 128
DEEPNORM_ALPHA = (2 * DEPTH) ** 0.25
DEEPNORM_BETA = (8 * DEPTH) ** -0.25
NORM_EPS = 1e-5
N_SSM_LAYERS = (DEPTH + N_MIXERS - 1) // N_MIXERS
N_ATTN_LAYERS = DEPTH // N_MIXERS

kernel_name = "hybrid_ssd_diffattn_hiermoe_deepnorm"


def layer_norm(x, g, b):
    xf = x.astype(jnp.float32)
    mu = jnp.mean(xf, axis=-1, keepdims=True)
    var = jnp.mean(jnp.square(xf - mu), axis=-1, keepdims=True)
    y = (xf - mu) * lax.rsqrt(var + NORM_EPS) * g.astype(jnp.float32) + b.astype(jnp.float32)
    return y.astype(x.dtype)


def causal_depthwise_conv(u, w, bias):
    k = w.shape[0]
    out = lax.conv_general_dilated(u, w[:, None, :], window_strides=(1,), padding=[(k - 1, 0)],
                                   dimension_numbers=("NWC", "WIO", "NWC"),
                                   feature_group_count=u.shape[-1])
    return out + bias


def ssd_chunked(xh, dt, a, bm, cm):
    b, s = xh.shape[0], xh.shape[1]
    c = s // SSM_CHUNK
    G, R, P, N, L = SSM_N_GROUPS, SSM_HEADS_PER_GROUP, SSM_HEADDIM, SSM_D_STATE, SSM_CHUNK
    dtc = dt.reshape(b, c, L, G, R)
    xdt = xh.reshape(b, c, L, G, R, P) * dtc[..., None]
    bc = bm.reshape(b, c, L, G, N)
    cc = cm.reshape(b, c, L, G, N)
    a_cum = jnp.cumsum(dtc * a, axis=2)
    causal = jnp.tril(jnp.ones((L, L), dtype=bool))
    seg = a_cum[:, :, :, None] - a_cum[:, :, None, :]
    decay_ls = jnp.exp(jnp.where(causal[:, :, None, None], seg, -jnp.inf))
    cb = jnp.einsum("bclgn,bcsgn->bclsg", cc, bc)
    y_diag = jnp.einsum("bclsgr,bcsgrp->bclgrp", cb[..., None] * decay_ls, xdt)
    decay_to_end = jnp.exp(a_cum[:, :, -1:] - a_cum)
    chunk_states = jnp.einsum("bclgn,bclgrp->bcgrpn", bc, xdt * decay_to_end[..., None])
    chunk_decay = jnp.exp(a_cum[:, :, -1])

    def step(h, inp):
        st, dec = inp
        return h * dec[..., None, None] + st, h

    h0 = jnp.zeros((b, G, R, P, N), jnp.float32)
    _, prev = lax.scan(step, h0, (jnp.moveaxis(chunk_states, 1, 0), jnp.moveaxis(chunk_decay, 1, 0)))
    prev = jnp.moveaxis(prev, 0, 1)
    y_off = jnp.einsum("bclgn,bcgrpn->bclgrp", cc, prev) * jnp.exp(a_cum)[..., None]
    return (y_diag + y_off).reshape(b, s, G, R, P)


def mamba2_mixer(x, w_in, conv_w, conv_b, dt_bias, a_log, d_skip, norm_w, w_out):
    b, s, _ = x.shape
    G, R, P, N = SSM_N_GROUPS, SSM_HEADS_PER_GROUP, SSM_HEADDIM, SSM_D_STATE
    zxbcdt = x @ w_in
    z = zxbcdt[..., :SSM_D_INNER]
    xbc = zxbcdt[..., SSM_D_INNER:SSM_D_INNER + SSM_CONV_DIM]
    dt_raw = zxbcdt[..., SSM_D_INNER + SSM_CONV_DIM:]
    xbc = jax.nn.silu(causal_depthwise_conv(xbc, conv_w, conv_b))
    xs = xbc[..., :SSM_D_INNER]
    bm = xbc[..., SSM_D_INNER:SSM_D_INNER + G * N].reshape(b, s, G, N).astype(jnp.float32)
    cm = xbc[..., SSM_D_INNER + G * N:].reshape(b, s, G, N).astype(jnp.float32)
    dt = jax.nn.softplus(dt_raw.astype(jnp.float32) + dt_bias.astype(jnp.float32)).reshape(b, s, G, R)
    a = -jnp.exp(a_log.astype(jnp.float32)).reshape(G, R)
    xh = xs.reshape(b, s, G, R, P).astype(jnp.float32)
    y = ssd_chunked(xh, dt, a, bm, cm) + d_skip.astype(jnp.float32).reshape(G, R)[..., None] * xh
    yg = (y.reshape(b, s, SSM_D_INNER) * jax.nn.silu(z.astype(jnp.float32))).reshape(b, s, G, SSM_D_INNER // G)
    yg = yg * lax.rsqrt(jnp.mean(jnp.square(yg), axis=-1, keepdims=True) + NORM_EPS)
    yg = yg.reshape(b, s, SSM_D_INNER) * norm_w.astype(jnp.float32)
    return yg.astype(x.dtype) @ w_out


def rope_tables(positions):
    inv = ROPE_THETA ** (-jnp.arange(0, ROT_DIM, 2, dtype=jnp.float32) / ROT_DIM)
    ang = positions.astype(jnp.float32)[..., None] * inv
    return jnp.cos(ang), jnp.sin(ang)


def apply_partial_rope(t, cos, sin):
    half = ROT_DIM // 2
    tf = t[..., :ROT_DIM].astype(jnp.float32)
    r1, r2 = tf[..., :half], tf[..., half:]
    c = cos[:, :, None, None, :]
    s = sin[:, :, None, None, :]
    rot = jnp.concatenate([r1 * c - r2 * s, r2 * c + r1 * s], axis=-1).astype(t.dtype)
    return jnp.concatenate([rot, t[..., ROT_DIM:]], axis=-1)


def diff_attention(x, cos, sin, w_qkv, lam_q1, lam_k1, lam_q2, lam_k2, subln_w, w_o, lambda_init):
    b, s, _ = x.shape
    H, Dh = ATTN_N_HEADS, ATTN_HEAD_DIM
    qkv = x @ w_qkv
    q = qkv[..., :D_MODEL].reshape(b, s, H, 2, Dh)
    k = qkv[..., D_MODEL:2 * D_MODEL].reshape(b, s, H, 2, Dh)
    v = qkv[..., 2 * D_MODEL:].reshape(b, s, H, ATTN_V_DIM)
    q = apply_partial_rope(q, cos, sin)
    k = apply_partial_rope(k, cos, sin)
    scale = Dh ** -0.5
    lam = (jnp.exp(jnp.sum(lam_q1.astype(jnp.float32) * lam_k1.astype(jnp.float32)))
           - jnp.exp(jnp.sum(lam_q2.astype(jnp.float32) * lam_k2.astype(jnp.float32))) + lambda_init)
    outs = []
    for i in range(s // Q_BLOCK):
        kv_len = (i + 1) * Q_BLOCK
        qb = q[:, i * Q_BLOCK:kv_len]
        kb = k[:, :kv_len]
        vb = v[:, :kv_len]
        sc = jnp.einsum("bqhcd,bkhcd->bhcqk", qb, kb).astype(jnp.float32) * scale
        q_pos = i * Q_BLOCK + jnp.arange(Q_BLOCK)
        mask = jnp.arange(kv_len)[None, :] <= q_pos[:, None]
        p = jax.nn.softmax(jnp.where(mask, sc, -jnp.inf), axis=-1)
        attn = p[:, :, 0] - lam * p[:, :, 1]
        outs.append(jnp.einsum("bhqk,bkhe->bqhe", attn.astype(v.dtype), vb))
    o = jnp.concatenate(outs, axis=1).astype(jnp.float32)
    o = o * lax.rsqrt(jnp.mean(jnp.square(o), axis=-1, keepdims=True) + NORM_EPS)
    o = o * subln_w.astype(jnp.float32) * (1.0 - lambda_init)
    return o.reshape(b, s, H * ATTN_V_DIM).astype(x.dtype) @ w_o


def hier_moe(x, w_group, w_expert, w_gate, w_up, w_down):
    b, s, d = x.shape
    T = b * s
    A = T * MOE_TOP_K
    xf = x.reshape(T, d)
    g_prob = jax.nn.softmax((xf @ w_group).astype(jnp.float32), axis=-1)
    g_sel = jnp.argmax(g_prob, axis=-1)
    g_gate = jnp.take_along_axis(g_prob, g_sel[:, None], axis=-1)[:, 0]
    e_logits = (xf @ w_expert).astype(jnp.float32).reshape(T, MOE_GROUPS, MOE_EXPERTS_PER_GROUP)
    e_logits = jnp.take_along_axis(e_logits, g_sel[:, None, None], axis=1)[:, 0]
    top_p, top_i = lax.top_k(jax.nn.softmax(e_logits, axis=-1), MOE_TOP_K)
    top_p = top_p / jnp.sum(top_p, axis=-1, keepdims=True)
    weight = (g_gate[:, None] * top_p).reshape(A)
    expert_id = (g_sel[:, None] * MOE_EXPERTS_PER_GROUP + top_i).reshape(A).astype(jnp.int32)
    token_id = jnp.repeat(jnp.arange(T, dtype=jnp.int32), MOE_TOP_K)
    order = jnp.argsort(expert_id)
    sorted_e = expert_id[order]
    counts = jax.ops.segment_sum(jnp.ones((A,), jnp.int32), expert_id, num_segments=MOE_N_EXPERTS)
    padded = ((counts + MOE_BLOCK - 1) // MOE_BLOCK) * MOE_BLOCK
    starts = jnp.cumsum(counts) - counts
    pends = jnp.cumsum(padded)
    pstarts = pends - padded
    dest = pstarts[sorted_e] + (jnp.arange(A, dtype=jnp.int32) - starts[sorted_e])
    n_blocks = -(-A // MOE_BLOCK) + MOE_N_EXPERTS
    P = n_blocks * MOE_BLOCK
    row_tok = jnp.full((P,), T, jnp.int32).at[dest].set(token_id[order])
    row_w = jnp.zeros((P,), x.dtype).at[dest].set(weight[order].astype(x.dtype))
    block_expert = jnp.minimum(jnp.searchsorted(pends, jnp.arange(n_blocks, dtype=jnp.int32) * MOE_BLOCK,
                                                side="right"), MOE_N_EXPERTS - 1)
    xpad = jnp.concatenate([xf, jnp.zeros((1, d), x.dtype)], axis=0)
    rows = xpad[row_tok].reshape(n_blocks, MOE_BLOCK, d)

    def expert_block(args):
        xb, e = args
        h = jax.nn.silu(xb @ w_gate[e]) * (xb @ w_up[e])
        return h @ w_down[e]

    yb = lax.map(expert_block, (rows, block_expert)).reshape(P, d)
    out = jax.ops.segment_sum(yb * row_w[:, None], row_tok, num_segments=T + 1)[:T]
    return out.reshape(b, s, d)


def setup_inputs(seed: int = 0) -> dict:
    key = jax.random.key(seed)
    ks = jax.random.split(key, 32)
    f32 = jnp.float32
    nrm = lambda k, shape, sc: jax.random.normal(k, shape, f32) * sc
    x = jax.random.normal(ks[0], (BATCH, SEQ, D_MODEL), f32)
    start = jax.random.randint(ks[1], (BATCH, 1), 0, 1024, dtype=jnp.int32)
    positions = (start + jnp.arange(SEQ, dtype=jnp.int32)[None, :]).astype(jnp.int32)
    ln_mix_g = 1.0 + nrm(ks[2], (DEPTH, D_MODEL), 0.02)
    ln_mix_b = nrm(ks[3], (DEPTH, D_MODEL), 0.02)
    ln_ffn_g = 1.0 + nrm(ks[4], (DEPTH, D_MODEL), 0.02)
    ln_ffn_b = nrm(ks[5], (DEPTH, D_MODEL), 0.02)
    ns = N_SSM_LAYERS
    ssm_w_in = nrm(ks[6], (ns, D_MODEL, SSM_IN_DIM), D_MODEL ** -0.5)
    ssm_conv_w = nrm(ks[7], (ns, SSM_CONV_K, SSM_CONV_DIM), SSM_CONV_K ** -0.5)
    ssm_conv_b = nrm(ks[8], (ns, SSM_CONV_DIM), 0.02)
    u = jax.random.uniform(ks[9], (ns, SSM_N_HEADS), f32)
    dt0 = jnp.exp(u * (math.log(0.1) - math.log(0.001)) + math.log(0.001))
    ssm_dt_bias = dt0 + jnp.log(-jnp.expm1(-dt0))
    ssm_a_log = jnp.log(jax.random.uniform(ks[10], (ns, SSM_N_HEADS), f32, 1.0, 16.0))
    ssm_d = 1.0 + nrm(ks[11], (ns, SSM_N_HEADS), 0.02)
    ssm_norm_w = 1.0 + nrm(ks[12], (ns, SSM_D_INNER), 0.02)
    ssm_w_out = nrm(ks[13], (ns, SSM_D_INNER, D_MODEL), SSM_D_INNER ** -0.5 * DEEPNORM_BETA)
    na = N_ATTN_LAYERS
    attn_w_qkv = nrm(ks[14], (na, D_MODEL, ATTN_QKV_DIM), D_MODEL ** -0.5)
    attn_lam_q1 = nrm(ks[15], (na, ATTN_HEAD_DIM), 0.1)
    attn_lam_k1 = nrm(ks[16], (na, ATTN_HEAD_DIM), 0.1)
    attn_lam_q2 = nrm(ks[17], (na, ATTN_HEAD_DIM), 0.1)
    attn_lam_k2 = nrm(ks[18], (na, ATTN_HEAD_DIM), 0.1)
    attn_subln_w = 1.0 + nrm(ks[19], (na, ATTN_V_DIM), 0.02)
    attn_w_o = nrm(ks[20], (na, ATTN_N_HEADS * ATTN_V_DIM, D_MODEL), D_MODEL ** -0.5 * DEEPNORM_BETA)
    moe_w_group = nrm(ks[21], (DEPTH, D_MODEL, MOE_GROUPS), D_MODEL ** -0.5)
    moe_w_expert = nrm(ks[22], (DEPTH, D_MODEL, MOE_N_EXPERTS), D_MODEL ** -0.5)
    moe_w_gate = nrm(ks[23], (DEPTH, MOE_N_EXPERTS, D_MODEL, MOE_D_FF), D_MODEL ** -0.5)
    moe_w_up = nrm(ks[24], (DEPTH, MOE_N_EXPERTS, D_MODEL, MOE_D_FF), D_MODEL ** -0.5)
    moe_w_down = nrm(ks[25], (DEPTH, MOE_N_EXPERTS, MOE_D_FF, D_MODEL), MOE_D_FF ** -0.5 * DEEPNORM_BETA)
    return {"x": x, "positions": positions,
            "ln_mix_g": ln_mix_g, "ln_mix_b": ln_mix_b, "ln_ffn_g": ln_ffn_g, "ln_ffn_b": ln_ffn_b,
            "ssm_w_in": ssm_w_in, "ssm_conv_w": ssm_conv_w, "ssm_conv_b": ssm_conv_b,
            "ssm_dt_bias": ssm_dt_bias, "ssm_a_log": ssm_a_log, "ssm_d": ssm_d,
            "ssm_norm_w": ssm_norm_w, "ssm_w_out": ssm_w_out,
            "attn_w_qkv": attn_w_qkv, "attn_lam_q1": attn_lam_q1, "attn_lam_k1": attn_lam_k1,
            "attn_lam_q2": attn_lam_q2, "attn_lam_k2": attn_lam_k2, "attn_subln_w": attn_subln_w,
            "attn_w_o": attn_w_o,
            "moe_w_group": moe_w_group, "moe_w_expert": moe_w_expert,
            "moe_w_gate": moe_w_gate, "moe_w_up": moe_w_up, "moe_w_down": moe_w_down}


def reference(x, positions, ln_mix_g, ln_mix_b, ln_ffn_g, ln_ffn_b,
              ssm_w_in, ssm_conv_w, ssm_conv_b, ssm_dt_bias, ssm_a_log, ssm_d, ssm_norm_w, ssm_w_out,
              attn_w_qkv, attn_lam_q1, attn_lam_k1, attn_lam_q2, attn_lam_k2, attn_subln_w, attn_w_o,
              moe_w_group, moe_w_expert, moe_w_gate, moe_w_up, moe_w_down):
    cos, sin = rope_tables(positions)
    h = x
    for layer in range(DEPTH):
        j = layer // N_MIXERS
        if layer % N_MIXERS == 0:
            mix = mamba2_mixer(h, ssm_w_in[j], ssm_conv_w[j], ssm_conv_b[j], ssm_dt_bias[j],
                               ssm_a_log[j], ssm_d[j], ssm_norm_w[j], ssm_w_out[j])
        else:
            lambda_init = 0.8 - 0.6 * math.exp(-0.3 * layer)
            mix = diff_attention(h, cos, sin, attn_w_qkv[j], attn_lam_q1[j], attn_lam_k1[j],
                                 attn_lam_q2[j], attn_lam_k2[j], attn_subln_w[j], attn_w_o[j], lambda_init)
        h = layer_norm(DEEPNORM_ALPHA * h + mix, ln_mix_g[layer], ln_mix_b[layer])
        ffn = hier_moe(h, moe_w_group[layer], moe_w_expert[layer], moe_w_gate[layer],
                       moe_w_up[layer], moe_w_down[layer])
        h = layer_norm(DEEPNORM_ALPHA * h + ffn, ln_ffn_g[layer], ln_ffn_b[layer])
    return h
```

```python
import math
from contextlib import ExitStack

import numpy as np
import concourse.bass as bass
import concourse.mybir as mybir
from concourse.bass_utils import run_bass_kernel_spmd

F32 = mybir.dt.float32
BF16 = mybir.dt.bfloat16
I32 = mybir.dt.int32
U32 = mybir.dt.uint32
AF = mybir.ActivationFunctionType
ALU = mybir.AluOpType
AX = mybir.AxisListType


class _Op:
    __slots__ = ("eng", "fn", "reads", "writes", "kind", "sem", "val", "final", "deps", "slot")

    def __init__(self, eng, fn, reads, writes, kind, final=False):
        self.eng, self.fn, self.reads, self.writes, self.kind, self.final = eng, fn, reads, writes, kind, final
        self.sem = None
        self.val = 0
        self.deps = ()
        self.slot = -1


class Prog:
    ENGS = ("pe", "act", "dve", "pool", "sp")
    NDMA = 24

    def __init__(self, nc):
        self.nc = nc
        self.ops = []
        self.stack = ExitStack()
        self._init_sems()

    sid = 0

    def sb(self, name, shape, dt):
        return self.stack.enter_context(self.nc.sbuf_tensor("%s_s%d" % (name, self.sid), list(shape), dt))

    def ps(self, name, shape, dt):
        return self.stack.enter_context(self.nc.psum_tensor("%s_p%d" % (name, self.sid), list(shape), dt))

    def dram(self, name, shape, dt, kind="Internal"):
        return self.nc.dram_tensor(name, list(shape), dt, kind=kind).ap()

    def op(self, eng, fn, reads=(), writes=()):
        o = _Op(eng, fn, tuple(reads), tuple(writes), "c")
        self.ops.append(o)
        return o

    def dma(self, q, out=None, in_=None, reads=(), writes=(), final=False, indirect=None, **kw):
        if indirect is None:
            fn = lambda e: e.dma_start(out=out, in_=in_, **kw)
        else:
            fn = indirect
        o = _Op(q, fn, tuple(reads), tuple(writes), "d", final)
        self.ops.append(o)
        return o

    def make_identity(self, t, dt):
        nc = self.nc
        n = t.shape[0]

        def f(e):
            e.memset(t[:], 0.0)
            return e.affine_select(out=t[:], in_=t[:], pattern=[[-1, n]], compare_op=ALU.not_equal,
                                   fill=1.0, base=0, channel_multiplier=1)
        self.op("pool", f, writes=[t.name])

    def _init_sems(self):
        nc, st = self.nc, self.stack
        self.sems = {e: st.enter_context(nc.semaphore("s_" + e)) for e in ("pe", "act", "dve", "pool")}
        self.dsems = {q: [st.enter_context(nc.semaphore("d_%s%d" % (q, i))) for i in range(self.NDMA)]
                      for q in ("sp", "pool", "act")}
        self.cnt = {e: 0 for e in self.sems}
        self.dcnt = {q: [0] * self.NDMA for q in self.dsems}
        self.dnext = {q: 0 for q in self.dsems}
        self.waited = {e: {} for e in self.ENGS}

    def stage(self):
        prog = self

        class _S:
            def __enter__(s):
                s.outer = prog.stack
                prog.stack = ExitStack()
                prog.sid += 1
                return prog

            def __exit__(s, *a):
                if a[0] is None:
                    prog.flush()
                prog.stack.close()
                prog.stack = s.outer
                return False
        return _S()

    def reg(self, eng, value):
        r = self._regs.get(value)
        if r is None:
            r = self._regs[value] = eng.to_reg(value)
        return r

    def flush(self):
        nc = self.nc
        self._regs = {}
        sems, dsems, cnt, dcnt, dnext = self.sems, self.dsems, self.cnt, self.dcnt, self.dnext
        last_w = {}
        readers = {}
        per_eng = {e: [] for e in self.ENGS}
        for o in self.ops:
            ps_r = [k for k in o.reads if "_p" in k]
            if ps_r:
                o.reads = tuple(k for k in o.reads if "_p" not in k)
                o.writes = tuple(o.writes) + tuple(ps_r)
            deps = []
            for k in o.reads:
                w = last_w.get(k)
                if w is not None:
                    deps.append(w)
            for k in o.writes:
                w = last_w.get(k)
                if w is not None:
                    deps.append(w)
                deps.extend(readers.get(k, ()))
            o.deps = [d for d in dict.fromkeys(deps) if d is not o]
            for k in o.reads:
                readers.setdefault(k, []).append(o)
            for k in o.writes:
                last_w[k] = o
                readers[k] = []
            if o.kind == "c":
                cnt[o.eng] += 1
                o.sem, o.val = sems[o.eng], cnt[o.eng]
            else:
                q = o.eng
                s = dnext[q]
                dnext[q] = (s + 1) % self.NDMA
                dcnt[q][s] += 16
                o.sem, o.val = dsems[q][s], dcnt[q][s]
            per_eng[o.eng].append(o)
        self.ops = []

        def emit(eng_name, e):
            waited = self.waited[eng_name]

            def wait(sem, val):
                key = id(sem)
                if waited.get(key, 0) >= val:
                    return
                waited[key] = val
                e.wait_ge(sem, val)

            for o in per_eng[eng_name]:
                for d in o.deps:
                    if d.kind == "c" and d.eng == eng_name and eng_name == "pe":
                        continue
                    wait(d.sem, d.val)
                if o.kind == "d" and o.val > 16:
                    wait(o.sem, o.val - 16)
                ins = o.fn(e)
                ins.then_inc(o.sem, 16 if o.kind == "d" else 1)
            if eng_name == "sp":
                for q in dsems:
                    for i, s in enumerate(dsems[q]):
                        if dcnt[q][i]:
                            wait(s, dcnt[q][i])

        with nc.Block() as block:
            @block.tensor
            def _(e):
                emit("pe", e)

            @block.scalar
            def _(e):
                emit("act", e)

            @block.vector
            def _(e):
                emit("dve", e)

            @block.gpsimd
            def _(e):
                emit("pool", e)

            @block.sync
            def _(e):
                emit("sp", e)

    def finish(self):
        if self.ops:
            self.flush()
        self.stack.close()


D = 1024
DI = 2048
NH = 32
HP = 64
NG = 8
NS = 128
INDIM = 6176
ALPHA = 4.0 ** 0.25
EPS = 1e-5


class Builder:
    def __init__(self, S, dbg=()):
        self.S = S
        self.T = S // 128
        self.nc = bass.Bass("TRN2", target_bir_lowering=False)
        self.P = Prog(self.nc)
        self.dbg = set(dbg)

    def inp(self, name, shape, dt=F32):
        return self.nc.dram_tensor(name, list(shape), dt, kind="ExternalInput").ap()

    def scratch(self, name, shape, dt):
        kind = "ExternalOutput" if name in self.dbg else "Internal"
        return self.nc.dram_tensor(name, list(shape), dt, kind=kind).ap()

    def l0_in(self, x_d, w_in_d, convw_d, convb_d, hp_d, cst_d, zs_d, xbcT_d, dtd_d):
        P, S, T = self.P, self.S, self.T
        w3 = w_in_d.rearrange("(c p) n -> p c n", p=128)
        with P.stage():
            cst = P.sb("cst", [128, 5, 128], F32)
            identb = P.sb("identb", [128, 128], BF16)
            hp = P.sb("hp", [128, 3, 32], F32)
            abc = P.sb("abc", [128, 32], F32)
            convw = P.sb("convw", [128, 32, 4], F32)
            convb = P.sb("convb", [128, 32], F32)
            xT = P.sb("xT", [128, 8, S], BF16)
            wz = P.sb("wz", [128, 8, 2048], BF16)
            wdt = P.sb("wdt", [128, 8, 32], BF16)
            xin = [P.sb("xin%d" % i, [128, 1024], F32) for i in range(2)]
            zsb = [P.sb("zsb%d" % i, [128, 2048], BF16) for i in range(2)]
            dts = [P.sb("dts%d" % i, [128, 64], F32) for i in range(2)]
            t0 = P.sb("t0", [128, 32], F32)
            ab = P.sb("ab", [128, 32], F32)
            e1 = P.sb("e1", [128, 32], F32)
            l1 = P.sb("l1", [128, 32], F32)
            wblk = [P.sb("wblk%d" % i, [128, 8, 512], BF16) for i in range(2)]
            ub = [P.sb("ub%d" % i, [128, 3 + S], BF16) for i in range(2)]
            dg = [P.sb("dg%d" % i, [128, 4, 128], BF16) for i in range(2)]
            xo = [P.sb("xo%d" % i, [128, 512], BF16) for i in range(2)]
            ptr = [P.ps("ptr%d" % i, [128, 512], F32) for i in range(2)]
            pz = [P.ps("pz%d" % i, [128, 512], F32) for i in range(2)]
            pu = [P.ps("pu%d" % i, [128, 512], F32) for i in range(2)]
            pc = [P.ps("pc%d" % i, [128, 512], F32) for i in range(2)]

            P.dma("sp", cst[:], cst_d, writes=["cst"])
            P.dma("sp", hp[:], hp_d, writes=["hp"])
            P.dma("sp", convw[:], convw_d, writes=["convw"])
            P.dma("sp", convb[:], convb_d, writes=["convb"])
            P.op("dve", lambda e: e.tensor_copy(out=identb[:], in_=cst[:, 0, :]), reads=["cst"], writes=["identb"])
            P.op("act", lambda e: e.activation(out=abc[:], in_=hp[:, 1, :], func=AF.Exp), reads=["hp"], writes=["abc"])
            P.op("dve", lambda e: e.tensor_scalar(out=abc[:], in0=abc[:], scalar1=-1.0, scalar2=None, op0=ALU.mult),
                 reads=["abc"], writes=["abc"])
            for j in range(4):
                P.dma("pool", wz[:, :, j * 512:(j + 1) * 512], w3[:, :, j * 512:(j + 1) * 512], writes=["wz%d" % j])
            P.dma("pool", wdt[:], w3[:, :, 6144:6176], writes=["wdt"])
            for i in range(2):
                P.op("pool", lambda e, i=i: e.memset(ub[i][:, 0:3], 0.0), writes=["ubpad%d" % i])

            for i in range(T):
                xi = xin[i % 2]
                P.dma("sp", xi[:], x_d[i * 128:(i + 1) * 128, :], writes=[xi.name])
                for hf in range(2):
                    def ftr(pe, xi=xi, hf=hf):
                        for k in range(4):
                            kk = hf * 4 + k
                            ins = pe.transpose(ptr[hf][:, k * 128:(k + 1) * 128], xi[:, kk * 128:(kk + 1) * 128], cst[:, 0, :])
                        return ins
                    P.op("pe", ftr, reads=[xi.name, "cst"], writes=[ptr[hf].name])
                    P.op("act", lambda e, i=i, hf=hf: e.activation(
                        out=xT[:, hf * 4:(hf + 1) * 4, i * 128:(i + 1) * 128],
                        in_=ptr[hf][:].rearrange("p (k c) -> p k c", c=128), func=AF.Copy),
                        reads=[ptr[hf].name], writes=["xT%d" % i])
                zb = zsb[i % 2]
                for cbk in range(4):
                    pzz = pz[cbk % 2]

                    def fz(pe, i=i, cbk=cbk, pzz=pzz):
                        for k in range(8):
                            ins = pe.matmul(pzz[:], lhsT=xT[:, k, i * 128:(i + 1) * 128], rhs=wz[:, k, cbk * 512:(cbk + 1) * 512],
                                            start=(k == 0), stop=(k == 7))
                        return ins
                    P.op("pe", fz, reads=["xT%d" % i, "wz%d" % cbk], writes=[pzz.name])
                    P.op("act", lambda e, zb=zb, cbk=cbk, pzz=pzz: e.activation(
                        out=zb[:, cbk * 512:(cbk + 1) * 512], in_=pzz[:], func=AF.Silu),
                        reads=[pzz.name], writes=[zb.name])
                P.dma("sp", zs_d[i * 128:(i + 1) * 128, :], zb[:], reads=[zb.name], writes=["zs_d%d" % i])
                pzz = pz[0]

                def fdt(pe, i=i, pzz=pzz):
                    for k in range(8):
                        ins = pe.matmul(pzz[:, 0:32], lhsT=xT[:, k, i * 128:(i + 1) * 128], rhs=wdt[:, k, :],
                                        start=(k == 0), stop=(k == 7))
                    return ins
                P.op("pe", fdt, reads=["xT%d" % i, "wdt"], writes=[pzz.name])
                dd = dts[i % 2]
                P.op("dve", lambda e, pzz=pzz: e.tensor_tensor(out=t0[:], in0=pzz[:, 0:32], in1=hp[:, 0, :], op=ALU.add),
                     reads=[pzz.name, "hp"], writes=["t0"])
                P.op("dve", lambda e: e.scalar_tensor_tensor(out=ab[:], in0=t0[:], scalar=-1.0, in1=t0[:], op0=ALU.mult, op1=ALU.max),
                     reads=["t0"], writes=["ab"])
                P.op("act", lambda e: e.activation(out=e1[:], in_=ab[:], func=AF.Exp, scale=-1.0), reads=["ab"], writes=["e1"])
                P.op("act", lambda e: e.activation(out=l1[:], in_=e1[:], func=AF.Ln, bias=1.0, scale=1.0),
                     reads=["e1"], writes=["l1"])
                P.op("dve", lambda e, dd=dd: e.scalar_tensor_tensor(out=dd[:, 0:32], in0=t0[:], scalar=0.0, in1=l1[:],
                                                                    op0=ALU.max, op1=ALU.add),
                     reads=["t0", "l1"], writes=[dd.name])
                P.op("dve", lambda e, dd=dd: e.tensor_tensor(out=dd[:, 32:64], in0=dd[:, 0:32], in1=abc[:], op=ALU.mult),
                     reads=[dd.name, "abc"], writes=[dd.name])
                P.dma("sp", dtd_d[i * 128:(i + 1) * 128, :], dd[:], reads=[dd.name], writes=["dtd_d%d" % i])

            NTG = S // 512
            for sb4 in range(8):
                wb = wblk[sb4 % 2]
                c0 = 2048 + sb4 * 512
                P.dma("pool", wb[:], w3[:, :, c0:c0 + 512], writes=[wb.name])
                for j in range(4):
                    cb = sb4 * 4 + j
                    dgc = dg[cb % 2]
                    u = ub[cb % 2]

                    def fdg(e, dgc=dgc, cb=cb):
                        for k in range(4):
                            ins = e.tensor_scalar(out=dgc[:, k, :], in0=identb[:], scalar1=convw[:, cb, k:k + 1],
                                                  scalar2=None, op0=ALU.mult)
                        return ins
                    P.op("dve", fdg, reads=["identb", "convw"], writes=[dgc.name])
                    for tg in range(NTG):
                        puu = pu[tg % 2]
                        pcc = pc[tg % 2]
                        xoo = xo[tg % 2]

                        def fu(pe, wb=wb, j=j, tg=tg, puu=puu):
                            for k in range(8):
                                ins = pe.matmul(puu[:], lhsT=wb[:, k, j * 128:(j + 1) * 128],
                                                rhs=xT[:, k, tg * 512:(tg + 1) * 512], start=(k == 0), stop=(k == 7))
                            return ins
                        P.op("pe", fu, reads=[wb.name] + ["xT%d" % t for t in range(tg * 4, tg * 4 + 4)], writes=[puu.name])
                        P.op("act", lambda e, u=u, tg=tg, puu=puu: e.activation(
                            out=u[:, 3 + tg * 512:3 + (tg + 1) * 512], in_=puu[:], func=AF.Copy),
                            reads=[puu.name], writes=["%s_%d" % (u.name, tg)])

                        def fcv(pe, u=u, tg=tg, pcc=pcc, dgc=dgc):
                            for k in range(4):
                                ins = pe.matmul(pcc[:], lhsT=dgc[:, k, :], rhs=u[:, tg * 512 + k:tg * 512 + k + 512],
                                                start=(k == 0), stop=(k == 3))
                            return ins
                        rk = ["%s_%d" % (u.name, tg), dgc.name, "ubpad%d" % (cb % 2)]
                        if tg > 0:
                            rk.append("%s_%d" % (u.name, tg - 1))
                        P.op("pe", fcv, reads=rk, writes=[pcc.name])
                        P.op("act", lambda e, xoo=xoo, pcc=pcc, cb=cb: e.activation(
                            out=xoo[:], in_=pcc[:], func=AF.Silu, bias=convb[:, cb:cb + 1], scale=1.0),
                            reads=[pcc.name, "convb"], writes=[xoo.name])
                        P.dma("sp", xbcT_d[cb * 128:(cb + 1) * 128, tg * 512:(tg + 1) * 512], xoo[:],
                              reads=[xoo.name])


    def ln_tiles(self, names):
        P = self.P
        d = {}
        d["tres"] = P.sb("tres", [128, 1024], F32)
        d["sq"] = P.sb("lnsq", [128, 1024], F32)
        d["s12"] = P.sb("s12", [128, 2], F32)
        d["m2"] = P.sb("m2", [128, 1], F32)
        d["mv"] = P.sb("mv", [128, 2], F32)
        d["rstd"] = P.sb("rstd", [128, 1], F32)
        d["hn"] = P.sb("hn", [128, 1024], F32)
        d["ho"] = [P.sb("ho%d" % i, [128, 1024], F32) for i in range(2)]
        d["hob"] = [P.sb("hob%d" % i, [128, 1024], BF16) for i in range(2)]
        return d

    def ln_emit(self, L, i, lng, lnb, h_d, hb_d, gkey):
        P = self.P
        tres, mv, rstd, hn = L["tres"], L["mv"], L["rstd"], L["hn"]
        ho, hob = L["ho"][i % 2], L["hob"][i % 2]

        sq, s12, m2 = L["sq"], L["s12"], L["m2"]
        P.op("pool", lambda e: e.tensor_tensor(out=sq[:], in0=tres[:], in1=tres[:], op=ALU.mult), reads=["tres"], writes=["lnsq"])
        P.op("dve", lambda e: e.tensor_reduce(out=s12[:, 0:1], in_=tres[:], axis=AX.X, op=ALU.add), reads=["tres"], writes=["s1"])
        P.op("dve", lambda e: e.tensor_reduce(out=s12[:, 1:2], in_=sq[:], axis=AX.X, op=ALU.add), reads=["lnsq"], writes=["s2"])
        P.op("dve", lambda e: e.tensor_scalar(out=mv[:, 0:1], in0=s12[:, 0:1], scalar1=1.0 / 1024, scalar2=None, op0=ALU.mult),
             reads=["s1"], writes=["mv"])
        P.op("dve", lambda e: e.tensor_tensor(out=m2[:], in0=mv[:, 0:1], in1=mv[:, 0:1], op=ALU.mult), reads=["mv"], writes=["m2"])
        P.op("dve", lambda e: e.scalar_tensor_tensor(out=mv[:, 1:2], in0=s12[:, 1:2], scalar=1.0 / 1024, in1=m2[:],
                                                     op0=ALU.mult, op1=ALU.subtract), reads=["s2", "m2", "mv"], writes=["mv"])
        P.op("act", lambda e: e.activation(out=rstd[:], in_=mv[:, 1:2], func=AF.Sqrt, bias=EPS, scale=1.0),
             reads=["mv"], writes=["rstd"])
        P.op("dve", lambda e: e.reciprocal(out=rstd[:], in_=rstd[:]), reads=["rstd"], writes=["rstd"])
        P.op("dve", lambda e: e.tensor_scalar(out=hn[:], in0=tres[:], scalar1=mv[:, 0:1], scalar2=rstd[:, 0:1],
                                              op0=ALU.subtract, op1=ALU.mult), reads=["tres", "mv", "rstd"], writes=["hn"])
        P.op("pool", lambda e: e.tensor_tensor(out=ho[:], in0=hn[:], in1=lng[:], op=ALU.mult),
             reads=["hn", gkey + "_g"], writes=[ho.name])
        P.op("dve", lambda e: e.tensor_tensor(out=ho[:], in0=ho[:], in1=lnb[:], op=ALU.add),
             reads=[ho.name, gkey + "_b"], writes=[ho.name])
        P.dma("sp", h_d[i * 128:(i + 1) * 128, :], ho[:], reads=[ho.name])
        if hb_d is not None:
            P.op("act", lambda e: e.activation(out=hob[:], in_=ho[:], func=AF.Copy), reads=[ho.name], writes=[hob.name])
            P.dma("sp", hb_d[i * 128:(i + 1) * 128, :], hob[:], reads=[hob.name])

    def l0_ssd(self, x_d, zs_d, xbcT_d, dtd_d, w_out_d, normw_d, hp_d, cst_d, lng_d, lnb_d, h_d, hb_d):
        P, S, T = self.P, self.S, self.T
        wo3 = w_out_d.rearrange("(c p) n -> p c n", p=128)
        xbc3 = xbcT_d.rearrange("(b p) t -> p b t", p=128)
        with P.stage():
            cst = P.sb("cst", [128, 5, 128], F32)
            identb = P.sb("identb", [128, 128], BF16)
            hp = P.sb("hp", [128, 3, 32], F32)
            normw = P.sb("normw", [128, 2048], F32)
            lng = P.sb("lng", [128, 1024], F32)
            lnb = P.sb("lnb", [128, 1024], F32)
            wout = P.sb("wout", [128, 16, 1024], BF16)
            St = P.sb("St", [128, 8, 256], F32)
            prevb = P.sb("prevb", [128, 8, 256], BF16)
            xTc = [P.sb("xTc%d" % i, [128, 16, 128], BF16) for i in range(2)]
            bcT = [P.sb("bcT%d" % i, [128, 16, 128], BF16) for i in range(2)]
            dtc = [P.sb("dtc%d" % i, [128, 64], F32) for i in range(2)]
            zc = [P.sb("zc%d" % i, [128, 2048], BF16) for i in range(2)]
            xres = [P.sb("xres%d" % i, [128, 1024], F32) for i in range(2)]
            ex = [P.sb("ex%d" % i, [128, 96], F32) for i in range(2)]
            dte = [P.sb("dte%d" % i, [128, 32], F32) for i in range(2)]
            R = P.sb("R", [128, 32, 128], F32)
            xraw = P.sb("xraw", [128, 2048], BF16)
            xdt = P.sb("xdt", [128, 2048], BF16)
            xde = P.sb("xde", [128, 2048], BF16)
            xD = P.sb("xD", [128, 2048], BF16)
            Btm = P.sb("Btm", [128, 8, 128], BF16)
            dec = [P.sb("dec%d" % i, [128, 512], F32) for i in range(2)]
            cbm = [P.sb("cbm%d" % i, [128, 128], F32) for i in range(2)]
            MT = [P.sb("MT%d" % i, [128, 4, 128], BF16) for i in range(2)]
            t1 = [P.sb("t1%d" % i, [128, 256], F32) for i in range(2)]
            yg = [P.sb("yg%d" % i, [128, 256], F32) for i in range(2)]
            sqj = P.sb("sqj", [128, 256], F32)
            ss = [P.sb("ss%d" % i, [128, 1], F32) for i in range(2)]
            ygn = [P.sb("ygn%d" % i, [128, 256], BF16) for i in range(2)]
            ygT = [P.sb("ygT%d" % i, [128, 16, 128], BF16) for i in range(2)]
            L = self.ln_tiles(None)
            tres = L["tres"]
            pX = [P.ps("pX%d" % i, [128, 512], F32) for i in range(2)]
            pG = [P.ps("pG%d" % i, [128, 512], F32) for i in range(2)]
            pM = [P.ps("pM%d" % i, [128, 512], F32) for i in range(2)]
            pO = [P.ps("pO%d" % i, [128, 512], F32) for i in range(2)]
            pXb = [p[:].bitcast(BF16) for p in pX]
            tri, su, ones = cst[:, 1, :], cst[:, 2, :], cst[:, 3, :]

            P.dma("sp", cst[:], cst_d, writes=["cst"])
            P.dma("sp", hp[:], hp_d, writes=["hp"])
            P.dma("sp", normw[:], normw_d, writes=["normw"])
            P.dma("sp", lng[:], lng_d, writes=["lngb_g"])
            P.dma("sp", lnb[:], lnb_d, writes=["lngb_b"])
            for j in range(2):
                P.dma("pool", wout[:, :, j * 512:(j + 1) * 512], wo3[:, :, j * 512:(j + 1) * 512], writes=["wout%d" % j])
            P.op("dve", lambda e: e.tensor_copy(out=identb[:], in_=cst[:, 0, :]), reads=["cst"], writes=["identb"])
            P.op("pool", lambda e: e.memset(St[:], 0.0), writes=["St%d" % q for q in range(8)])

            for c in range(T):
                b = c % 2
                rows = slice(c * 128, (c + 1) * 128)
                P.dma("sp", dtc[b][:], dtd_d[rows, :], writes=[dtc[b].name])
                P.dma("sp", xTc[b][:], xbc3[:, 0:16, rows], writes=[xTc[b].name])
                P.dma("sp", bcT[b][:], xbc3[:, 16:32, rows], writes=[bcT[b].name])
                P.dma("sp", zc[b][:], zs_d[rows, :], writes=[zc[b].name])
                P.dma("sp", xres[b][:], x_d[rows, :], writes=[xres[b].name])
                dtk, xk, bk, zk, exk, dtek = dtc[b], xTc[b], bcT[b], zc[b], ex[b], dte[b]
                P.op("pool", lambda e, dtk=dtk: e.tensor_tensor(
                    out=R[:], in0=dtk[:, 32:64].unsqueeze(2).to_broadcast([128, 32, 128]),
                    in1=tri.unsqueeze(1).to_broadcast([128, 32, 128]), op=ALU.mult),
                    reads=[dtk.name, "cst"], writes=["R"])
                pa = pG[0]

                def fpa(pe, dtk=dtk, pa=pa):
                    pe.matmul(pa[:, 0:32], lhsT=tri, rhs=dtk[:, 32:64], start=True, stop=True)
                    pe.matmul(pa[:, 32:64], lhsT=su, rhs=dtk[:, 32:64], start=True, stop=True)
                    return pe.matmul(pa[:, 64:96], lhsT=ones, rhs=dtk[:, 32:64], start=True, stop=True)
                P.op("pe", fpa, reads=[dtk.name, "cst"], writes=[pa.name])
                P.op("act", lambda e, exk=exk, pa=pa: e.activation(out=exk[:], in_=pa[:, 0:96], func=AF.Exp),
                     reads=[pa.name], writes=[exk.name])
                P.op("dve", lambda e, dtek=dtek, dtk=dtk, exk=exk: e.tensor_tensor(
                    out=dtek[:], in0=dtk[:, 0:32], in1=exk[:, 32:64], op=ALU.mult),
                    reads=[dtk.name, exk.name], writes=[dtek.name])
                for hf in range(2):
                    def ftx(pe, xk=xk, hf=hf):
                        for k in range(8):
                            ins = pe.transpose(pXb[hf][:, k * 128:(k + 1) * 128], xk[:, hf * 8 + k, :], identb[:])
                        return ins
                    P.op("pe", ftx, reads=[xk.name, "identb"], writes=[pX[hf].name])
                    sl = slice(hf * 1024, (hf + 1) * 1024)
                    P.op("act", lambda e, hf=hf, sl=sl: e.activation(out=xraw[:, sl], in_=pXb[hf][:], func=AF.Copy),
                         reads=[pX[hf].name], writes=["xraw%d" % hf])
                    P.op("dve", lambda e, hf=hf, sl=sl, dtk=dtk: e.tensor_tensor(
                        out=xdt[:, sl].rearrange("p (h q) -> p h q", q=64),
                        in0=pXb[hf][:].rearrange("p (h q) -> p h q", q=64),
                        in1=dtk[:, hf * 16:(hf + 1) * 16].unsqueeze(2).to_broadcast([128, 16, 64]), op=ALU.mult),
                        reads=[pX[hf].name, dtk.name], writes=["xdt%d" % hf])
                    P.op("dve", lambda e, hf=hf, sl=sl, dtek=dtek: e.tensor_tensor(
                        out=xde[:, sl].rearrange("p (h q) -> p h q", q=64),
                        in0=pXb[hf][:].rearrange("p (h q) -> p h q", q=64),
                        in1=dtek[:, hf * 16:(hf + 1) * 16].unsqueeze(2).to_broadcast([128, 16, 64]), op=ALU.mult),
                        reads=[pX[hf].name, dtek.name], writes=["xde%d" % hf])
                    P.op("pool", lambda e, hf=hf, sl=sl: e.tensor_tensor(
                        out=xD[:, sl].rearrange("p (h q) -> p h q", q=64),
                        in0=xraw[:, sl].rearrange("p (h q) -> p h q", q=64),
                        in1=hp[:, 2, hf * 16:(hf + 1) * 16].unsqueeze(2).to_broadcast([128, 16, 64]), op=ALU.mult),
                        reads=["xraw%d" % hf, "hp"], writes=["xD%d" % hf])

                def ftb(pe, bk=bk):
                    for k in range(8):
                        ins = pe.transpose(pXb[0][:, k * 128:(k + 1) * 128], bk[:, k, :], identb[:])
                    return ins
                P.op("pe", ftb, reads=[bk.name, "identb"], writes=[pX[0].name])
                P.op("act", lambda e: e.activation(out=Btm[:], in_=pXb[0][:].rearrange("p (k c) -> p k c", c=128), func=AF.Copy),
                     reads=[pX[0].name], writes=["Btm"])
                P.op("act", lambda e: e.activation(out=prevb[:], in_=St[:], func=AF.Copy), reads=["St%d" % q for q in range(8)], writes=["prevb"])

                for g in range(NG):
                    gb = g % 2
                    pg, decg, cbmg, MTg, t1g, ygg, ssg, ygng = pG[gb], dec[gb], cbm[gb], MT[gb], t1[gb], yg[gb], ss[gb], ygn[gb]
                    hf = g // 4
                    P.op("pe", lambda pe, gb=gb, g=g, pg=pg: pe.matmul(
                        pg[:], lhsT=su, rhs=R[:, 4 * g:4 * g + 4, :], start=True, stop=True),
                        reads=["R", "cst"], writes=[pg.name])
                    P.op("act", lambda e, gb=gb, pg=pg, decg=decg: e.activation(out=decg[:], in_=pg[:], func=AF.Exp),
                         reads=[pg.name], writes=[decg.name])
                    P.op("pe", lambda pe, gb=gb, g=g, bk=bk: pe.matmul(pM[gb][:, 0:128], lhsT=bk[:, g, :], rhs=bk[:, 8 + g, :],
                                                              start=True, stop=True),
                         reads=[bk.name], writes=[pM[gb].name])
                    P.op("dve", lambda e, gb=gb, cbmg=cbmg: e.tensor_tensor(out=cbmg[:], in0=pM[gb][:, 0:128], in1=tri, op=ALU.mult),
                         reads=[pM[gb].name, "cst"], writes=[cbmg.name])
                    P.op("dve", lambda e, gb=gb, MTg=MTg, decg=decg, cbmg=cbmg: e.tensor_tensor(
                        out=MTg[:], in0=decg[:].rearrange("p (r l) -> p r l", l=128),
                        in1=cbmg[:].unsqueeze(1).to_broadcast([128, 4, 128]), op=ALU.mult),
                        reads=[decg.name, cbmg.name], writes=[MTg.name])

                    def fy(pe, gb=gb, g=g, MTg=MTg):
                        for r in range(4):
                            h = 4 * g + r
                            pe.matmul(pO[gb][:, r * 64:(r + 1) * 64], lhsT=MTg[:, r, :], rhs=xdt[:, h * 64:(h + 1) * 64],
                                      start=True, stop=False)
                            ins = pe.matmul(pO[gb][:, r * 64:(r + 1) * 64], lhsT=identb[:], rhs=xD[:, h * 64:(h + 1) * 64],
                                            start=False, stop=True)
                        return ins
                    P.op("pe", fy, reads=[MTg.name, "xdt%d" % hf, "xD%d" % hf, "identb"], writes=[pO[gb].name])
                    P.op("pe", lambda pe, gb=gb, g=g, bk=bk: pe.matmul(pM[gb][:, 128:384], lhsT=bk[:, 8 + g, :], rhs=prevb[:, g, :],
                                                              start=True, stop=True),
                         reads=[bk.name, "prevb"], writes=[pM[gb].name])
                    P.op("pe", lambda pe, gb=gb, g=g: pe.matmul(pO[gb][:, 256:512], lhsT=Btm[:, g, :], rhs=xde[:, g * 256:(g + 1) * 256],
                                                       start=True, stop=True),
                         reads=["Btm", "xde%d" % hf], writes=[pO[gb].name])
                    P.op("dve", lambda e, gb=gb, g=g, t1g=t1g, exk=exk: e.tensor_tensor(
                        out=t1g[:].rearrange("p (r q) -> p r q", q=64),
                        in0=pM[gb][:, 128:384].rearrange("p (r q) -> p r q", q=64),
                        in1=exk[:, 4 * g:4 * g + 4].unsqueeze(2).to_broadcast([128, 4, 64]), op=ALU.mult),
                        reads=[pM[gb].name, exk.name], writes=[t1g.name])
                    P.op("dve", lambda e, gb=gb, t1g=t1g: e.tensor_tensor(out=t1g[:], in0=pO[gb][:, 0:256], in1=t1g[:], op=ALU.add),
                         reads=[pO[gb].name, t1g.name], writes=[t1g.name])
                    if "ydbg" in self.dbg:
                        P.dma("sp", self.ydbg[rows, g * 256:(g + 1) * 256], t1g[:], reads=[t1g.name])
                    P.op("dve", lambda e, gb=gb, g=g, t1g=t1g, ygg=ygg, zk=zk: e.tensor_tensor(
                        out=ygg[:], in0=t1g[:], in1=zk[:, g * 256:(g + 1) * 256], op=ALU.mult),
                        reads=[t1g.name, zk.name], writes=[ygg.name])
                    P.op("pool", lambda e, gb=gb, ygg=ygg: e.tensor_tensor(out=sqj[:], in0=ygg[:], in1=ygg[:], op=ALU.mult),
                         reads=[ygg.name], writes=["sqj"])
                    P.op("dve", lambda e, gb=gb, ssg=ssg: e.tensor_reduce(out=ssg[:], in_=sqj[:], axis=AX.X, op=ALU.add),
                         reads=["sqj"], writes=[ssg.name])
                    P.op("act", lambda e, gb=gb, ssg=ssg: e.activation(out=ssg[:], in_=ssg[:], func=AF.Sqrt, bias=EPS, scale=1.0 / 256),
                         reads=[ssg.name], writes=[ssg.name])
                    P.op("dve", lambda e, gb=gb, ssg=ssg: e.reciprocal(out=ssg[:], in_=ssg[:]), reads=[ssg.name], writes=[ssg.name])
                    P.op("dve", lambda e, gb=gb, g=g, ygg=ygg, ssg=ssg, ygng=ygng: e.scalar_tensor_tensor(
                        out=ygng[:], in0=ygg[:], scalar=ssg[:, 0:1], in1=normw[:, g * 256:(g + 1) * 256],
                        op0=ALU.mult, op1=ALU.mult), reads=[ygg.name, ssg.name, "normw"], writes=[ygng.name])
                    P.op("dve", lambda e, gb=gb, g=g, exk=exk: e.tensor_tensor(
                        out=St[:, g, :].rearrange("p (r q) -> p r q", q=64),
                        in0=St[:, g, :].rearrange("p (r q) -> p r q", q=64),
                        in1=exk[:, 64 + 4 * g:64 + 4 * g + 4].unsqueeze(2).to_broadcast([128, 4, 64]), op=ALU.mult),
                        reads=["St%d" % g, exk.name], writes=["St%d" % g])
                    P.op("dve", lambda e, gb=gb, g=g: e.tensor_tensor(out=St[:, g, :], in0=St[:, g, :], in1=pO[gb][:, 256:512], op=ALU.add),
                         reads=["St%d" % g, pO[gb].name], writes=["St%d" % g])

                    def fty(pe, gb=gb, ygng=ygng):
                        for j in range(2):
                            ins = pe.transpose(pXb[1][:, j * 128:(j + 1) * 128], ygng[:, j * 128:(j + 1) * 128], identb[:])
                        return ins
                    P.op("pe", fty, reads=[ygng.name, "identb"], writes=[pX[1].name])
                    P.op("act", lambda e, gb=gb, g=g, b=b: e.activation(
                        out=ygT[b][:, 2 * g:2 * g + 2, :], in_=pXb[1][:, 0:256].rearrange("p (j c) -> p j c", c=128), func=AF.Copy),
                        reads=[pX[1].name], writes=[ygT[b].name])

                for dh in range(2):
                    def fo(pe, b=b, dh=dh):
                        for cb in range(16):
                            ins = pe.matmul(pG[dh][:], lhsT=ygT[b][:, cb, :], rhs=wout[:, cb, dh * 512:(dh + 1) * 512],
                                            start=(cb == 0), stop=(cb == 15))
                        return ins
                    P.op("pe", fo, reads=[ygT[b].name, "wout%d" % dh], writes=[pG[dh].name])
                    P.op("dve", lambda e, dh=dh, b=b: e.scalar_tensor_tensor(
                        out=tres[:, dh * 512:(dh + 1) * 512], in0=xres[b][:, dh * 512:(dh + 1) * 512], scalar=ALPHA,
                        in1=pG[dh][:], op0=ALU.mult, op1=ALU.add), reads=[xres[b].name, pG[dh].name], writes=["tres"])
                if "tdbg" in self.dbg:
                    P.dma("sp", self.tdbg[rows, :], tres[:], reads=["tres"])
                self.ln_emit(L, c, lng, lnb, h_d, hb_d, "lngb")


    def moe(self, h_d, hb_d, wr_d, wg_d, wu_d, wd_d, cst_d, erow_d, lng_d, lnb_d, xpad_d, ypad_d, ho_d, hob_d, C):
        P, S, T = self.P, self.S, self.T
        NSL = 32 * C
        wr3 = wr_d.rearrange("(c p) n -> p c n", p=128)
        with P.stage():
            cst = P.sb("cst", [128, 5, 128], F32)
            identb = P.sb("identb", [128, 128], BF16)
            onesb = P.sb("onesb", [128, 128], BF16)
            slb = P.sb("slb", [128, 128], BF16)
            erow = P.sb("erow", [128, 32], F32)
            lng = P.sb("lng", [128, 1024], F32)
            lnb = P.sb("lnb", [128, 1024], F32)
            wr = P.sb("wr", [128, 8, 36], F32)
            cnt = P.sb("cnt", [128, 32], F32)
            slots = P.sb("slots", [128, T, 2], I32)
            wts = P.sb("wts", [128, T, 2], F32)
            hin = [P.sb("hin%d" % i, [128, 1024], F32) for i in range(2)]
            hbt = [P.sb("hbt%d" % i, [128, 1024], BF16) for i in range(2)]
            hT32 = P.sb("hT32", [128, 8, 128], F32)
            lg = P.sb("lg", [128, 36], F32)
            sm = P.sb("sm", [128, 16], F32)
            ohg = P.sb("ohg", [128, 4], F32)
            ge = P.sb("ge", [128, 4], F32)
            pen = P.sb("pen", [128, 4], F32)
            msk = P.sb("msk", [128, 32], F32)
            top8 = P.sb("top8", [128, 8], F32)
            sel = P.sb("sel", [128, 32], F32)
            selb = P.sb("selb", [128, 32], BF16)
            is1 = P.sb("is1", [128, 32], F32)
            is2 = P.sb("is2", [128, 32], F32)
            rk = P.sb("rk", [128, 32], F32)
            val = P.sb("val", [128, 32], F32)
            vld = P.sb("vld", [128, 32], F32)
            tmp32 = P.sb("tmp32", [128, 32], F32)
            slf = P.sb("slf", [128, 2], F32)
            wgt = [P.sb("wgt%d" % i, [128, 8, 512], BF16) for i in range(2)]
            wut = [P.sb("wut%d" % i, [128, 8, 512], BF16) for i in range(2)]
            wdt = [P.sb("wdt%d" % i, [128, 4, 1024], BF16) for i in range(2)]
            wstage = [P.sb("wstage%d" % i, [128, 4, 512], F32) for i in range(3)] if MOE_STAGED_WEIGHTS else None
            xs = [P.sb("xs%d" % i, [128, 1024], BF16) for i in range(2)]
            xsT = P.sb("xsT", [128, 8, C], BF16)
            sg = [P.sb("sg%d" % i, [128, C], F32) for i in range(2)]
            hh = P.sb("hh", [128, 4, C], BF16)
            yo = [P.sb("yo%d" % i, [128, 1024], BF16) for i in range(2)]
            yA = [P.sb("yA%d" % i, [128, 1024], BF16) for i in range(2)]
            yB = [P.sb("yB%d" % i, [128, 1024], BF16) for i in range(2)]
            L = self.ln_tiles(None)
            tres = L["tres"]
            pA = [P.ps("pA%d" % i, [128, 512], F32) for i in range(2)]
            pB = [P.ps("pB%d" % i, [128, 512], F32) for i in range(2)]
            pC = [P.ps("pC%d" % i, [128, 512], F32) for i in range(2)]
            pD = [P.ps("pD%d" % i, [128, 512], F32) for i in range(2)]
            pDb = pD[0][:].bitcast(BF16)

            P.dma("sp", cst[:], cst_d, writes=["cst"])
            P.dma("sp", erow[:], erow_d, writes=["erow"])
            P.dma("sp", lng[:], lng_d, writes=["lngb_g"])
            P.dma("sp", lnb[:], lnb_d, writes=["lngb_b"])
            P.dma("sp", wr[:], wr3, writes=["wr"])
            P.op("dve", lambda e: e.tensor_copy(out=identb[:], in_=cst[:, 0, :]), reads=["cst"], writes=["identb"])
            P.op("dve", lambda e: e.tensor_copy(out=onesb[:], in_=cst[:, 3, :]), reads=["cst"], writes=["onesb"])
            P.op("dve", lambda e: e.tensor_copy(out=slb[:], in_=cst[:, 4, :]), reads=["cst"], writes=["slb"])
            P.op("pool", lambda e: e.memset(cnt[:], 0.0), writes=["cnt"])
            for i in range(2):
                P.op("pool", lambda e, i=i: e.memset(yA[i][:], 0.0), writes=[yA[i].name])
                P.op("pool", lambda e, i=i: e.memset(yB[i][:], 0.0), writes=[yB[i].name])

            for i in range(T):
                b = i % 2
                rows = slice(i * 128, (i + 1) * 128)
                hi, hb = hin[b], hbt[b]
                P.dma("sp", hi[:], h_d[rows, :], writes=[hi.name])
                P.dma("sp", hb[:], hb_d[rows, :], writes=[hb.name])
                for hf in range(2):
                    def ftr(pe, hi=hi, hf=hf):
                        for k in range(4):
                            kk = hf * 4 + k
                            ins = pe.transpose(pA[hf][:, k * 128:(k + 1) * 128], hi[:, kk * 128:(kk + 1) * 128], cst[:, 0, :])
                        return ins
                    P.op("pe", ftr, reads=[hi.name, "cst"], writes=[pA[hf].name])
                    P.op("act", lambda e, hf=hf: e.activation(out=hT32[:, hf * 4:(hf + 1) * 4, :],
                                                               in_=pA[hf][:].rearrange("p (k c) -> p k c", c=128), func=AF.Copy),
                         reads=[pA[hf].name], writes=["hT32_%d" % hf])

                def frt(pe):
                    for k in range(8):
                        ins = pe.matmul(pB[0][:, 0:36], lhsT=hT32[:, k, :], rhs=wr[:, k, :], start=(k == 0), stop=(k == 7))
                    return ins
                P.op("pe", frt, reads=["hT32_0", "hT32_1", "wr"], writes=[pB[0].name])
                P.op("act", lambda e: e.activation(out=lg[:], in_=pB[0][:, 0:36], func=AF.Copy), reads=[pB[0].name], writes=["lg"])
                P.op("dve", lambda e: e.tensor_reduce(out=sm[:, 0:1], in_=lg[:, 0:4], axis=AX.X, op=ALU.max), reads=["lg"], writes=["sm0"])
                P.op("dve", lambda e: e.tensor_scalar(out=ohg[:], in0=lg[:, 0:4], scalar1=sm[:, 0:1], scalar2=None, op0=ALU.is_equal),
                     reads=["lg", "sm0"], writes=["ohg"])
                P.op("dve", lambda e: e.tensor_scalar(out=sm[:, 1:2], in0=sm[:, 0:1], scalar1=-1.0, scalar2=None, op0=ALU.mult),
                     reads=["sm0"], writes=["sm1"])
                P.op("act", lambda e: e.activation(out=ge[:], in_=lg[:, 0:4], func=AF.Exp, bias=sm[:, 1:2], scale=1.0),
                     reads=["lg", "sm1"], writes=["ge"])
                P.op("dve", lambda e: e.tensor_reduce(out=sm[:, 2:3], in_=ge[:], axis=AX.X, op=ALU.add), reads=["ge"], writes=["sm2"])
                P.op("dve", lambda e: e.reciprocal(out=sm[:, 3:4], in_=sm[:, 2:3]), reads=["sm2"], writes=["sm3"])
                P.op("dve", lambda e: e.tensor_scalar(out=pen[:], in0=ohg[:], scalar1=1.0, scalar2=1e30, op0=ALU.subtract, op1=ALU.mult),
                     reads=["ohg"], writes=["pen"])
                P.op("dve", lambda e: e.tensor_tensor(out=msk[:].rearrange("p (g j) -> p g j", j=8),
                                                      in0=lg[:, 4:36].rearrange("p (g j) -> p g j", j=8),
                                                      in1=pen[:].unsqueeze(2).to_broadcast([128, 4, 8]), op=ALU.add),
                     reads=["lg", "pen"], writes=["msk"])
                P.op("dve", lambda e: e.max(out=top8[:], in_=msk[:]), reads=["msk"], writes=["top8"])
                P.op("dve", lambda e: e.tensor_scalar(out=sel[:], in0=msk[:], scalar1=top8[:, 1:2], scalar2=None, op0=ALU.is_ge),
                     reads=["msk", "top8"], writes=["sel"])
                P.op("dve", lambda e: e.tensor_copy(out=selb[:], in_=sel[:]), reads=["sel"], writes=["selb"])
                P.op("dve", lambda e: e.tensor_scalar(out=is1[:], in0=msk[:], scalar1=top8[:, 0:1], scalar2=None, op0=ALU.is_equal),
                     reads=["msk", "top8"], writes=["is1"])
                P.op("dve", lambda e: e.tensor_tensor(out=is2[:], in0=sel[:], in1=is1[:], op=ALU.subtract), reads=["sel", "is1"], writes=["is2"])
                P.op("dve", lambda e: e.tensor_tensor(out=sm[:, 4:5], in0=top8[:, 0:1], in1=top8[:, 1:2], op=ALU.subtract),
                     reads=["top8"], writes=["sm4"])
                P.op("act", lambda e: e.activation(out=sm[:, 5:6], in_=sm[:, 4:5], func=AF.Sigmoid), reads=["sm4"], writes=["sm5"])
                def frk(pe):
                    pe.matmul(pB[1][:, 0:32], lhsT=slb[:], rhs=selb[:], start=True, stop=True)
                    return pe.matmul(pB[1][:, 32:64], lhsT=onesb[:], rhs=selb[:], start=True, stop=True)
                P.op("pe", frk, reads=["slb", "onesb", "selb"], writes=[pB[1].name])
                P.op("dve", lambda e: e.tensor_tensor(out=rk[:], in0=pB[1][:, 0:32], in1=cnt[:], op=ALU.add),
                     reads=[pB[1].name, "cnt"], writes=["rk"])
                P.op("dve", lambda e: e.tensor_tensor(out=cnt[:], in0=pB[1][:, 32:64], in1=cnt[:], op=ALU.add),
                     reads=[pB[1].name, "cnt", "rk"], writes=["cnt"])
                P.op("dve", lambda e: e.tensor_scalar(out=vld[:], in0=rk[:], scalar1=float(C), scalar2=None, op0=ALU.is_lt),
                     reads=["rk"], writes=["vld"])
                P.op("dve", lambda e: e.tensor_tensor(out=val[:], in0=rk[:], in1=erow[:], op=ALU.add), reads=["rk", "erow"], writes=["val"])
                BIG = float(4 * NSL)
                P.op("dve", lambda e: e.scalar_tensor_tensor(out=val[:], in0=val[:], scalar=-BIG, in1=vld[:], op0=ALU.add, op1=ALU.mult),
                     reads=["val", "vld"], writes=["val"])
                P.op("dve", lambda e: e.tensor_scalar(out=val[:], in0=val[:], scalar1=BIG, scalar2=None, op0=ALU.add),
                     reads=["val"], writes=["val"])
                for q, isq in ((0, is1), (1, is2)):
                    P.op("dve", lambda e, isq=isq: e.tensor_tensor(out=tmp32[:], in0=isq[:], in1=val[:], op=ALU.mult),
                         reads=["is1", "is2", "val"], writes=["tmp32"])
                    P.op("dve", lambda e, q=q: e.tensor_reduce(out=slf[:, q:q + 1], in_=tmp32[:], axis=AX.X, op=ALU.add),
                         reads=["tmp32"], writes=["slf%d" % q])
                    P.op("dve", lambda e, isq=isq: e.tensor_tensor(out=tmp32[:], in0=isq[:], in1=vld[:], op=ALU.mult),
                         reads=["is1", "is2", "vld", "slf%d" % q], writes=["tmp32"])
                    P.op("dve", lambda e, q=q: e.tensor_reduce(out=sm[:, 8 + q:9 + q], in_=tmp32[:], axis=AX.X, op=ALU.add),
                         reads=["tmp32"], writes=["sm%d" % (8 + q)])
                P.op("dve", lambda e, i=i: e.tensor_copy(out=slots[:, i, :], in_=slf[:]), reads=["slf0", "slf1"], writes=["slots%d" % i])
                P.op("dve", lambda e: e.tensor_tensor(out=sm[:, 6:7], in0=sm[:, 3:4], in1=sm[:, 5:6], op=ALU.mult),
                     reads=["sm3", "sm5"], writes=["sm6"])
                P.op("dve", lambda e: e.tensor_tensor(out=sm[:, 7:8], in0=sm[:, 3:4], in1=sm[:, 6:7], op=ALU.subtract),
                     reads=["sm3", "sm6"], writes=["sm7"])
                P.op("dve", lambda e, i=i: e.tensor_tensor(out=wts[:, i, :], in0=sm[:, 6:8], in1=sm[:, 8:10], op=ALU.mult),
                     reads=["sm6", "sm7", "sm8", "sm9"], writes=["wts%d" % i])
                for q in range(2):
                    P.dma("pool", None, None, reads=["slots%d" % i, hb.name], writes=["xsc_%d_%d" % (i, q)],
                          indirect=lambda g, i=i, q=q, hb=hb: g.indirect_dma_start(
                        out=xpad_d[:, :], out_offset=bass.IndirectOffsetOnAxis(ap=slots[:, i, q:q + 1], axis=0),
                        in_=hb[:], in_offset=None, bounds_check=P.reg(g, NSL - 1), oob_is_err=False))

            P.op("pool", lambda e: e.memset(tmp32[:], 0.0), reads=["xsc_%d_%d" % (i, q) for i in range(T) for q in range(2)],
                 writes=["xpad_ready", "tmp32"])

            wg4 = wg_d.rearrange("e (c p) f -> e p c f", p=128)
            wu4 = wu_d.rearrange("e (c p) f -> e p c f", p=128)
            wd4 = wd_d.rearrange("e (c p) d -> e p c d", p=128)
            NST = C // 128
            npiece = [0]

            def load_w(ex):
                b = ex % 2
                pieces = []
                for hk in range(2):
                    pieces.append((wgt[b][:, hk * 4:(hk + 1) * 4, :], wg4[ex][:, hk * 4:(hk + 1) * 4, :], "%s_%d" % (wgt[b].name, hk)))
                for hk in range(2):
                    pieces.append((wut[b][:, hk * 4:(hk + 1) * 4, :], wu4[ex][:, hk * 4:(hk + 1) * 4, :], "%s_%d" % (wut[b].name, hk)))
                for hk in range(2):
                    pieces.append((wdt[b][:, hk * 2:(hk + 1) * 2, :], wd4[ex][:, hk * 2:(hk + 1) * 2, :], "%s_%d" % (wdt[b].name, hk)))
                for pi, (dst, srcap, key) in enumerate(pieces):
                    n = npiece[0]
                    npiece[0] += 1
                    stg = wstage[n % 3]
                    sv = stg[:] if pi < 4 else stg[:].rearrange("p a b -> p (a b)").rearrange("p (a b) -> p a b", b=1024)
                    P.dma("sp", sv, srcap, writes=[stg.name])
                    eng = "pool" if n % 2 == 0 else "dve"
                    P.op(eng, lambda e, dst=dst, sv=sv: e.tensor_copy(out=dst, in_=sv), reads=[stg.name], writes=[key])

            for ex in range(32):
                b = ex % 2
                wg, wu, wd = wgt[b], wut[b], wdt[b]
                if MOE_STAGED_WEIGHTS:
                    if ex == 0:
                        load_w(0)
                    if ex + 1 < 32:
                        load_w(ex + 1)
                else:
                    P.dma("pool", wg[:], wg4[ex], writes=[wg.name + "_0", wg.name + "_1"])
                    P.dma("pool", wu[:], wu4[ex], writes=[wu.name + "_0", wu.name + "_1"])
                    P.dma("pool", wd[:], wd4[ex], writes=[wd.name + "_0", wd.name + "_1"])
                for st in range(NST):
                    xx = xs[st % 2]
                    r0 = ex * C + st * 128
                    P.dma("sp", xx[:], xpad_d[r0:r0 + 128, :], reads=["xpad_ready"], writes=[xx.name])

                    def ftx(pe, xx=xx):
                        for k in range(8):
                            ins = pe.transpose(pDb[:, k * 128:(k + 1) * 128], xx[:, k * 128:(k + 1) * 128], identb[:])
                        return ins
                    P.op("pe", ftx, reads=[xx.name, "identb"], writes=[pD[0].name])
                    P.op("act", lambda e, st=st: e.activation(out=xsT[:, :, st * 128:(st + 1) * 128],
                                                               in_=pDb[:].rearrange("p (k c) -> p k c", c=128), func=AF.Copy),
                         reads=[pD[0].name], writes=["xsT%d" % st])
                xk = ["xsT%d" % st for st in range(NST)]
                for fb in range(4):
                    pg, pu, sgg = pA[fb % 2], pB[fb % 2], sg[fb % 2]

                    def fgu(pe, wg=wg, wu=wu, fb=fb, pg=pg, pu=pu):
                        for k in range(8):
                            pe.matmul(pg[:, 0:C], lhsT=wg[:, k, fb * 128:(fb + 1) * 128], rhs=xsT[:, k, :], start=(k == 0), stop=(k == 7))
                        for k in range(8):
                            ins = pe.matmul(pu[:, 0:C], lhsT=wu[:, k, fb * 128:(fb + 1) * 128], rhs=xsT[:, k, :], start=(k == 0), stop=(k == 7))
                        return ins
                    P.op("pe", fgu, reads=[wg.name + "_0", wg.name + "_1", wu.name + "_0", wu.name + "_1"] + xk, writes=[pg.name, pu.name])
                    P.op("act", lambda e, pg=pg, sgg=sgg: e.activation(out=sgg[:], in_=pg[:, 0:C], func=AF.Silu),
                         reads=[pg.name], writes=[sgg.name])
                    P.op("dve", lambda e, fb=fb, pu=pu, sgg=sgg: e.tensor_tensor(out=hh[:, fb, :], in0=sgg[:], in1=pu[:, 0:C], op=ALU.mult),
                         reads=[sgg.name, pu.name], writes=["hh%d" % fb])
                for st in range(NST):
                    yy = yo[st % 2]
                    for dh in range(2):
                        def fdn(pe, st=st, dh=dh, wd=wd):
                            for fb in range(4):
                                ins = pe.matmul(pC[dh][:], lhsT=hh[:, fb, st * 128:(st + 1) * 128], rhs=wd[:, fb, dh * 512:(dh + 1) * 512],
                                                start=(fb == 0), stop=(fb == 3))
                            return ins
                        P.op("pe", fdn, reads=[wd.name + "_0", wd.name + "_1"] + ["hh%d" % fb for fb in range(4)], writes=[pC[dh].name])
                        P.op("act", lambda e, yy=yy, dh=dh: e.activation(out=yy[:, dh * 512:(dh + 1) * 512], in_=pC[dh][:], func=AF.Copy),
                             reads=[pC[dh].name], writes=[yy.name])
                    r0 = ex * C + st * 128
                    P.dma("sp", ypad_d[r0:r0 + 128, :], yy[:], reads=[yy.name], writes=["ypad_%d_%d" % (ex, st)])

            P.op("pool", lambda e: e.memset(tmp32[:], 0.0), reads=["ypad_%d_%d" % (ex, st) for ex in range(32) for st in range(NST)],
                 writes=["ypad_ready", "tmp32"])

            for i in range(T):
                b = i % 2
                rows = slice(i * 128, (i + 1) * 128)
                hi = hin[b]
                P.dma("sp", hi[:], h_d[rows, :], writes=[hi.name])
                for q, yq in ((0, yA[b]), (1, yB[b])):
                    P.dma("pool", None, None, reads=["ypad_ready", "slots%d" % i], writes=[yq.name],
                          indirect=lambda g, i=i, q=q, yq=yq: g.indirect_dma_start(
                              out=yq[:], out_offset=None, in_=ypad_d[:, :],
                              in_offset=bass.IndirectOffsetOnAxis(ap=slots[:, i, q:q + 1], axis=0),
                              bounds_check=P.reg(g, NSL - 1), oob_is_err=False))
                P.op("act", lambda e, hi=hi: e.activation(out=tres[:], in_=hi[:], func=AF.Copy, scale=ALPHA), reads=[hi.name], writes=["tres"])
                for q, yq in ((0, yA[b]), (1, yB[b])):
                    P.op("dve", lambda e, i=i, q=q, yq=yq: e.scalar_tensor_tensor(
                        out=tres[:], in0=yq[:], scalar=wts[:, i, q:q + 1], in1=tres[:], op0=ALU.mult, op1=ALU.add),
                        reads=[yq.name, "wts%d" % i, "tres"], writes=["tres"])
                self.ln_emit(L, i, lng, lnb, ho_d, hob_d, "lngb")


    def attn(self, h_d, hb_d, wqkv_d, wo_d, pos_d, invr_d, lamv_d, subw_d, cst_d, lng_d, lnb_d,
             qT_d, kT_d, v_d, on_d, ho_d, hob_d, lambda_init):
        P, S, T = self.P, self.S, self.T
        TWO_PI = 2.0 * math.pi
        MAGIC = 12582912.0
        wq3 = wqkv_d.rearrange("(c p) n -> p c n", p=128)
        wo3 = wo_d.rearrange("(c p) n -> p c n", p=128)
        qT3 = qT_d.rearrange("(j p) t -> p j t", p=128)
        kT3 = kT_d.rearrange("(j p) t -> p j t", p=128)
        v3 = v_d.rearrange("(t p) c -> p t c", p=128)

        with P.stage():
            cst = P.sb("cst", [128, 5, 128], F32)
            identb = P.sb("identb", [128, 128], BF16)
            invr = P.sb("invr", [128, 8], F32)
            wqkv = P.sb("wqkv", [128, 8, 3072], BF16)
            hbt = [P.sb("hbt%d" % i, [128, 1024], BF16) for i in range(2)]
            hTt = P.sb("hTt", [128, 8, 128], BF16)
            qkv = P.sb("qkv", [128, 3072], F32)
            posi = [P.sb("posi%d" % i, [128, 1], I32) for i in range(2)]
            posf = P.sb("posf", [128, 1], F32)
            a16 = P.sb("a16", [128, 16], F32)
            kk = P.sb("kk", [128, 16], F32)
            sc16 = P.sb("sc16", [128, 16], F32)
            tt4 = [P.sb("tt%d" % i, [128, 32, 8], F32) for i in range(4)]
            qkb = P.sb("qkb", [128, 2048], BF16)
            vb = [P.sb("vb%d" % i, [128, 1024], BF16) for i in range(2)]
            qkT = [P.sb("qkT%d" % i, [128, 16, 128], BF16) for i in range(2)]
            pT_ = [P.ps("pT%d" % i, [128, 512], F32) for i in range(2)]
            pQ = [P.ps("pQ%d" % i, [128, 512], F32) for i in range(2)]
            pTb = [p[:].bitcast(BF16) for p in pT_]

            P.dma("sp", cst[:], cst_d, writes=["cst"])
            P.dma("sp", invr[:], invr_d, writes=["invr"])
            P.op("dve", lambda e: e.tensor_copy(out=identb[:], in_=cst[:, 0, :]), reads=["cst"], writes=["identb"])
            for j in range(6):
                P.dma("pool", wqkv[:, :, j * 512:(j + 1) * 512], wq3[:, :, j * 512:(j + 1) * 512], writes=["wqkv%d" % j])
            for i in range(T):
                b = i % 2
                rows = slice(i * 128, (i + 1) * 128)
                hb = hbt[b]
                P.dma("sp", hb[:], hb_d[rows, :], writes=[hb.name])
                P.dma("sp", posi[b][:], pos_d[rows, :], writes=[posi[b].name])

                def fth(pe, hb=hb):
                    for k in range(8):
                        ins = pe.transpose(pTb[0][:, k * 128:(k + 1) * 128], hb[:, k * 128:(k + 1) * 128], identb[:])
                    return ins
                P.op("pe", fth, reads=[hb.name, "identb"], writes=[pT_[0].name])
                P.op("act", lambda e: e.activation(out=hTt[:], in_=pTb[0][:].rearrange("p (k c) -> p k c", c=128), func=AF.Copy),
                     reads=[pT_[0].name], writes=["hTt"])
                for cbk in range(6):
                    pq = pQ[cbk % 2]

                    def fq(pe, cbk=cbk, pq=pq):
                        for k in range(8):
                            ins = pe.matmul(pq[:], lhsT=hTt[:, k, :], rhs=wqkv[:, k, cbk * 512:(cbk + 1) * 512], start=(k == 0), stop=(k == 7))
                        return ins
                    P.op("pe", fq, reads=["hTt", "wqkv%d" % cbk], writes=[pq.name])
                    P.op("act", lambda e, cbk=cbk, pq=pq: e.activation(out=qkv[:, cbk * 512:(cbk + 1) * 512], in_=pq[:], func=AF.Copy),
                         reads=[pq.name], writes=["qkv%d" % cbk])
                P.op("dve", lambda e, b=b: e.tensor_copy(out=posf[:], in_=posi[b][:]), reads=[posi[b].name], writes=["posf"])
                P.op("dve", lambda e: e.tensor_scalar(out=a16[:, 0:8], in0=invr[:], scalar1=posf[:, 0:1], scalar2=None, op0=ALU.mult),
                     reads=["invr", "posf"], writes=["a16a"])
                P.op("dve", lambda e: e.tensor_scalar(out=a16[:, 8:16], in0=a16[:, 0:8], scalar1=0.5 * math.pi, scalar2=None, op0=ALU.add),
                     reads=["a16a"], writes=["a16b"])
                P.op("dve", lambda e: e.tensor_scalar(out=kk[:], in0=a16[:], scalar1=1.0 / TWO_PI, scalar2=MAGIC, op0=ALU.mult, op1=ALU.add),
                     reads=["a16a", "a16b"], writes=["kk"])
                P.op("dve", lambda e: e.tensor_scalar(out=kk[:], in0=kk[:], scalar1=-MAGIC, scalar2=None, op0=ALU.add), reads=["kk"], writes=["kk"])
                P.op("dve", lambda e: e.scalar_tensor_tensor(out=kk[:], in0=kk[:], scalar=-TWO_PI, in1=a16[:], op0=ALU.mult, op1=ALU.add),
                     reads=["kk", "a16a", "a16b"], writes=["kk"])
                P.op("dve", lambda e: e.tensor_scalar(out=kk[:], in0=kk[:], scalar1=-math.pi, scalar2=math.pi, op0=ALU.max, op1=ALU.min),
                     reads=["kk"], writes=["kk"])
                P.op("act", lambda e: e.activation(out=sc16[:], in_=kk[:], func=AF.Sin), reads=["kk"], writes=["sc16"])
                qk3 = qkv[:, 0:2048].rearrange("p (g d) -> p g d", d=64)
                r1, r2 = qk3[:, :, 0:8], qk3[:, :, 8:16]
                sinb = sc16[:, 0:8].unsqueeze(1).to_broadcast([128, 32, 8])
                cosb = sc16[:, 8:16].unsqueeze(1).to_broadcast([128, 32, 8])
                qkeys = ["qkv%d" % j for j in range(4)]
                for n, (aa, bb) in enumerate(((r1, cosb), (r2, sinb), (r2, cosb), (r1, sinb))):
                    P.op("dve", lambda e, n=n, aa=aa, bb=bb: e.tensor_tensor(out=tt4[n][:], in0=aa, in1=bb, op=ALU.mult),
                         reads=qkeys + ["sc16"], writes=[tt4[n].name])
                P.op("dve", lambda e: e.tensor_tensor(out=r1, in0=tt4[0][:], in1=tt4[1][:], op=ALU.subtract),
                     reads=[tt4[0].name, tt4[1].name, tt4[2].name, tt4[3].name], writes=qkeys)
                P.op("dve", lambda e: e.tensor_tensor(out=r2, in0=tt4[2][:], in1=tt4[3][:], op=ALU.add),
                     reads=[tt4[2].name, tt4[3].name], writes=qkeys)
                P.op("act", lambda e: e.activation(out=qkb[:], in_=qkv[:, 0:2048], func=AF.Copy), reads=qkeys, writes=["qkb"])
                P.op("act", lambda e, b=b: e.activation(out=vb[b][:], in_=qkv[:, 2048:3072], func=AF.Copy),
                     reads=["qkv4", "qkv5"], writes=[vb[b].name])
                P.dma("sp", v_d[rows, :], vb[b][:], reads=[vb[b].name])
                for hf in range(2):
                    def ftq(pe, hf=hf):
                        for k in range(8):
                            j = hf * 8 + k
                            ins = pe.transpose(pTb[hf][:, k * 128:(k + 1) * 128], qkb[:, j * 128:(j + 1) * 128], identb[:])
                        return ins
                    P.op("pe", ftq, reads=["qkb", "identb"], writes=[pT_[hf].name])
                    P.op("act", lambda e, hf=hf, b=b: e.activation(out=qkT[b][:, hf * 8:(hf + 1) * 8, :],
                                                                   in_=pTb[hf][:].rearrange("p (k c) -> p k c", c=128), func=AF.Copy),
                         reads=[pT_[hf].name], writes=["%s_%d" % (qkT[b].name, hf)])
                P.dma("sp", qT3[:, :, rows], qkT[b][:, 0:8, :], reads=["%s_0" % qkT[b].name])
                P.dma("sp", kT3[:, :, rows], qkT[b][:, 8:16, :], reads=["%s_1" % qkT[b].name])

        with P.stage():
            cst = P.sb("cst", [128, 5, 128], F32)
            trib = P.sb("trib", [128, 128], BF16)
            lamv = P.sb("lamv", [128, 4, 64], F32)
            lpr = P.sb("lpr", [128, 2, 64], F32)
            ls = P.sb("ls", [128, 4], F32)
            subw = P.sb("subw", [128, 128], F32)
            KT = [P.sb("KT%d" % i, [128, S], BF16) for i in range(2)]
            QT = [P.sb("QT%d" % i, [128, S], BF16) for i in range(2)]
            Vx = [P.sb("Vx%d" % i, [128, T, 129], BF16) for i in range(2)]
            pTt = [P.sb("pTt%d" % i, [128, 512], BF16) for i in range(4)]
            rr = [P.sb("rr%d" % i, [128, 2], F32) for i in range(2)]
            oA = [P.sb("oA%d" % i, [128, 128], F32) for i in range(2)]
            oo = [P.sb("oo%d" % i, [128, 128], F32) for i in range(2)]
            osq = P.sb("osq", [128, 128], F32)
            oss = [P.sb("oss%d" % i, [128, 1], F32) for i in range(2)]
            onb = [P.sb("onb%d" % i, [128, 128], BF16) for i in range(2)]
            pS = [P.ps("pS%d" % i, [128, 512], F32) for i in range(4)]
            pO = [P.ps("pO%d" % i, [128, 512], F32) for i in range(4)]
            scale = 64 ** -0.5

            P.dma("sp", cst[:], cst_d, writes=["cst"])
            P.dma("sp", lamv[:], lamv_d, writes=["lamv"])
            P.dma("sp", subw[:], subw_d, writes=["subw"])
            P.op("dve", lambda e: e.tensor_copy(out=trib[:], in_=cst[:, 1, :]), reads=["cst"], writes=["trib"])
            P.op("dve", lambda e: e.tensor_scalar(out=subw[:], in0=subw[:], scalar1=1.0 - lambda_init, scalar2=None, op0=ALU.mult),
                 reads=["subw"], writes=["subw"])
            P.op("dve", lambda e: e.tensor_tensor(out=lpr[:, 0, :], in0=lamv[:, 0, :], in1=lamv[:, 1, :], op=ALU.mult), reads=["lamv"], writes=["lpr0"])
            P.op("dve", lambda e: e.tensor_tensor(out=lpr[:, 1, :], in0=lamv[:, 2, :], in1=lamv[:, 3, :], op=ALU.mult), reads=["lamv"], writes=["lpr1"])
            P.op("dve", lambda e: e.tensor_reduce(out=ls[:, 0:2], in_=lpr[:], axis=AX.X, op=ALU.add), reads=["lpr0", "lpr1"], writes=["ls"])
            P.op("act", lambda e: e.activation(out=ls[:, 0:2], in_=ls[:, 0:2], func=AF.Exp), reads=["ls"], writes=["ls"])
            P.op("dve", lambda e: e.tensor_tensor(out=ls[:, 2:3], in0=ls[:, 0:1], in1=ls[:, 1:2], op=ALU.subtract), reads=["ls"], writes=["ls"])
            P.op("dve", lambda e: e.tensor_scalar(out=ls[:, 3:4], in0=ls[:, 2:3], scalar1=lambda_init, scalar2=-1.0, op0=ALU.add, op1=ALU.mult),
                 reads=["ls"], writes=["ls"])
            for i in range(2):
                P.op("pool", lambda e, i=i: e.memset(Vx[i][:, :, 128:129], 1.0), writes=["%s_one" % Vx[i].name])
            gcount = 0
            for h in range(8):
                hb_ = h % 2
                kt, qt, vx = KT[hb_], QT[hb_], Vx[hb_]
                P.dma("sp", kt[:], kT_d[h * 128:(h + 1) * 128, :], writes=[kt.name])
                P.dma("sp", qt[:], qT_d[h * 128:(h + 1) * 128, :], writes=[qt.name])
                P.dma("sp", vx[:, :, 0:128], v3[:, :, h * 128:(h + 1) * 128], writes=[vx.name])
                for i in range(T):
                    ib = i % 2
                    for g0 in range(0, i + 1, 4):
                        kbs = list(range(g0, min(g0 + 4, i + 1)))
                        n = len(kbs)
                        gb = gcount % 2
                        gcount += 1
                        psc = [pS[gb * 2], pS[gb * 2 + 1]]
                        ptc = [pTt[gb * 2], pTt[gb * 2 + 1]]

                        def fs(pe, kbs=kbs, psc=psc, kt=kt, qt=qt, i=i):
                            for j, kb in enumerate(kbs):
                                for c in range(2):
                                    cs = slice(c * 64, (c + 1) * 64)
                                    ins = pe.matmul(psc[c][:, j * 128:(j + 1) * 128], lhsT=kt[cs, kb * 128:(kb + 1) * 128],
                                                    rhs=qt[cs, i * 128:(i + 1) * 128], start=True, stop=True)
                            return ins
                        P.op("pe", fs, reads=[kt.name, qt.name], writes=[psc[0].name, psc[1].name])
                        for c in range(2):
                            ps_, pt = psc[c], ptc[c]
                            po = pO[ib * 2 + c]
                            P.op("act", lambda e, n=n, ps_=ps_, pt=pt: e.activation(out=pt[:, 0:n * 128], in_=ps_[:, 0:n * 128],
                                                                                   func=AF.Exp, scale=scale),
                                 reads=[ps_.name], writes=[pt.name])
                            if kbs[-1] == i:
                                jd = n - 1
                                P.op("dve", lambda e, jd=jd, pt=pt: e.tensor_tensor(out=pt[:, jd * 128:(jd + 1) * 128],
                                                                                    in0=pt[:, jd * 128:(jd + 1) * 128], in1=trib[:], op=ALU.mult),
                                     reads=[pt.name, "trib"], writes=[pt.name])

                            def fav(pe, kbs=kbs, pt=pt, vx=vx, po=po, i=i):
                                for j, kb in enumerate(kbs):
                                    ins = pe.matmul(po[:, 0:129], lhsT=pt[:, j * 128:(j + 1) * 128], rhs=vx[:, kb, :],
                                                    start=(kb == 0), stop=(kb == i))
                                return ins
                            P.op("pe", fav, reads=[pt.name, vx.name, "%s_one" % vx.name], writes=[po.name])
                    p0, p1 = pO[ib * 2], pO[ib * 2 + 1]
                    rq, oa, o_, os_, ob = rr[ib], oA[ib], oo[ib], oss[ib], onb[ib]
                    P.op("dve", lambda e, rq=rq, p0=p0: e.reciprocal(out=rq[:, 0:1], in_=p0[:, 128:129]), reads=[p0.name], writes=[rq.name + "a"])
                    P.op("dve", lambda e, rq=rq, p1=p1: e.reciprocal(out=rq[:, 1:2], in_=p1[:, 128:129]), reads=[p1.name], writes=[rq.name + "b"])
                    P.op("dve", lambda e, rq=rq: e.tensor_tensor(out=rq[:, 1:2], in0=rq[:, 1:2], in1=ls[:, 3:4], op=ALU.mult),
                         reads=[rq.name + "b", "ls"], writes=[rq.name + "b"])
                    P.op("act", lambda e, rq=rq, oa=oa, p0=p0: e.activation(out=oa[:], in_=p0[:, 0:128], func=AF.Copy, scale=rq[:, 0:1]),
                         reads=[p0.name, rq.name + "a"], writes=[oa.name])
                    P.op("dve", lambda e, rq=rq, oa=oa, o_=o_, p1=p1: e.scalar_tensor_tensor(
                        out=o_[:], in0=p1[:, 0:128], scalar=rq[:, 1:2], in1=oa[:], op0=ALU.mult, op1=ALU.add),
                        reads=[p1.name, rq.name + "b", oa.name], writes=[o_.name])
                    P.op("pool", lambda e, o_=o_: e.tensor_tensor(out=osq[:], in0=o_[:], in1=o_[:], op=ALU.mult), reads=[o_.name], writes=["osq"])
                    P.op("dve", lambda e, os_=os_: e.tensor_reduce(out=os_[:], in_=osq[:], axis=AX.X, op=ALU.add), reads=["osq"], writes=[os_.name])
                    P.op("act", lambda e, os_=os_: e.activation(out=os_[:], in_=os_[:], func=AF.Sqrt, bias=EPS, scale=1.0 / 128),
                         reads=[os_.name], writes=[os_.name])
                    P.op("dve", lambda e, os_=os_: e.reciprocal(out=os_[:], in_=os_[:]), reads=[os_.name], writes=[os_.name])
                    P.op("dve", lambda e, o_=o_, os_=os_, ob=ob: e.scalar_tensor_tensor(
                        out=ob[:], in0=o_[:], scalar=os_[:, 0:1], in1=subw[:], op0=ALU.mult, op1=ALU.mult),
                        reads=[o_.name, os_.name, "subw"], writes=[ob.name])
                    P.dma("sp", on_d[i * 128:(i + 1) * 128, h * 128:(h + 1) * 128], ob[:], reads=[ob.name])

        with P.stage():
            cst = P.sb("cst", [128, 5, 128], F32)
            identb = P.sb("identb", [128, 128], BF16)
            lng = P.sb("lng", [128, 1024], F32)
            lnb = P.sb("lnb", [128, 1024], F32)
            wo = P.sb("wo", [128, 8, 1024], BF16)
            ont = [P.sb("ont%d" % i, [128, 1024], BF16) for i in range(2)]
            hin = [P.sb("hin%d" % i, [128, 1024], F32) for i in range(2)]
            onT = P.sb("onT", [128, 8, 128], BF16)
            L = self.ln_tiles(None)
            tres = L["tres"]
            pT_ = P.ps("pT", [128, 512], F32)
            pTb = pT_[:].bitcast(BF16)
            pO = [P.ps("pO%d" % i, [128, 512], F32) for i in range(2)]
            P.dma("sp", cst[:], cst_d, writes=["cst"])
            P.dma("sp", lng[:], lng_d, writes=["lngb_g"])
            P.dma("sp", lnb[:], lnb_d, writes=["lngb_b"])
            for j in range(2):
                P.dma("pool", wo[:, :, j * 512:(j + 1) * 512], wo3[:, :, j * 512:(j + 1) * 512], writes=["wo%d" % j])
            P.op("dve", lambda e: e.tensor_copy(out=identb[:], in_=cst[:, 0, :]), reads=["cst"], writes=["identb"])
            for i in range(T):
                b = i % 2
                rows = slice(i * 128, (i + 1) * 128)
                P.dma("sp", ont[b][:], on_d[rows, :], writes=[ont[b].name])
                P.dma("sp", hin[b][:], h_d[rows, :], writes=[hin[b].name])

                def fto(pe, b=b):
                    for k in range(8):
                        ins = pe.transpose(pTb[:, k * 128:(k + 1) * 128], ont[b][:, k * 128:(k + 1) * 128], identb[:])
                    return ins
                P.op("pe", fto, reads=[ont[b].name, "identb"], writes=[pT_.name])
                P.op("act", lambda e: e.activation(out=onT[:], in_=pTb[:].rearrange("p (k c) -> p k c", c=128), func=AF.Copy),
                     reads=[pT_.name], writes=["onT"])
                for dh in range(2):
                    def fo(pe, dh=dh):
                        for cb in range(8):
                            ins = pe.matmul(pO[dh][:], lhsT=onT[:, cb, :], rhs=wo[:, cb, dh * 512:(dh + 1) * 512], start=(cb == 0), stop=(cb == 7))
                        return ins
                    P.op("pe", fo, reads=["onT", "wo%d" % dh], writes=[pO[dh].name])
                    P.op("dve", lambda e, dh=dh, b=b: e.scalar_tensor_tensor(
                        out=tres[:, dh * 512:(dh + 1) * 512], in0=hin[b][:, dh * 512:(dh + 1) * 512], scalar=ALPHA,
                        in1=pO[dh][:], op0=ALU.mult, op1=ALU.add), reads=[hin[b].name, pO[dh].name], writes=["tres"])
                self.ln_emit(L, i, lng, lnb, ho_d, hob_d, "lngb")


SEQ = 4096
MOE_STAGED_WEIGHTS = True
CAP0 = 512


def _consts():
    c = np.zeros((128, 5, 128), np.float32)
    j = np.arange(128)
    c[:, 0, :] = np.eye(128)
    c[:, 1, :] = (j[:, None] <= j[None, :])
    c[:, 2, :] = (j[:, None] > j[None, :])
    c[:, 3, :] = 1.0
    c[:, 4, :] = (j[:, None] < j[None, :])
    return c


def _rep(v):
    return np.ascontiguousarray(np.broadcast_to(np.asarray(v, np.float32).reshape(1, -1), (128, np.asarray(v).size)))


def build_full(S, C, dbg=()):
    b = Builder(S, dbg=dbg)
    i = b.inp
    x_d = i("x", [S, 1024])
    pos_d = i("pos", [S, 1], I32)
    cst_d = i("cst", [128, 5, 128])
    w_in_d = i("w_in", [1024, INDIM])
    convw_d = i("convw", [128, 32, 4])
    convb_d = i("convb", [128, 32])
    hp_d = i("hp", [128, 3, 32])
    w_out_d = i("w_out", [2048, 1024])
    normw_d = i("normw", [128, 2048])
    ln_d = i("ln", [8, 128, 1024])
    erow_d = i("erow", [128, 32])
    wr_d = [i("wr%d" % l, [1024, 36]) for l in range(2)]
    wg_d = [i("wg%d" % l, [32, 1024, 512]) for l in range(2)]
    wu_d = [i("wu%d" % l, [32, 1024, 512]) for l in range(2)]
    wd_d = [i("wd%d" % l, [32, 512, 1024]) for l in range(2)]
    wqkv_d = i("wqkv", [1024, 3072])
    wo_d = i("wo", [1024, 1024])
    invr_d = i("invr", [128, 8])
    lamv_d = i("lamv", [128, 4, 64])
    subw_d = i("subw", [128, 128])
    s = b.scratch
    zs_d = s("zs_d", [S, 2048], BF16)
    xbcT_d = s("xbcT_d", [4096, S], BF16)
    dtd_d = s("dtd_d", [S, 64], F32)
    hs = [s("h%d_d" % k, [S, 1024], F32) for k in range(1, 4)]
    hbs = [s("h%db_d" % k, [S, 1024], BF16) for k in range(1, 4)]
    xpad_d = s("xpad_d", [32 * C, 1024], BF16)
    ypad_d = s("ypad_d", [32 * C, 1024], BF16)
    qT_d = s("qT_d", [1024, S], BF16)
    kT_d = s("kT_d", [1024, S], BF16)
    v_d = s("v_d", [S, 1024], BF16)
    on_d = s("on_d", [S, 1024], BF16)
    out_d = b.nc.dram_tensor("out", [S, 1024], F32, kind="ExternalOutput").ap()
    lambda_init = 0.8 - 0.6 * math.exp(-0.3 * 1)
    b.l0_in(x_d, w_in_d, convw_d, convb_d, hp_d, cst_d, zs_d, xbcT_d, dtd_d)
    b.l0_ssd(x_d, zs_d, xbcT_d, dtd_d, w_out_d, normw_d, hp_d, cst_d, ln_d[0], ln_d[1], hs[0], hbs[0])
    b.moe(hs[0], hbs[0], wr_d[0], wg_d[0], wu_d[0], wd_d[0], cst_d, erow_d, ln_d[2], ln_d[3], xpad_d, ypad_d, hs[1], hbs[1], C)
    b.attn(hs[1], hbs[1], wqkv_d, wo_d, pos_d, invr_d, lamv_d, subw_d, cst_d, ln_d[4], ln_d[5],
           qT_d, kT_d, v_d, on_d, hs[2], hbs[2], lambda_init)
    b.moe(hs[2], hbs[2], wr_d[1], wg_d[1], wu_d[1], wd_d[1], cst_d, erow_d, ln_d[6], ln_d[7], xpad_d, ypad_d, out_d, None, C)
    b.P.finish()
    return b


def make_in_maps(inputs, S, C, n_cores=8):
    f = lambda k: np.asarray(inputs[k])
    conv_w = f("ssm_conv_w")[0]
    conv_b = f("ssm_conv_b")[0]
    shared = {
        "cst": _consts(),
        "w_in": np.ascontiguousarray(f("ssm_w_in")[0]),
        "convw": np.ascontiguousarray(conv_w.T.reshape(32, 128, 4).transpose(1, 0, 2)),
        "convb": np.ascontiguousarray(conv_b.reshape(32, 128).T),
        "hp": np.ascontiguousarray(np.stack([_rep(f("ssm_dt_bias")[0]), _rep(f("ssm_a_log")[0]), _rep(f("ssm_d")[0])], axis=1)),
        "w_out": np.ascontiguousarray(f("ssm_w_out")[0]),
        "normw": _rep(f("ssm_norm_w")[0]),
        "ln": np.ascontiguousarray(np.stack([_rep(f("ln_mix_g")[0]), _rep(f("ln_mix_b")[0]), _rep(f("ln_ffn_g")[0]), _rep(f("ln_ffn_b")[0]),
                                             _rep(f("ln_mix_g")[1]), _rep(f("ln_mix_b")[1]), _rep(f("ln_ffn_g")[1]), _rep(f("ln_ffn_b")[1])])),
        "erow": _rep(np.arange(32, dtype=np.float32) * C),
        "wqkv": np.ascontiguousarray(f("attn_w_qkv")[0]),
        "wo": np.ascontiguousarray(f("attn_w_o")[0]),
        "invr": _rep((500000.0 ** (-np.arange(0, 16, 2, dtype=np.float32) / 16)).astype(np.float32)),
        "lamv": np.ascontiguousarray(np.broadcast_to(
            np.stack([f("attn_lam_q1")[0], f("attn_lam_k1")[0], f("attn_lam_q2")[0], f("attn_lam_k2")[0]])[None], (128, 4, 64))).astype(np.float32),
        "subw": _rep(f("attn_subln_w")[0]),
    }
    for l in range(2):
        shared["wr%d" % l] = np.ascontiguousarray(np.concatenate([f("moe_w_group")[l], f("moe_w_expert")[l]], axis=1))
        shared["wg%d" % l] = np.ascontiguousarray(f("moe_w_gate")[l])
        shared["wu%d" % l] = np.ascontiguousarray(f("moe_w_up")[l])
        shared["wd%d" % l] = np.ascontiguousarray(f("moe_w_down")[l])
    maps = []
    for c in range(n_cores):
        bi = c // 2
        m = dict(shared)
        m["x"] = np.ascontiguousarray(f("x")[bi, :S])
        m["pos"] = np.ascontiguousarray(f("positions")[bi, :S].reshape(S, 1).astype(np.int32))
        maps.append(m)
    return maps


def kernel(**inputs):
    S, C = SEQ, CAP0
    b = build_full(S, C)
    maps = make_in_maps(inputs, S, C)
    res = run_bass_kernel_spmd(b.nc, maps, core_ids=list(range(8)))
    out = np.stack([np.asarray(res.results[2 * bi]["out"]) for bi in range(4)], axis=0)
    return out.astype(np.float32)
```

```python
import math
from contextlib import ExitStack

import numpy as np
import concourse.bass as bass
import concourse.mybir as mybir
from concourse.bass_utils import run_bass_kernel_spmd

F32 = mybir.dt.float32
BF16 = mybir.dt.bfloat16
I32 = mybir.dt.int32
U32 = mybir.dt.uint32
AF = mybir.ActivationFunctionType
ALU = mybir.AluOpType
AX = mybir.AxisListType


class _Op:
    __slots__ = ("eng", "fn", "reads", "writes", "kind", "sem", "val", "final", "deps", "slot")

    def __init__(self, eng, fn, reads, writes, kind, final=False):
        self.eng, self.fn, self.reads, self.writes, self.kind, self.final = eng, fn, reads, writes, kind, final
        self.sem = None
        self.val = 0
        self.deps = ()
        self.slot = -1


class Prog:
    ENGS = ("pe", "act", "dve", "pool", "sp")
    NDMA = 24

    def __init__(self, nc):
        self.nc = nc
        self.ops = []
        self.stack = ExitStack()
        self._init_sems()

    sid = 0

    def sb(self, name, shape, dt):
        return self.stack.enter_context(self.nc.sbuf_tensor("%s_s%d" % (name, self.sid), list(shape), dt))

    def ps(self, name, shape, dt):
        return self.stack.enter_context(self.nc.psum_tensor("%s_p%d" % (name, self.sid), list(shape), dt))

    def dram(self, name, shape, dt, kind="Internal"):
        return self.nc.dram_tensor(name, list(shape), dt, kind=kind).ap()

    def op(self, eng, fn, reads=(), writes=()):
        o = _Op(eng, fn, tuple(reads), tuple(writes), "c")
        self.ops.append(o)
        return o

    def dma(self, q, out=None, in_=None, reads=(), writes=(), final=False, indirect=None, **kw):
        if indirect is None:
            fn = lambda e: e.dma_start(out=out, in_=in_, **kw)
        else:
            fn = indirect
        o = _Op(q, fn, tuple(reads), tuple(writes), "d", final)
        self.ops.append(o)
        return o

    def make_identity(self, t, dt):
        nc = self.nc
        n = t.shape[0]

        def f(e):
            e.memset(t[:], 0.0)
            return e.affine_select(out=t[:], in_=t[:], pattern=[[-1, n]], compare_op=ALU.not_equal,
                                   fill=1.0, base=0, channel_multiplier=1)
        self.op("pool", f, writes=[t.name])

    def _init_sems(self):
        nc, st = self.nc, self.stack
        self.sems = {e: st.enter_context(nc.semaphore("s_" + e)) for e in ("pe", "act", "dve", "pool")}
        self.dsems = {q: [st.enter_context(nc.semaphore("d_%s%d" % (q, i))) for i in range(self.NDMA)]
                      for q in ("sp", "pool", "act")}
        self.cnt = {e: 0 for e in self.sems}
        self.dcnt = {q: [0] * self.NDMA for q in self.dsems}
        self.dnext = {q: 0 for q in self.dsems}
        self.waited = {e: {} for e in self.ENGS}

    def stage(self):
        prog = self

        class _S:
            def __enter__(s):
                s.outer = prog.stack
                prog.stack = ExitStack()
                prog.sid += 1
                return prog

            def __exit__(s, *a):
                if a[0] is None:
                    prog.flush()
                prog.stack.close()
                prog.stack = s.outer
                return False
        return _S()

    def reg(self, eng, value):
        r = self._regs.get(value)
        if r is None:
            r = self._regs[value] = eng.to_reg(value)
        return r

    def flush(self):
        nc = self.nc
        self._regs = {}
        sems, dsems, cnt, dcnt, dnext = self.sems, self.dsems, self.cnt, self.dcnt, self.dnext
        last_w = {}
        readers = {}
        per_eng = {e: [] for e in self.ENGS}
        for o in self.ops:
            ps_r = [k for k in o.reads if "_p" in k]
            if ps_r:
                o.reads = tuple(k for k in o.reads if "_p" not in k)
                o.writes = tuple(o.writes) + tuple(ps_r)
            deps = []
            for k in o.reads:
                w = last_w.get(k)
                if w is not None:
                    deps.append(w)
            for k in o.writes:
                w = last_w.get(k)
                if w is not None:
                    deps.append(w)
                deps.extend(readers.get(k, ()))
            o.deps = [d for d in dict.fromkeys(deps) if d is not o]
            for k in o.reads:
                readers.setdefault(k, []).append(o)
            for k in o.writes:
                last_w[k] = o
                readers[k] = []
            if o.kind == "c":
                cnt[o.eng] += 1
                o.sem, o.val = sems[o.eng], cnt[o.eng]
            else:
                q = o.eng
                s = dnext[q]
                dnext[q] = (s + 1) % self.NDMA
                dcnt[q][s] += 16
                o.sem, o.val = dsems[q][s], dcnt[q][s]
            per_eng[o.eng].append(o)
        self.ops = []

        def emit(eng_name, e):
            waited = self.waited[eng_name]

            def wait(sem, val):
                key = id(sem)
                if waited.get(key, 0) >= val:
                    return
                waited[key] = val
                e.wait_ge(sem, val)

            for o in per_eng[eng_name]:
                for d in o.deps:
                    if d.kind == "c" and d.eng == eng_name and eng_name == "pe":
                        continue
                    wait(d.sem, d.val)
                if o.kind == "d" and o.val > 16:
                    wait(o.sem, o.val - 16)
                ins = o.fn(e)
                ins.then_inc(o.sem, 16 if o.kind == "d" else 1)
            if eng_name == "sp":
                for q in dsems:
                    for i, s in enumerate(dsems[q]):
                        if dcnt[q][i]:
                            wait(s, dcnt[q][i])

        with nc.Block() as block:
            @block.tensor
            def _(e):
                emit("pe", e)

            @block.scalar
            def _(e):
                emit("act", e)

            @block.vector
            def _(e):
                emit("dve", e)

            @block.gpsimd
            def _(e):
                emit("pool", e)

            @block.sync
            def _(e):
                emit("sp", e)

    def finish(self):
        if self.ops:
            self.flush()
        self.stack.close()


D = 1024
DI = 2048
NH = 32
HP = 64
NG = 8
NS = 128
INDIM = 6176
ALPHA = 4.0 ** 0.25
EPS = 1e-5


class Builder:
    def __init__(self, S, dbg=()):
        self.S = S
        self.T = S // 128
        self.nc = bass.Bass("TRN2", target_bir_lowering=False)
        self.P = Prog(self.nc)
        self.dbg = set(dbg)

    def inp(self, name, shape, dt=F32):
        return self.nc.dram_tensor(name, list(shape), dt, kind="ExternalInput").ap()

    def scratch(self, name, shape, dt):
        kind = "ExternalOutput" if name in self.dbg else "Internal"
        return self.nc.dram_tensor(name, list(shape), dt, kind=kind).ap()

    def l0_in(self, x_d, w_in_d, convw_d, convb_d, hp_d, cst_d, zs_d, xbcT_d, dtd_d):
        P, S, T = self.P, self.S, self.T
        w3 = w_in_d.rearrange("(c p) n -> p c n", p=128)
        with P.stage():
            cst = P.sb("cst", [128, 5, 128], F32)
            identb = P.sb("identb", [128, 128], BF16)
            hp = P.sb("hp", [128, 3, 32], F32)
            abc = P.sb("abc", [128, 32], F32)
            convw = P.sb("convw", [128, 32, 4], F32)
            convb = P.sb("convb", [128, 32], F32)
            xT = P.sb("xT", [128, 8, S], BF16)
            wz = P.sb("wz", [128, 8, 2048], BF16)
            wdt = P.sb("wdt", [128, 8, 32], BF16)
            xin = [P.sb("xin%d" % i, [128, 1024], F32) for i in range(2)]
            zsb = [P.sb("zsb%d" % i, [128, 2048], BF16) for i in range(2)]
            dts = [P.sb("dts%d" % i, [128, 64], F32) for i in range(2)]
            t0 = P.sb("t0", [128, 32], F32)
            ab = P.sb("ab", [128, 32], F32)
            e1 = P.sb("e1", [128, 32], F32)
            l1 = P.sb("l1", [128, 32], F32)
            wblk = [P.sb("wblk%d" % i, [128, 8, 512], BF16) for i in range(2)]
            ub = [P.sb("ub%d" % i, [128, 3 + S], BF16) for i in range(2)]
            dg = [P.sb("dg%d" % i, [128, 4, 128], BF16) for i in range(2)]
            xo = [P.sb("xo%d" % i, [128, 512], BF16) for i in range(2)]
            ptr = [P.ps("ptr%d" % i, [128, 512], F32) for i in range(2)]
            pz = [P.ps("pz%d" % i, [128, 512], F32) for i in range(2)]
            pu = [P.ps("pu%d" % i, [128, 512], F32) for i in range(2)]
            pc = [P.ps("pc%d" % i, [128, 512], F32) for i in range(2)]

            P.dma("sp", cst[:], cst_d, writes=["cst"])
            P.dma("sp", hp[:], hp_d, writes=["hp"])
            P.dma("sp", convw[:], convw_d, writes=["convw"])
            P.dma("sp", convb[:], convb_d, writes=["convb"])
            P.op("dve", lambda e: e.tensor_copy(out=identb[:], in_=cst[:, 0, :]), reads=["cst"], writes=["identb"])
            P.op("act", lambda e: e.activation(out=abc[:], in_=hp[:, 1, :], func=AF.Exp), reads=["hp"], writes=["abc"])
            P.op("dve", lambda e: e.tensor_scalar(out=abc[:], in0=abc[:], scalar1=-1.0, scalar2=None, op0=ALU.mult),
                 reads=["abc"], writes=["abc"])
            for j in range(4):
                P.dma("pool", wz[:, :, j * 512:(j + 1) * 512], w3[:, :, j * 512:(j + 1) * 512], writes=["wz%d" % j])
            P.dma("pool", wdt[:], w3[:, :, 6144:6176], writes=["wdt"])
            for i in range(2):
                P.op("pool", lambda e, i=i: e.memset(ub[i][:, 0:3], 0.0), writes=["ubpad%d" % i])

            for i in range(T):
                xi = xin[i % 2]
                P.dma("sp", xi[:], x_d[i * 128:(i + 1) * 128, :], writes=[xi.name])
                for hf in range(2):
                    def ftr(pe, xi=xi, hf=hf):
                        for k in range(4):
                            kk = hf * 4 + k
                            ins = pe.transpose(ptr[hf][:, k * 128:(k + 1) * 128], xi[:, kk * 128:(kk + 1) * 128], cst[:, 0, :])
                        return ins
                    P.op("pe", ftr, reads=[xi.name, "cst"], writes=[ptr[hf].name])
                    P.op("act", lambda e, i=i, hf=hf: e.activation(
                        out=xT[:, hf * 4:(hf + 1) * 4, i * 128:(i + 1) * 128],
                        in_=ptr[hf][:].rearrange("p (k c) -> p k c", c=128), func=AF.Copy),
                        reads=[ptr[hf].name], writes=["xT%d" % i])
                zb = zsb[i % 2]
                for cbk in range(4):
                    pzz = pz[cbk % 2]

                    def fz(pe, i=i, cbk=cbk, pzz=pzz):
                        for k in range(8):
                            ins = pe.matmul(pzz[:], lhsT=xT[:, k, i * 128:(i + 1) * 128], rhs=wz[:, k, cbk * 512:(cbk + 1) * 512],
                                            start=(k == 0), stop=(k == 7))
                        return ins
                    P.op("pe", fz, reads=["xT%d" % i, "wz%d" % cbk], writes=[pzz.name])
                    P.op("act", lambda e, zb=zb, cbk=cbk, pzz=pzz: e.activation(
                        out=zb[:, cbk * 512:(cbk + 1) * 512], in_=pzz[:], func=AF.Silu),
                        reads=[pzz.name], writes=[zb.name])
                P.dma("sp", zs_d[i * 128:(i + 1) * 128, :], zb[:], reads=[zb.name], writes=["zs_d%d" % i])
                pzz = pz[0]

                def fdt(pe, i=i, pzz=pzz):
                    for k in range(8):
                        ins = pe.matmul(pzz[:, 0:32], lhsT=xT[:, k, i * 128:(i + 1) * 128], rhs=wdt[:, k, :],
                                        start=(k == 0), stop=(k == 7))
                    return ins
                P.op("pe", fdt, reads=["xT%d" % i, "wdt"], writes=[pzz.name])
                dd = dts[i % 2]
                P.op("dve", lambda e, pzz=pzz: e.tensor_tensor(out=t0[:], in0=pzz[:, 0:32], in1=hp[:, 0, :], op=ALU.add),
                     reads=[pzz.name, "hp"], writes=["t0"])
                P.op("dve", lambda e: e.scalar_tensor_tensor(out=ab[:], in0=t0[:], scalar=-1.0, in1=t0[:], op0=ALU.mult, op1=ALU.max),
                     reads=["t0"], writes=["ab"])
                P.op("act", lambda e: e.activation(out=e1[:], in_=ab[:], func=AF.Exp, scale=-1.0), reads=["ab"], writes=["e1"])
                P.op("act", lambda e: e.activation(out=l1[:], in_=e1[:], func=AF.Ln, bias=1.0, scale=1.0),
                     reads=["e1"], writes=["l1"])
                P.op("dve", lambda e, dd=dd: e.scalar_tensor_tensor(out=dd[:, 0:32], in0=t0[:], scalar=0.0, in1=l1[:],
                                                                    op0=ALU.max, op1=ALU.add),
                     reads=["t0", "l1"], writes=[dd.name])
                P.op("dve", lambda e, dd=dd: e.tensor_tensor(out=dd[:, 32:64], in0=dd[:, 0:32], in1=abc[:], op=ALU.mult),
                     reads=[dd.name, "abc"], writes=[dd.name])
                P.dma("sp", dtd_d[i * 128:(i + 1) * 128, :], dd[:], reads=[dd.name], writes=["dtd_d%d" % i])

            NTG = S // 512
            for sb4 in range(8):
                wb = wblk[sb4 % 2]
                c0 = 2048 + sb4 * 512
                P.dma("pool", wb[:], w3[:, :, c0:c0 + 512], writes=[wb.name])
                for j in range(4):
                    cb = sb4 * 4 + j
                    dgc = dg[cb % 2]
                    u = ub[cb % 2]

                    def fdg(e, dgc=dgc, cb=cb):
                        for k in range(4):
                            ins = e.tensor_scalar(out=dgc[:, k, :], in0=identb[:], scalar1=convw[:, cb, k:k + 1],
                                                  scalar2=None, op0=ALU.mult)
                        return ins
                    P.op("dve", fdg, reads=["identb", "convw"], writes=[dgc.name])
                    for tg in range(NTG):
                        puu = pu[tg % 2]
                        pcc = pc[tg % 2]
                        xoo = xo[tg % 2]

                        def fu(pe, wb=wb, j=j, tg=tg, puu=puu):
                            for k in range(8):
                                ins = pe.matmul(puu[:], lhsT=wb[:, k, j * 128:(j + 1) * 128],
                                                rhs=xT[:, k, tg * 512:(tg + 1) * 512], start=(k == 0), stop=(k == 7))
                            return ins
                        P.op("pe", fu, reads=[wb.name] + ["xT%d" % t for t in range(tg * 4, tg * 4 + 4)], writes=[puu.name])
                        P.op("act", lambda e, u=u, tg=tg, puu=puu: e.activation(
                            out=u[:, 3 + tg * 512:3 + (tg + 1) * 512], in_=puu[:], func=AF.Copy),
                            reads=[puu.name], writes=["%s_%d" % (u.name, tg)])

                        def fcv(pe, u=u, tg=tg, pcc=pcc, dgc=dgc):
                            for k in range(4):
                                ins = pe.matmul(pcc[:], lhsT=dgc[:, k, :], rhs=u[:, tg * 512 + k:tg * 512 + k + 512],
                                                start=(k == 0), stop=(k == 3))
                            return ins
                        rk = ["%s_%d" % (u.name, tg), dgc.name, "ubpad%d" % (cb % 2)]
                        if tg > 0:
                            rk.append("%s_%d" % (u.name, tg - 1))
                        P.op("pe", fcv, reads=rk, writes=[pcc.name])
                        P.op("act", lambda e, xoo=xoo, pcc=pcc, cb=cb: e.activation(
                            out=xoo[:], in_=pcc[:], func=AF.Silu, bias=convb[:, cb:cb + 1], scale=1.0),
                            reads=[pcc.name, "convb"], writes=[xoo.name])
                        P.dma("sp", xbcT_d[cb * 128:(cb + 1) * 128, tg * 512:(tg + 1) * 512], xoo[:],
                              reads=[xoo.name])


    def ln_tiles(self, names):
        P = self.P
        d = {}
        d["tres"] = P.sb("tres", [128, 1024], F32)
        d["sq"] = P.sb("lnsq", [128, 1024], F32)
        d["s12"] = P.sb("s12", [128, 2], F32)
        d["m2"] = P.sb("m2", [128, 1], F32)
        d["mv"] = P.sb("mv", [128, 2], F32)
        d["rstd"] = P.sb("rstd", [128, 1], F32)
        d["hn"] = P.sb("hn", [128, 1024], F32)
        d["ho"] = [P.sb("ho%d" % i, [128, 1024], F32) for i in range(2)]
        d["hob"] = [P.sb("hob%d" % i, [128, 1024], BF16) for i in range(2)]
        return d

    def ln_emit(self, L, i, lng, lnb, h_d, hb_d, gkey):
        P = self.P
        tres, mv, rstd, hn = L["tres"], L["mv"], L["rstd"], L["hn"]
        ho, hob = L["ho"][i % 2], L["hob"][i % 2]

        sq, s12, m2 = L["sq"], L["s12"], L["m2"]
        P.op("pool", lambda e: e.tensor_tensor(out=sq[:], in0=tres[:], in1=tres[:], op=ALU.mult), reads=["tres"], writes=["lnsq"])
        P.op("dve", lambda e: e.tensor_reduce(out=s12[:, 0:1], in_=tres[:], axis=AX.X, op=ALU.add), reads=["tres"], writes=["s1"])
        P.op("dve", lambda e: e.tensor_reduce(out=s12[:, 1:2], in_=sq[:], axis=AX.X, op=ALU.add), reads=["lnsq"], writes=["s2"])
        P.op("dve", lambda e: e.tensor_scalar(out=mv[:, 0:1], in0=s12[:, 0:1], scalar1=1.0 / 1024, scalar2=None, op0=ALU.mult),
             reads=["s1"], writes=["mv"])
        P.op("dve", lambda e: e.tensor_tensor(out=m2[:], in0=mv[:, 0:1], in1=mv[:, 0:1], op=ALU.mult), reads=["mv"], writes=["m2"])
        P.op("dve", lambda e: e.scalar_tensor_tensor(out=mv[:, 1:2], in0=s12[:, 1:2], scalar=1.0 / 1024, in1=m2[:],
                                                     op0=ALU.mult, op1=ALU.subtract), reads=["s2", "m2", "mv"], writes=["mv"])
        P.op("act", lambda e: e.activation(out=rstd[:], in_=mv[:, 1:2], func=AF.Sqrt, bias=EPS, scale=1.0),
             reads=["mv"], writes=["rstd"])
        P.op("dve", lambda e: e.reciprocal(out=rstd[:], in_=rstd[:]), reads=["rstd"], writes=["rstd"])
        P.op("dve", lambda e: e.tensor_scalar(out=hn[:], in0=tres[:], scalar1=mv[:, 0:1], scalar2=rstd[:, 0:1],
                                              op0=ALU.subtract, op1=ALU.mult), reads=["tres", "mv", "rstd"], writes=["hn"])
        P.op("pool", lambda e: e.tensor_tensor(out=ho[:], in0=hn[:], in1=lng[:], op=ALU.mult),
             reads=["hn", gkey + "_g"], writes=[ho.name])
        P.op("dve", lambda e: e.tensor_tensor(out=ho[:], in0=ho[:], in1=lnb[:], op=ALU.add),
             reads=[ho.name, gkey + "_b"], writes=[ho.name])
        P.dma("sp", h_d[i * 128:(i + 1) * 128, :], ho[:], reads=[ho.name])
        if hb_d is not None:
            P.op("act", lambda e: e.activation(out=hob[:], in_=ho[:], func=AF.Copy), reads=[ho.name], writes=[hob.name])
            P.dma("sp", hb_d[i * 128:(i + 1) * 128, :], hob[:], reads=[hob.name])

    def l0_ssd(self, x_d, zs_d, xbcT_d, dtd_d, w_out_d, normw_d, hp_d, cst_d, lng_d, lnb_d, h_d, hb_d):
        P, S, T = self.P, self.S, self.T
        wo3 = w_out_d.rearrange("(c p) n -> p c n", p=128)
        xbc3 = xbcT_d.rearrange("(b p) t -> p b t", p=128)
        with P.stage():
            cst = P.sb("cst", [128, 5, 128], F32)
            identb = P.sb("identb", [128, 128], BF16)
            hp = P.sb("hp", [128, 3, 32], F32)
            normw = P.sb("normw", [128, 2048], F32)
            lng = P.sb("lng", [128, 1024], F32)
            lnb = P.sb("lnb", [128, 1024], F32)
            wout = P.sb("wout", [128, 16, 1024], BF16)
            St = P.sb("St", [128, 8, 256], F32)
            prevb = P.sb("prevb", [128, 8, 256], BF16)
            xTc = [P.sb("xTc%d" % i, [128, 16, 128], BF16) for i in range(2)]
            bcT = [P.sb("bcT%d" % i, [128, 16, 128], BF16) for i in range(2)]
            dtc = [P.sb("dtc%d" % i, [128, 64], F32) for i in range(2)]
            zc = [P.sb("zc%d" % i, [128, 2048], BF16) for i in range(2)]
            xres = [P.sb("xres%d" % i, [128, 1024], F32) for i in range(2)]
            ex = [P.sb("ex%d" % i, [128, 96], F32) for i in range(2)]
            dte = [P.sb("dte%d" % i, [128, 32], F32) for i in range(2)]
            R = P.sb("R", [128, 32, 128], F32)
            xraw = P.sb("xraw", [128, 2048], BF16)
            xdt = P.sb("xdt", [128, 2048], BF16)
            xde = P.sb("xde", [128, 2048], BF16)
            xD = P.sb("xD", [128, 2048], BF16)
            Btm = P.sb("Btm", [128, 8, 128], BF16)
            dec = [P.sb("dec%d" % i, [128, 512], F32) for i in range(2)]
            cbm = [P.sb("cbm%d" % i, [128, 128], F32) for i in range(2)]
            MT = [P.sb("MT%d" % i, [128, 4, 128], BF16) for i in range(2)]
            t1 = [P.sb("t1%d" % i, [128, 256], F32) for i in range(2)]
            yg = [P.sb("yg%d" % i, [128, 256], F32) for i in range(2)]
            sqj = P.sb("sqj", [128, 256], F32)
            ss = [P.sb("ss%d" % i, [128, 1], F32) for i in range(2)]
            ygn = [P.sb("ygn%d" % i, [128, 256], BF16) for i in range(2)]
            ygT = [P.sb("ygT%d" % i, [128, 16, 128], BF16) for i in range(2)]
            L = self.ln_tiles(None)
            tres = L["tres"]
            pX = [P.ps("pX%d" % i, [128, 512], F32) for i in range(2)]
            pG = [P.ps("pG%d" % i, [128, 512], F32) for i in range(2)]
            pM = [P.ps("pM%d" % i, [128, 512], F32) for i in range(2)]
            pO = [P.ps("pO%d" % i, [128, 512], F32) for i in range(2)]
            pXb = [p[:].bitcast(BF16) for p in pX]
            tri, su, ones = cst[:, 1, :], cst[:, 2, :], cst[:, 3, :]

            P.dma("sp", cst[:], cst_d, writes=["cst"])
            P.dma("sp", hp[:], hp_d, writes=["hp"])
            P.dma("sp", normw[:], normw_d, writes=["normw"])
            P.dma("sp", lng[:], lng_d, writes=["lngb_g"])
            P.dma("sp", lnb[:], lnb_d, writes=["lngb_b"])
            for j in range(2):
                P.dma("pool", wout[:, :, j * 512:(j + 1) * 512], wo3[:, :, j * 512:(j + 1) * 512], writes=["wout%d" % j])
            P.op("dve", lambda e: e.tensor_copy(out=identb[:], in_=cst[:, 0, :]), reads=["cst"], writes=["identb"])
            P.op("pool", lambda e: e.memset(St[:], 0.0), writes=["St%d" % q for q in range(8)])

            for c in range(T):
                b = c % 2
                rows = slice(c * 128, (c + 1) * 128)
                P.dma("sp", dtc[b][:], dtd_d[rows, :], writes=[dtc[b].name])
                P.dma("sp", xTc[b][:], xbc3[:, 0:16, rows], writes=[xTc[b].name])
                P.dma("sp", bcT[b][:], xbc3[:, 16:32, rows], writes=[bcT[b].name])
                P.dma("sp", zc[b][:], zs_d[rows, :], writes=[zc[b].name])
                P.dma("sp", xres[b][:], x_d[rows, :], writes=[xres[b].name])
                dtk, xk, bk, zk, exk, dtek = dtc[b], xTc[b], bcT[b], zc[b], ex[b], dte[b]
                P.op("pool", lambda e, dtk=dtk: e.tensor_tensor(
                    out=R[:], in0=dtk[:, 32:64].unsqueeze(2).to_broadcast([128, 32, 128]),
                    in1=tri.unsqueeze(1).to_broadcast([128, 32, 128]), op=ALU.mult),
                    reads=[dtk.name, "cst"], writes=["R"])
                pa = pG[0]

                def fpa(pe, dtk=dtk, pa=pa):
                    pe.matmul(pa[:, 0:32], lhsT=tri, rhs=dtk[:, 32:64], start=True, stop=True)
                    pe.matmul(pa[:, 32:64], lhsT=su, rhs=dtk[:, 32:64], start=True, stop=True)
                    return pe.matmul(pa[:, 64:96], lhsT=ones, rhs=dtk[:, 32:64], start=True, stop=True)
                P.op("pe", fpa, reads=[dtk.name, "cst"], writes=[pa.name])
                P.op("act", lambda e, exk=exk, pa=pa: e.activation(out=exk[:], in_=pa[:, 0:96], func=AF.Exp),
                     reads=[pa.name], writes=[exk.name])
                P.op("dve", lambda e, dtek=dtek, dtk=dtk, exk=exk: e.tensor_tensor(
                    out=dtek[:], in0=dtk[:, 0:32], in1=exk[:, 32:64], op=ALU.mult),
                    reads=[dtk.name, exk.name], writes=[dtek.name])
                for hf in range(2):
                    def ftx(pe, xk=xk, hf=hf):
                        for k in range(8):
                            ins = pe.transpose(pXb[hf][:, k * 128:(k + 1) * 128], xk[:, hf * 8 + k, :], identb[:])
                        return ins
                    P.op("pe", ftx, reads=[xk.name, "identb"], writes=[pX[hf].name])
                    sl = slice(hf * 1024, (hf + 1) * 1024)
                    P.op("act", lambda e, hf=hf, sl=sl: e.activation(out=xraw[:, sl], in_=pXb[hf][:], func=AF.Copy),
                         reads=[pX[hf].name], writes=["xraw%d" % hf])
                    P.op("dve", lambda e, hf=hf, sl=sl, dtk=dtk: e.tensor_tensor(
                        out=xdt[:, sl].rearrange("p (h q) -> p h q", q=64),
                        in0=pXb[hf][:].rearrange("p (h q) -> p h q", q=64),
                        in1=dtk[:, hf * 16:(hf + 1) * 16].unsqueeze(2).to_broadcast([128, 16, 64]), op=ALU.mult),
                        reads=[pX[hf].name, dtk.name], writes=["xdt%d" % hf])
                    P.op("dve", lambda e, hf=hf, sl=sl, dtek=dtek: e.tensor_tensor(
                        out=xde[:, sl].rearrange("p (h q) -> p h q", q=64),
                        in0=pXb[hf][:].rearrange("p (h q) -> p h q", q=64),
                        in1=dtek[:, hf * 16:(hf + 1) * 16].unsqueeze(2).to_broadcast([128, 16, 64]), op=ALU.mult),
                        reads=[pX[hf].name, dtek.name], writes=["xde%d" % hf])
                    P.op("pool", lambda e, hf=hf, sl=sl: e.tensor_tensor(
                        out=xD[:, sl].rearrange("p (h q) -> p h q", q=64),
                        in0=xraw[:, sl].rearrange("p (h q) -> p h q", q=64),
                        in1=hp[:, 2, hf * 16:(hf + 1) * 16].unsqueeze(2).to_broadcast([128, 16, 64]), op=ALU.mult),
                        reads=["xraw%d" % hf, "hp"], writes=["xD%d" % hf])

                def ftb(pe, bk=bk):
                    for k in range(8):
                        ins = pe.transpose(pXb[0][:, k * 128:(k + 1) * 128], bk[:, k, :], identb[:])
                    return ins
                P.op("pe", ftb, reads=[bk.name, "identb"], writes=[pX[0].name])
                P.op("act", lambda e: e.activation(out=Btm[:], in_=pXb[0][:].rearrange("p (k c) -> p k c", c=128), func=AF.Copy),
                     reads=[pX[0].name], writes=["Btm"])
                P.op("act", lambda e: e.activation(out=prevb[:], in_=St[:], func=AF.Copy), reads=["St%d" % q for q in range(8)], writes=["prevb"])

                for g in range(NG):
                    gb = g % 2
                    pg, decg, cbmg, MTg, t1g, ygg, ssg, ygng = pG[gb], dec[gb], cbm[gb], MT[gb], t1[gb], yg[gb], ss[gb], ygn[gb]
                    hf = g // 4
                    P.op("pe", lambda pe, gb=gb, g=g, pg=pg: pe.matmul(
                        pg[:], lhsT=su, rhs=R[:, 4 * g:4 * g + 4, :], start=True, stop=True),
                        reads=["R", "cst"], writes=[pg.name])
                    P.op("act", lambda e, gb=gb, pg=pg, decg=decg: e.activation(out=decg[:], in_=pg[:], func=AF.Exp),
                         reads=[pg.name], writes=[decg.name])
                    P.op("pe", lambda pe, gb=gb, g=g, bk=bk: pe.matmul(pM[gb][:, 0:128], lhsT=bk[:, g, :], rhs=bk[:, 8 + g, :],
                                                              start=True, stop=True),
                         reads=[bk.name], writes=[pM[gb].name])
                    P.op("dve", lambda e, gb=gb, cbmg=cbmg: e.tensor_tensor(out=cbmg[:], in0=pM[gb][:, 0:128], in1=tri, op=ALU.mult),
                         reads=[pM[gb].name, "cst"], writes=[cbmg.name])
                    P.op("dve", lambda e, gb=gb, MTg=MTg, decg=decg, cbmg=cbmg: e.tensor_tensor(
                        out=MTg[:], in0=decg[:].rearrange("p (r l) -> p r l", l=128),
                        in1=cbmg[:].unsqueeze(1).to_broadcast([128, 4, 128]), op=ALU.mult),
                        reads=[decg.name, cbmg.name], writes=[MTg.name])

                    def fy(pe, gb=gb, g=g, MTg=MTg):
                        for r in range(4):
                            h = 4 * g + r
                            pe.matmul(pO[gb][:, r * 64:(r + 1) * 64], lhsT=MTg[:, r, :], rhs=xdt[:, h * 64:(h + 1) * 64],
                                      start=True, stop=False)
                            ins = pe.matmul(pO[gb][:, r * 64:(r + 1) * 64], lhsT=identb[:], rhs=xD[:, h * 64:(h + 1) * 64],
                                            start=False, stop=True)
                        return ins
                    P.op("pe", fy, reads=[MTg.name, "xdt%d" % hf, "xD%d" % hf, "identb"], writes=[pO[gb].name])
                    P.op("pe", lambda pe, gb=gb, g=g, bk=bk: pe.matmul(pM[gb][:, 128:384], lhsT=bk[:, 8 + g, :], rhs=prevb[:, g, :],
                                                              start=True, stop=True),
                         reads=[bk.name, "prevb"], writes=[pM[gb].name])
                    P.op("pe", lambda pe, gb=gb, g=g: pe.matmul(pO[gb][:, 256:512], lhsT=Btm[:, g, :], rhs=xde[:, g * 256:(g + 1) * 256],
                                                       start=True, stop=True),
                         reads=["Btm", "xde%d" % hf], writes=[pO[gb].name])
                    P.op("dve", lambda e, gb=gb, g=g, t1g=t1g, exk=exk: e.tensor_tensor(
                        out=t1g[:].rearrange("p (r q) -> p r q", q=64),
                        in0=pM[gb][:, 128:384].rearrange("p (r q) -> p r q", q=64),
                        in1=exk[:, 4 * g:4 * g + 4].unsqueeze(2).to_broadcast([128, 4, 64]), op=ALU.mult),
                        reads=[pM[gb].name, exk.name], writes=[t1g.name])
                    P.op("dve", lambda e, gb=gb, t1g=t1g: e.tensor_tensor(out=t1g[:], in0=pO[gb][:, 0:256], in1=t1g[:], op=ALU.add),
                         reads=[pO[gb].name, t1g.name], writes=[t1g.name])
                    if "ydbg" in self.dbg:
                        P.dma("sp", self.ydbg[rows, g * 256:(g + 1) * 256], t1g[:], reads=[t1g.name])
                    P.op("dve", lambda e, gb=gb, g=g, t1g=t1g, ygg=ygg, zk=zk: e.tensor_tensor(
                        out=ygg[:], in0=t1g[:], in1=zk[:, g * 256:(g + 1) * 256], op=ALU.mult),
                        reads=[t1g.name, zk.name], writes=[ygg.name])
                    P.op("pool", lambda e, gb=gb, ygg=ygg: e.tensor_tensor(out=sqj[:], in0=ygg[:], in1=ygg[:], op=ALU.mult),
                         reads=[ygg.name], writes=["sqj"])
                    P.op("dve", lambda e, gb=gb, ssg=ssg: e.tensor_reduce(out=ssg[:], in_=sqj[:], axis=AX.X, op=ALU.add),
                         reads=["sqj"], writes=[ssg.name])
                    P.op("act", lambda e, gb=gb, ssg=ssg: e.activation(out=ssg[:], in_=ssg[:], func=AF.Sqrt, bias=EPS, scale=1.0 / 256),
                         reads=[ssg.name], writes=[ssg.name])
                    P.op("dve", lambda e, gb=gb, ssg=ssg: e.reciprocal(out=ssg[:], in_=ssg[:]), reads=[ssg.name], writes=[ssg.name])
                    P.op("dve", lambda e, gb=gb, g=g, ygg=ygg, ssg=ssg, ygng=ygng: e.scalar_tensor_tensor(
                        out=ygng[:], in0=ygg[:], scalar=ssg[:, 0:1], in1=normw[:, g * 256:(g + 1) * 256],
                        op0=ALU.mult, op1=ALU.mult), reads=[ygg.name, ssg.name, "normw"], writes=[ygng.name])
                    P.op("dve", lambda e, gb=gb, g=g, exk=exk: e.tensor_tensor(
                        out=St[:, g, :].rearrange("p (r q) -> p r q", q=64),
                        in0=St[:, g, :].rearrange("p (r q) -> p r q", q=64),
                        in1=exk[:, 64 + 4 * g:64 + 4 * g + 4].unsqueeze(2).to_broadcast([128, 4, 64]), op=ALU.mult),
                        reads=["St%d" % g, exk.name], writes=["St%d" % g])
                    P.op("dve", lambda e, gb=gb, g=g: e.tensor_tensor(out=St[:, g, :], in0=St[:, g, :], in1=pO[gb][:, 256:512], op=ALU.add),
                         reads=["St%d" % g, pO[gb].name], writes=["St%d" % g])

                    def fty(pe, gb=gb, ygng=ygng):
                        for j in range(2):
                            ins = pe.transpose(pXb[1][:, j * 128:(j + 1) * 128], ygng[:, j * 128:(j + 1) * 128], identb[:])
                        return ins
                    P.op("pe", fty, reads=[ygng.name, "identb"], writes=[pX[1].name])
                    P.op("act", lambda e, gb=gb, g=g, b=b: e.activation(
                        out=ygT[b][:, 2 * g:2 * g + 2, :], in_=pXb[1][:, 0:256].rearrange("p (j c) -> p j c", c=128), func=AF.Copy),
                        reads=[pX[1].name], writes=[ygT[b].name])

                for dh in range(2):
                    def fo(pe, b=b, dh=dh):
                        for cb in range(16):
                            ins = pe.matmul(pG[dh][:], lhsT=ygT[b][:, cb, :], rhs=wout[:, cb, dh * 512:(dh + 1) * 512],
                                            start=(cb == 0), stop=(cb == 15))
                        return ins
                    P.op("pe", fo, reads=[ygT[b].name, "wout%d" % dh], writes=[pG[dh].name])
                    P.op("dve", lambda e, dh=dh, b=b: e.scalar_tensor_tensor(
                        out=tres[:, dh * 512:(dh + 1) * 512], in0=xres[b][:, dh * 512:(dh + 1) * 512], scalar=ALPHA,
                        in1=pG[dh][:], op0=ALU.mult, op1=ALU.add), reads=[xres[b].name, pG[dh].name], writes=["tres"])
                if "tdbg" in self.dbg:
                    P.dma("sp", self.tdbg[rows, :], tres[:], reads=["tres"])
                self.ln_emit(L, c, lng, lnb, h_d, hb_d, "lngb")


    def moe(self, h_d, hb_d, wr_d, wg_d, wu_d, wd_d, cst_d, erow_d, lng_d, lnb_d, xpad_d, ypad_d, ho_d, hob_d, C):
        P, S, T = self.P, self.S, self.T
        NSL = 32 * C
        wr3 = wr_d.rearrange("(c p) n -> p c n", p=128)
        with P.stage():
            cst = P.sb("cst", [128, 5, 128], F32)
            identb = P.sb("identb", [128, 128], BF16)
            onesb = P.sb("onesb", [128, 128], BF16)
            slb = P.sb("slb", [128, 128], BF16)
            erow = P.sb("erow", [128, 32], F32)
            lng = P.sb("lng", [128, 1024], F32)
            lnb = P.sb("lnb", [128, 1024], F32)
            wr = P.sb("wr", [128, 8, 36], F32)
            cnt = P.sb("cnt", [128, 32], F32)
            slots = P.sb("slots", [128, T, 2], I32)
            wts = P.sb("wts", [128, T, 2], F32)
            hin = [P.sb("hin%d" % i, [128, 1024], F32) for i in range(2)]
            hbt = [P.sb("hbt%d" % i, [128, 1024], BF16) for i in range(2)]
            hT32 = P.sb("hT32", [128, 8, 128], F32)
            lg = P.sb("lg", [128, 36], F32)
            sm = P.sb("sm", [128, 16], F32)
            ohg = P.sb("ohg", [128, 4], F32)
            ge = P.sb("ge", [128, 4], F32)
            pen = P.sb("pen", [128, 4], F32)
            msk = P.sb("msk", [128, 32], F32)
            top8 = P.sb("top8", [128, 8], F32)
            sel = P.sb("sel", [128, 32], F32)
            selb = P.sb("selb", [128, 32], BF16)
            is1 = P.sb("is1", [128, 32], F32)
            is2 = P.sb("is2", [128, 32], F32)
            rk = P.sb("rk", [128, 32], F32)
            val = P.sb("val", [128, 32], F32)
            vld = P.sb("vld", [128, 32], F32)
            tmp32 = P.sb("tmp32", [128, 32], F32)
            slf = P.sb("slf", [128, 2], F32)
            wgt = [P.sb("wgt%d" % i, [128, 8, 512], BF16) for i in range(2)]
            wut = [P.sb("wut%d" % i, [128, 8, 512], BF16) for i in range(2)]
            wdt = [P.sb("wdt%d" % i, [128, 4, 1024], F32) for i in range(2)]
            wstage = [P.sb("wstage%d" % i, [128, 4, 512], F32) for i in range(3)] if MOE_STAGED_WEIGHTS else None
            xs = [P.sb("xs%d" % i, [128, 1024], BF16) for i in range(2)]
            xsT = P.sb("xsT", [128, 8, C], BF16)
            sg = [P.sb("sg%d" % i, [128, C], F32) for i in range(2)]
            hh = P.sb("hh", [128, 4, C], F32)
            yo = [P.sb("yo%d" % i, [128, 1024], BF16) for i in range(2)]
            yA = [P.sb("yA%d" % i, [128, 1024], BF16) for i in range(2)]
            yB = [P.sb("yB%d" % i, [128, 1024], BF16) for i in range(2)]
            L = self.ln_tiles(None)
            tres = L["tres"]
            pA = [P.ps("pA%d" % i, [128, 512], F32) for i in range(2)]
            pB = [P.ps("pB%d" % i, [128, 512], F32) for i in range(2)]
            pC = [P.ps("pC%d" % i, [128, 512], F32) for i in range(2)]
            pD = [P.ps("pD%d" % i, [128, 512], F32) for i in range(2)]
            pDb = pD[0][:].bitcast(BF16)

            P.dma("sp", cst[:], cst_d, writes=["cst"])
            P.dma("sp", erow[:], erow_d, writes=["erow"])
            P.dma("sp", lng[:], lng_d, writes=["lngb_g"])
            P.dma("sp", lnb[:], lnb_d, writes=["lngb_b"])
            P.dma("sp", wr[:], wr3, writes=["wr"])
            P.op("dve", lambda e: e.tensor_copy(out=identb[:], in_=cst[:, 0, :]), reads=["cst"], writes=["identb"])
            P.op("dve", lambda e: e.tensor_copy(out=onesb[:], in_=cst[:, 3, :]), reads=["cst"], writes=["onesb"])
            P.op("dve", lambda e: e.tensor_copy(out=slb[:], in_=cst[:, 4, :]), reads=["cst"], writes=["slb"])
            P.op("pool", lambda e: e.memset(cnt[:], 0.0), writes=["cnt"])
            for i in range(2):
                P.op("pool", lambda e, i=i: e.memset(yA[i][:], 0.0), writes=[yA[i].name])
                P.op("pool", lambda e, i=i: e.memset(yB[i][:], 0.0), writes=[yB[i].name])

            for i in range(T):
                b = i % 2
                rows = slice(i * 128, (i + 1) * 128)
                hi, hb = hin[b], hbt[b]
                P.dma("sp", hi[:], h_d[rows, :], writes=[hi.name])
                P.dma("sp", hb[:], hb_d[rows, :], writes=[hb.name])
                for hf in range(2):
                    def ftr(pe, hi=hi, hf=hf):
                        for k in range(4):
                            kk = hf * 4 + k
                            ins = pe.transpose(pA[hf][:, k * 128:(k + 1) * 128], hi[:, kk * 128:(kk + 1) * 128], cst[:, 0, :])
                        return ins
                    P.op("pe", ftr, reads=[hi.name, "cst"], writes=[pA[hf].name])
                    P.op("act", lambda e, hf=hf: e.activation(out=hT32[:, hf * 4:(hf + 1) * 4, :],
                                                               in_=pA[hf][:].rearrange("p (k c) -> p k c", c=128), func=AF.Copy),
                         reads=[pA[hf].name], writes=["hT32_%d" % hf])

                def frt(pe):
                    for k in range(8):
                        ins = pe.matmul(pB[0][:, 0:36], lhsT=hT32[:, k, :], rhs=wr[:, k, :], start=(k == 0), stop=(k == 7))
                    return ins
                P.op("pe", frt, reads=["hT32_0", "hT32_1", "wr"], writes=[pB[0].name])
                P.op("act", lambda e: e.activation(out=lg[:], in_=pB[0][:, 0:36], func=AF.Copy), reads=[pB[0].name], writes=["lg"])
                P.op("dve", lambda e: e.tensor_reduce(out=sm[:, 0:1], in_=lg[:, 0:4], axis=AX.X, op=ALU.max), reads=["lg"], writes=["sm0"])
                P.op("dve", lambda e: e.tensor_scalar(out=ohg[:], in0=lg[:, 0:4], scalar1=sm[:, 0:1], scalar2=None, op0=ALU.is_equal),
                     reads=["lg", "sm0"], writes=["ohg"])
                P.op("dve", lambda e: e.tensor_scalar(out=sm[:, 1:2], in0=sm[:, 0:1], scalar1=-1.0, scalar2=None, op0=ALU.mult),
                     reads=["sm0"], writes=["sm1"])
                P.op("act", lambda e: e.activation(out=ge[:], in_=lg[:, 0:4], func=AF.Exp, bias=sm[:, 1:2], scale=1.0),
                     reads=["lg", "sm1"], writes=["ge"])
                P.op("dve", lambda e: e.tensor_reduce(out=sm[:, 2:3], in_=ge[:], axis=AX.X, op=ALU.add), reads=["ge"], writes=["sm2"])
                P.op("dve", lambda e: e.reciprocal(out=sm[:, 3:4], in_=sm[:, 2:3]), reads=["sm2"], writes=["sm3"])
                P.op("dve", lambda e: e.tensor_scalar(out=pen[:], in0=ohg[:], scalar1=1.0, scalar2=1e30, op0=ALU.subtract, op1=ALU.mult),
                     reads=["ohg"], writes=["pen"])
                P.op("dve", lambda e: e.tensor_tensor(out=msk[:].rearrange("p (g j) -> p g j", j=8),
                                                      in0=lg[:, 4:36].rearrange("p (g j) -> p g j", j=8),
                                                      in1=pen[:].unsqueeze(2).to_broadcast([128, 4, 8]), op=ALU.add),
                     reads=["lg", "pen"], writes=["msk"])
                P.op("dve", lambda e: e.max(out=top8[:], in_=msk[:]), reads=["msk"], writes=["top8"])
                P.op("dve", lambda e: e.tensor_scalar(out=sel[:], in0=msk[:], scalar1=top8[:, 1:2], scalar2=None, op0=ALU.is_ge),
                     reads=["msk", "top8"], writes=["sel"])
                P.op("dve", lambda e: e.tensor_copy(out=selb[:], in_=sel[:]), reads=["sel"], writes=["selb"])
                P.op("dve", lambda e: e.tensor_scalar(out=is1[:], in0=msk[:], scalar1=top8[:, 0:1], scalar2=None, op0=ALU.is_equal),
                     reads=["msk", "top8"], writes=["is1"])
                P.op("dve", lambda e: e.tensor_tensor(out=is2[:], in0=sel[:], in1=is1[:], op=ALU.subtract), reads=["sel", "is1"], writes=["is2"])
                P.op("dve", lambda e: e.tensor_tensor(out=sm[:, 4:5], in0=top8[:, 0:1], in1=top8[:, 1:2], op=ALU.subtract),
                     reads=["top8"], writes=["sm4"])
                P.op("act", lambda e: e.activation(out=sm[:, 5:6], in_=sm[:, 4:5], func=AF.Sigmoid), reads=["sm4"], writes=["sm5"])
                def frk(pe):
                    pe.matmul(pB[1][:, 0:32], lhsT=slb[:], rhs=selb[:], start=True, stop=True)
                    return pe.matmul(pB[1][:, 32:64], lhsT=onesb[:], rhs=selb[:], start=True, stop=True)
                P.op("pe", frk, reads=["slb", "onesb", "selb"], writes=[pB[1].name])
                P.op("dve", lambda e: e.tensor_tensor(out=rk[:], in0=pB[1][:, 0:32], in1=cnt[:], op=ALU.add),
                     reads=[pB[1].name, "cnt"], writes=["rk"])
                P.op("dve", lambda e: e.tensor_tensor(out=cnt[:], in0=pB[1][:, 32:64], in1=cnt[:], op=ALU.add),
                     reads=[pB[1].name, "cnt", "rk"], writes=["cnt"])
                P.op("dve", lambda e: e.tensor_scalar(out=vld[:], in0=rk[:], scalar1=float(C), scalar2=None, op0=ALU.is_lt),
                     reads=["rk"], writes=["vld"])
                P.op("dve", lambda e: e.tensor_tensor(out=val[:], in0=rk[:], in1=erow[:], op=ALU.add), reads=["rk", "erow"], writes=["val"])
                BIG = float(4 * NSL)
                P.op("dve", lambda e: e.scalar_tensor_tensor(out=val[:], in0=val[:], scalar=-BIG, in1=vld[:], op0=ALU.add, op1=ALU.mult),
                     reads=["val", "vld"], writes=["val"])
                P.op("dve", lambda e: e.tensor_scalar(out=val[:], in0=val[:], scalar1=BIG, scalar2=None, op0=ALU.add),
                     reads=["val"], writes=["val"])
                for q, isq in ((0, is1), (1, is2)):
                    P.op("dve", lambda e, isq=isq: e.tensor_tensor(out=tmp32[:], in0=isq[:], in1=val[:], op=ALU.mult),
                         reads=["is1", "is2", "val"], writes=["tmp32"])
                    P.op("dve", lambda e, q=q: e.tensor_reduce(out=slf[:, q:q + 1], in_=tmp32[:], axis=AX.X, op=ALU.add),
                         reads=["tmp32"], writes=["slf%d" % q])
                    P.op("dve", lambda e, isq=isq: e.tensor_tensor(out=tmp32[:], in0=isq[:], in1=vld[:], op=ALU.mult),
                         reads=["is1", "is2", "vld", "slf%d" % q], writes=["tmp32"])
                    P.op("dve", lambda e, q=q: e.tensor_reduce(out=sm[:, 8 + q:9 + q], in_=tmp32[:], axis=AX.X, op=ALU.add),
                         reads=["tmp32"], writes=["sm%d" % (8 + q)])
                P.op("dve", lambda e, i=i: e.tensor_copy(out=slots[:, i, :], in_=slf[:]), reads=["slf0", "slf1"], writes=["slots%d" % i])
                P.op("dve", lambda e: e.tensor_tensor(out=sm[:, 6:7], in0=sm[:, 3:4], in1=sm[:, 5:6], op=ALU.mult),
                     reads=["sm3", "sm5"], writes=["sm6"])
                P.op("dve", lambda e: e.tensor_tensor(out=sm[:, 7:8], in0=sm[:, 3:4], in1=sm[:, 6:7], op=ALU.subtract),
                     reads=["sm3", "sm6"], writes=["sm7"])
                P.op("dve", lambda e, i=i: e.tensor_tensor(out=wts[:, i, :], in0=sm[:, 6:8], in1=sm[:, 8:10], op=ALU.mult),
                     reads=["sm6", "sm7", "sm8", "sm9"], writes=["wts%d" % i])
                for q in range(2):
                    P.dma("pool", None, None, reads=["slots%d" % i, hb.name], writes=["xsc_%d_%d" % (i, q)],
                          indirect=lambda g, i=i, q=q, hb=hb: g.indirect_dma_start(
                        out=xpad_d[:, :], out_offset=bass.IndirectOffsetOnAxis(ap=slots[:, i, q:q + 1], axis=0),
                        in_=hb[:], in_offset=None, bounds_check=P.reg(g, NSL - 1), oob_is_err=False))

            P.op("pool", lambda e: e.memset(tmp32[:], 0.0), reads=["xsc_%d_%d" % (i, q) for i in range(T) for q in range(2)],
                 writes=["xpad_ready", "tmp32"])

            wg4 = wg_d.rearrange("e (c p) f -> e p c f", p=128)
            wu4 = wu_d.rearrange("e (c p) f -> e p c f", p=128)
            wd4 = wd_d.rearrange("e (c p) d -> e p c d", p=128)
            NST = C // 128
            npiece = [0]

            def load_w(ex):
                b = ex % 2
                pieces = []
                for hk in range(2):
                    pieces.append((wgt[b][:, hk * 4:(hk + 1) * 4, :], wg4[ex][:, hk * 4:(hk + 1) * 4, :], "%s_%d" % (wgt[b].name, hk)))
                for hk in range(2):
                    pieces.append((wut[b][:, hk * 4:(hk + 1) * 4, :], wu4[ex][:, hk * 4:(hk + 1) * 4, :], "%s_%d" % (wut[b].name, hk)))
                for hk in range(2):
                    pieces.append((wdt[b][:, hk * 2:(hk + 1) * 2, :], wd4[ex][:, hk * 2:(hk + 1) * 2, :], "%s_%d" % (wdt[b].name, hk)))
                for pi, (dst, srcap, key) in enumerate(pieces):
                    n = npiece[0]
                    npiece[0] += 1
                    stg = wstage[n % 3]
                    sv = stg[:] if pi < 4 else stg[:].rearrange("p a b -> p (a b)").rearrange("p (a b) -> p a b", b=1024)
                    P.dma("sp", sv, srcap, writes=[stg.name])
                    eng = "pool" if n % 2 == 0 else "dve"
                    P.op(eng, lambda e, dst=dst, sv=sv: e.tensor_copy(out=dst, in_=sv), reads=[stg.name], writes=[key])

            for ex in range(32):
                b = ex % 2
                wg, wu, wd = wgt[b], wut[b], wdt[b]
                if MOE_STAGED_WEIGHTS:
                    if ex == 0:
                        load_w(0)
                    if ex + 1 < 32:
                        load_w(ex + 1)
                else:
                    P.dma("pool", wg[:], wg4[ex], writes=[wg.name + "_0", wg.name + "_1"])
                    P.dma("pool", wu[:], wu4[ex], writes=[wu.name + "_0", wu.name + "_1"])
                    if ex == 0:
                        P.dma("sp", wd[:], wd4[ex], writes=[wd.name + "_0", wd.name + "_1"])
                for st in range(NST):
                    xx = xs[st % 2]
                    r0 = ex * C + st * 128
                    P.dma("sp", xx[:], xpad_d[r0:r0 + 128, :], reads=["xpad_ready"], writes=[xx.name])

                    def ftx(pe, xx=xx):
                        for k in range(8):
                            ins = pe.transpose(pDb[:, k * 128:(k + 1) * 128], xx[:, k * 128:(k + 1) * 128], identb[:])
                        return ins
                    P.op("pe", ftx, reads=[xx.name, "identb"], writes=[pD[0].name])
                    P.op("act", lambda e, st=st: e.activation(out=xsT[:, :, st * 128:(st + 1) * 128],
                                                               in_=pDb[:].rearrange("p (k c) -> p k c", c=128), func=AF.Copy),
                         reads=[pD[0].name], writes=["xsT%d" % st])
                if not MOE_STAGED_WEIGHTS and ex + 1 < 32:
                    wdn = wdt[(ex + 1) % 2]
                    P.dma("sp", wdn[:], wd4[ex + 1], writes=[wdn.name + "_0", wdn.name + "_1"])
                xk = ["xsT%d" % st for st in range(NST)]
                for fb in range(4):
                    pg, pu, sgg = pA[fb % 2], pB[fb % 2], sg[fb % 2]

                    def fgu(pe, wg=wg, wu=wu, fb=fb, pg=pg, pu=pu):
                        for k in range(8):
                            pe.matmul(pg[:, 0:C], lhsT=wg[:, k, fb * 128:(fb + 1) * 128], rhs=xsT[:, k, :], start=(k == 0), stop=(k == 7))
                        for k in range(8):
                            ins = pe.matmul(pu[:, 0:C], lhsT=wu[:, k, fb * 128:(fb + 1) * 128], rhs=xsT[:, k, :], start=(k == 0), stop=(k == 7))
                        return ins
                    P.op("pe", fgu, reads=[wg.name + "_0", wg.name + "_1", wu.name + "_0", wu.name + "_1"] + xk, writes=[pg.name, pu.name])
                    P.op("act", lambda e, pg=pg, sgg=sgg: e.activation(out=sgg[:], in_=pg[:, 0:C], func=AF.Silu),
                         reads=[pg.name], writes=[sgg.name])
                    P.op("dve", lambda e, fb=fb, pu=pu, sgg=sgg: e.tensor_tensor(out=hh[:, fb, :], in0=sgg[:], in1=pu[:, 0:C], op=ALU.mult),
                         reads=[sgg.name, pu.name], writes=["hh%d" % fb])
                for st in range(NST):
                    yy = yo[st % 2]
                    for dh in range(2):
                        def fdn(pe, st=st, dh=dh, wd=wd):
                            for fb in range(4):
                                ins = pe.matmul(pC[dh][:], lhsT=hh[:, fb, st * 128:(st + 1) * 128], rhs=wd[:, fb, dh * 512:(dh + 1) * 512],
                                                start=(fb == 0), stop=(fb == 3))
                            return ins
                        P.op("pe", fdn, reads=[wd.name + "_0", wd.name + "_1"] + ["hh%d" % fb for fb in range(4)], writes=[pC[dh].name])
                        P.op("act", lambda e, yy=yy, dh=dh: e.activation(out=yy[:, dh * 512:(dh + 1) * 512], in_=pC[dh][:], func=AF.Copy),
                             reads=[pC[dh].name], writes=[yy.name])
                    r0 = ex * C + st * 128
                    P.dma("sp", ypad_d[r0:r0 + 128, :], yy[:], reads=[yy.name], writes=["ypad_%d_%d" % (ex, st)])

            P.op("pool", lambda e: e.memset(tmp32[:], 0.0), reads=["ypad_%d_%d" % (ex, st) for ex in range(32) for st in range(NST)],
                 writes=["ypad_ready", "tmp32"])

            for i in range(T):
                b = i % 2
                rows = slice(i * 128, (i + 1) * 128)
                hi = hin[b]
                P.dma("sp", hi[:], h_d[rows, :], writes=[hi.name])
                for q, yq in ((0, yA[b]), (1, yB[b])):
                    P.dma("pool", None, None, reads=["ypad_ready", "slots%d" % i], writes=[yq.name],
                          indirect=lambda g, i=i, q=q, yq=yq: g.indirect_dma_start(
                              out=yq[:], out_offset=None, in_=ypad_d[:, :],
                              in_offset=bass.IndirectOffsetOnAxis(ap=slots[:, i, q:q + 1], axis=0),
                              bounds_check=P.reg(g, NSL - 1), oob_is_err=False))
                P.op("act", lambda e, hi=hi: e.activation(out=tres[:], in_=hi[:], func=AF.Copy, scale=ALPHA), reads=[hi.name], writes=["tres"])
                for q, yq in ((0, yA[b]), (1, yB[b])):
                    P.op("dve", lambda e, i=i, q=q, yq=yq: e.scalar_tensor_tensor(
                        out=tres[:], in0=yq[:], scalar=wts[:, i, q:q + 1], in1=tres[:], op0=ALU.mult, op1=ALU.add),
                        reads=[yq.name, "wts%d" % i, "tres"], writes=["tres"])
                self.ln_emit(L, i, lng, lnb, ho_d, hob_d, "lngb")


    def attn(self, h_d, hb_d, wqkv_d, wo_d, pos_d, invr_d, lamv_d, subw_d, cst_d, lng_d, lnb_d,
             qT_d, kT_d, v_d, on_d, ho_d, hob_d, lambda_init):
        P, S, T = self.P, self.S, self.T
        TWO_PI = 2.0 * math.pi
        MAGIC = 12582912.0
        wq3 = wqkv_d.rearrange("(c p) n -> p c n", p=128)
        wo3 = wo_d.rearrange("(c p) n -> p c n", p=128)
        qT3 = qT_d.rearrange("(j p) t -> p j t", p=128)
        kT3 = kT_d.rearrange("(j p) t -> p j t", p=128)
        v3 = v_d.rearrange("(t p) c -> p t c", p=128)

        with P.stage():
            cst = P.sb("cst", [128, 5, 128], F32)
            identb = P.sb("identb", [128, 128], BF16)
            invr = P.sb("invr", [128, 8], F32)
            wqkv = P.sb("wqkv", [128, 8, 3072], BF16)
            hbt = [P.sb("hbt%d" % i, [128, 1024], BF16) for i in range(2)]
            hTt = P.sb("hTt", [128, 8, 128], BF16)
            qkv = P.sb("qkv", [128, 3072], F32)
            posi = [P.sb("posi%d" % i, [128, 1], I32) for i in range(2)]
            posf = P.sb("posf", [128, 1], F32)
            a16 = P.sb("a16", [128, 16], F32)
            kk = P.sb("kk", [128, 16], F32)
            sc16 = P.sb("sc16", [128, 16], F32)
            tt4 = [P.sb("tt%d" % i, [128, 32, 8], F32) for i in range(4)]
            qkb = P.sb("qkb", [128, 2048], BF16)
            vb = [P.sb("vb%d" % i, [128, 1024], BF16) for i in range(2)]
            qkT = [P.sb("qkT%d" % i, [128, 16, 128], BF16) for i in range(2)]
            pT_ = [P.ps("pT%d" % i, [128, 512], F32) for i in range(2)]
            pQ = [P.ps("pQ%d" % i, [128, 512], F32) for i in range(2)]
            pTb = [p[:].bitcast(BF16) for p in pT_]

            P.dma("sp", cst[:], cst_d, writes=["cst"])
            P.dma("sp", invr[:], invr_d, writes=["invr"])
            P.op("dve", lambda e: e.tensor_copy(out=identb[:], in_=cst[:, 0, :]), reads=["cst"], writes=["identb"])
            for j in range(6):
                P.dma("pool", wqkv[:, :, j * 512:(j + 1) * 512], wq3[:, :, j * 512:(j + 1) * 512], writes=["wqkv%d" % j])
            for i in range(T):
                b = i % 2
                rows = slice(i * 128, (i + 1) * 128)
                hb = hbt[b]
                P.dma("sp", hb[:], hb_d[rows, :], writes=[hb.name])
                P.dma("sp", posi[b][:], pos_d[rows, :], writes=[posi[b].name])

                def fth(pe, hb=hb):
                    for k in range(8):
                        ins = pe.transpose(pTb[0][:, k * 128:(k + 1) * 128], hb[:, k * 128:(k + 1) * 128], identb[:])
                    return ins
                P.op("pe", fth, reads=[hb.name, "identb"], writes=[pT_[0].name])
                P.op("act", lambda e: e.activation(out=hTt[:], in_=pTb[0][:].rearrange("p (k c) -> p k c", c=128), func=AF.Copy),
                     reads=[pT_[0].name], writes=["hTt"])
                for cbk in range(6):
                    pq = pQ[cbk % 2]

                    def fq(pe, cbk=cbk, pq=pq):
                        for k in range(8):
                            ins = pe.matmul(pq[:], lhsT=hTt[:, k, :], rhs=wqkv[:, k, cbk * 512:(cbk + 1) * 512], start=(k == 0), stop=(k == 7))
                        return ins
                    P.op("pe", fq, reads=["hTt", "wqkv%d" % cbk], writes=[pq.name])
                    P.op("act", lambda e, cbk=cbk, pq=pq: e.activation(out=qkv[:, cbk * 512:(cbk + 1) * 512], in_=pq[:], func=AF.Copy),
                         reads=[pq.name], writes=["qkv%d" % cbk])
                P.op("dve", lambda e, b=b: e.tensor_copy(out=posf[:], in_=posi[b][:]), reads=[posi[b].name], writes=["posf"])
                P.op("dve", lambda e: e.tensor_scalar(out=a16[:, 0:8], in0=invr[:], scalar1=posf[:, 0:1], scalar2=None, op0=ALU.mult),
                     reads=["invr", "posf"], writes=["a16a"])
                P.op("dve", lambda e: e.tensor_scalar(out=a16[:, 8:16], in0=a16[:, 0:8], scalar1=0.5 * math.pi, scalar2=None, op0=ALU.add),
                     reads=["a16a"], writes=["a16b"])
                P.op("dve", lambda e: e.tensor_scalar(out=kk[:], in0=a16[:], scalar1=1.0 / TWO_PI, scalar2=MAGIC, op0=ALU.mult, op1=ALU.add),
                     reads=["a16a", "a16b"], writes=["kk"])
                P.op("dve", lambda e: e.tensor_scalar(out=kk[:], in0=kk[:], scalar1=-MAGIC, scalar2=None, op0=ALU.add), reads=["kk"], writes=["kk"])
                P.op("dve", lambda e: e.scalar_tensor_tensor(out=kk[:], in0=kk[:], scalar=-TWO_PI, in1=a16[:], op0=ALU.mult, op1=ALU.add),
                     reads=["kk", "a16a", "a16b"], writes=["kk"])
                P.op("dve", lambda e: e.tensor_scalar(out=kk[:], in0=kk[:], scalar1=-math.pi, scalar2=math.pi, op0=ALU.max, op1=ALU.min),
                     reads=["kk"], writes=["kk"])
                P.op("act", lambda e: e.activation(out=sc16[:], in_=kk[:], func=AF.Sin), reads=["kk"], writes=["sc16"])
                qk3 = qkv[:, 0:2048].rearrange("p (g d) -> p g d", d=64)
                r1, r2 = qk3[:, :, 0:8], qk3[:, :, 8:16]
                sinb = sc16[:, 0:8].unsqueeze(1).to_broadcast([128, 32, 8])
                cosb = sc16[:, 8:16].unsqueeze(1).to_broadcast([128, 32, 8])
                qkeys = ["qkv%d" % j for j in range(4)]
                for n, (aa, bb) in enumerate(((r1, cosb), (r2, sinb), (r2, cosb), (r1, sinb))):
                    P.op("dve", lambda e, n=n, aa=aa, bb=bb: e.tensor_tensor(out=tt4[n][:], in0=aa, in1=bb, op=ALU.mult),
                         reads=qkeys + ["sc16"], writes=[tt4[n].name])
                P.op("dve", lambda e: e.tensor_tensor(out=r1, in0=tt4[0][:], in1=tt4[1][:], op=ALU.subtract),
                     reads=[tt4[0].name, tt4[1].name, tt4[2].name, tt4[3].name], writes=qkeys)
                P.op("dve", lambda e: e.tensor_tensor(out=r2, in0=tt4[2][:], in1=tt4[3][:], op=ALU.add),
                     reads=[tt4[2].name, tt4[3].name], writes=qkeys)
                P.op("act", lambda e: e.activation(out=qkb[:], in_=qkv[:, 0:2048], func=AF.Copy), reads=qkeys, writes=["qkb"])
                P.op("act", lambda e, b=b: e.activation(out=vb[b][:], in_=qkv[:, 2048:3072], func=AF.Copy),
                     reads=["qkv4", "qkv5"], writes=[vb[b].name])
                P.dma("sp", v_d[rows, :], vb[b][:], reads=[vb[b].name])
                for hf in range(2):
                    def ftq(pe, hf=hf):
                        for k in range(8):
                            j = hf * 8 + k
                            ins = pe.transpose(pTb[hf][:, k * 128:(k + 1) * 128], qkb[:, j * 128:(j + 1) * 128], identb[:])
                        return ins
                    P.op("pe", ftq, reads=["qkb", "identb"], writes=[pT_[hf].name])
                    P.op("act", lambda e, hf=hf, b=b: e.activation(out=qkT[b][:, hf * 8:(hf + 1) * 8, :],
                                                                   in_=pTb[hf][:].rearrange("p (k c) -> p k c", c=128), func=AF.Copy),
                         reads=[pT_[hf].name], writes=["%s_%d" % (qkT[b].name, hf)])
                P.dma("sp", qT3[:, :, rows], qkT[b][:, 0:8, :], reads=["%s_0" % qkT[b].name])
                P.dma("sp", kT3[:, :, rows], qkT[b][:, 8:16, :], reads=["%s_1" % qkT[b].name])

        with P.stage():
            cst = P.sb("cst", [128, 5, 128], F32)
            trib = P.sb("trib", [128, 128], BF16)
            lamv = P.sb("lamv", [128, 4, 64], F32)
            lpr = P.sb("lpr", [128, 2, 64], F32)
            ls = P.sb("ls", [128, 4], F32)
            subw = P.sb("subw", [128, 128], F32)
            KT = [P.sb("KT%d" % i, [128, S], BF16) for i in range(2)]
            QT = [P.sb("QT%d" % i, [128, S], BF16) for i in range(2)]
            Vx = [P.sb("Vx%d" % i, [128, T, 129], BF16) for i in range(2)]
            pTt = [P.sb("pTt%d" % i, [128, 512], BF16) for i in range(4)]
            rr = [P.sb("rr%d" % i, [128, 2], F32) for i in range(2)]
            oA = [P.sb("oA%d" % i, [128, 128], F32) for i in range(2)]
            oo = [P.sb("oo%d" % i, [128, 128], F32) for i in range(2)]
            osq = P.sb("osq", [128, 128], F32)
            oss = [P.sb("oss%d" % i, [128, 1], F32) for i in range(2)]
            onb = [P.sb("onb%d" % i, [128, 128], BF16) for i in range(2)]
            pS = [P.ps("pS%d" % i, [128, 512], F32) for i in range(4)]
            pO = [P.ps("pO%d" % i, [128, 512], F32) for i in range(4)]
            scale = 64 ** -0.5

            P.dma("sp", cst[:], cst_d, writes=["cst"])
            P.dma("sp", lamv[:], lamv_d, writes=["lamv"])
            P.dma("sp", subw[:], subw_d, writes=["subw"])
            P.op("dve", lambda e: e.tensor_copy(out=trib[:], in_=cst[:, 1, :]), reads=["cst"], writes=["trib"])
            P.op("dve", lambda e: e.tensor_scalar(out=subw[:], in0=subw[:], scalar1=1.0 - lambda_init, scalar2=None, op0=ALU.mult),
                 reads=["subw"], writes=["subw"])
            P.op("dve", lambda e: e.tensor_tensor(out=lpr[:, 0, :], in0=lamv[:, 0, :], in1=lamv[:, 1, :], op=ALU.mult), reads=["lamv"], writes=["lpr0"])
            P.op("dve", lambda e: e.tensor_tensor(out=lpr[:, 1, :], in0=lamv[:, 2, :], in1=lamv[:, 3, :], op=ALU.mult), reads=["lamv"], writes=["lpr1"])
            P.op("dve", lambda e: e.tensor_reduce(out=ls[:, 0:2], in_=lpr[:], axis=AX.X, op=ALU.add), reads=["lpr0", "lpr1"], writes=["ls"])
            P.op("act", lambda e: e.activation(out=ls[:, 0:2], in_=ls[:, 0:2], func=AF.Exp), reads=["ls"], writes=["ls"])
            P.op("dve", lambda e: e.tensor_tensor(out=ls[:, 2:3], in0=ls[:, 0:1], in1=ls[:, 1:2], op=ALU.subtract), reads=["ls"], writes=["ls"])
            P.op("dve", lambda e: e.tensor_scalar(out=ls[:, 3:4], in0=ls[:, 2:3], scalar1=lambda_init, scalar2=-1.0, op0=ALU.add, op1=ALU.mult),
                 reads=["ls"], writes=["ls"])
            for i in range(2):
                P.op("pool", lambda e, i=i: e.memset(Vx[i][:, :, 128:129], 1.0), writes=["%s_one" % Vx[i].name])
            gcount = 0
            for h in range(8):
                hb_ = h % 2
                kt, qt, vx = KT[hb_], QT[hb_], Vx[hb_]
                P.dma("sp", kt[:], kT_d[h * 128:(h + 1) * 128, :], writes=[kt.name])
                P.dma("sp", qt[:], qT_d[h * 128:(h + 1) * 128, :], writes=[qt.name])
                P.dma("sp", vx[:, :, 0:128], v3[:, :, h * 128:(h + 1) * 128], writes=[vx.name])
                for i in range(T):
                    ib = i % 2
                    for g0 in range(0, i + 1, 4):
                        kbs = list(range(g0, min(g0 + 4, i + 1)))
                        n = len(kbs)
                        gb = gcount % 2
                        gcount += 1
                        psc = [pS[gb * 2], pS[gb * 2 + 1]]
                        ptc = [pTt[gb * 2], pTt[gb * 2 + 1]]

                        def fs(pe, kbs=kbs, psc=psc, kt=kt, qt=qt, i=i):
                            for j, kb in enumerate(kbs):
                                for c in range(2):
                                    cs = slice(c * 64, (c + 1) * 64)
                                    ins = pe.matmul(psc[c][:, j * 128:(j + 1) * 128], lhsT=kt[cs, kb * 128:(kb + 1) * 128],
                                                    rhs=qt[cs, i * 128:(i + 1) * 128], start=True, stop=True)
                            return ins
                        P.op("pe", fs, reads=[kt.name, qt.name], writes=[psc[0].name, psc[1].name])
                        for c in range(2):
                            ps_, pt = psc[c], ptc[c]
                            po = pO[ib * 2 + c]
                            P.op("act", lambda e, n=n, ps_=ps_, pt=pt: e.activation(out=pt[:, 0:n * 128], in_=ps_[:, 0:n * 128],
                                                                                   func=AF.Exp, scale=scale),
                                 reads=[ps_.name], writes=[pt.name])
                            if kbs[-1] == i:
                                jd = n - 1
                                P.op("dve", lambda e, jd=jd, pt=pt: e.tensor_tensor(out=pt[:, jd * 128:(jd + 1) * 128],
                                                                                    in0=pt[:, jd * 128:(jd + 1) * 128], in1=trib[:], op=ALU.mult),
                                     reads=[pt.name, "trib"], writes=[pt.name])

                            def fav(pe, kbs=kbs, pt=pt, vx=vx, po=po, i=i):
                                for j, kb in enumerate(kbs):
                                    ins = pe.matmul(po[:, 0:129], lhsT=pt[:, j * 128:(j + 1) * 128], rhs=vx[:, kb, :],
                                                    start=(kb == 0), stop=(kb == i))
                                return ins
                            P.op("pe", fav, reads=[pt.name, vx.name, "%s_one" % vx.name], writes=[po.name])
                    p0, p1 = pO[ib * 2], pO[ib * 2 + 1]
                    rq, oa, o_, os_, ob = rr[ib], oA[ib], oo[ib], oss[ib], onb[ib]
                    P.op("dve", lambda e, rq=rq, p0=p0: e.reciprocal(out=rq[:, 0:1], in_=p0[:, 128:129]), reads=[p0.name], writes=[rq.name + "a"])
                    P.op("dve", lambda e, rq=rq, p1=p1: e.reciprocal(out=rq[:, 1:2], in_=p1[:, 128:129]), reads=[p1.name], writes=[rq.name + "b"])
                    P.op("dve", lambda e, rq=rq: e.tensor_tensor(out=rq[:, 1:2], in0=rq[:, 1:2], in1=ls[:, 3:4], op=ALU.mult),
                         reads=[rq.name + "b", "ls"], writes=[rq.name + "b"])
                    P.op("act", lambda e, rq=rq, oa=oa, p0=p0: e.activation(out=oa[:], in_=p0[:, 0:128], func=AF.Copy, scale=rq[:, 0:1]),
                         reads=[p0.name, rq.name + "a"], writes=[oa.name])
                    P.op("dve", lambda e, rq=rq, oa=oa, o_=o_, p1=p1: e.scalar_tensor_tensor(
                        out=o_[:], in0=p1[:, 0:128], scalar=rq[:, 1:2], in1=oa[:], op0=ALU.mult, op1=ALU.add),
                        reads=[p1.name, rq.name + "b", oa.name], writes=[o_.name])
                    P.op("pool", lambda e, o_=o_: e.tensor_tensor(out=osq[:], in0=o_[:], in1=o_[:], op=ALU.mult), reads=[o_.name], writes=["osq"])
                    P.op("dve", lambda e, os_=os_: e.tensor_reduce(out=os_[:], in_=osq[:], axis=AX.X, op=ALU.add), reads=["osq"], writes=[os_.name])
                    P.op("act", lambda e, os_=os_: e.activation(out=os_[:], in_=os_[:], func=AF.Sqrt, bias=EPS, scale=1.0 / 128),
                         reads=[os_.name], writes=[os_.name])
                    P.op("dve", lambda e, os_=os_: e.reciprocal(out=os_[:], in_=os_[:]), reads=[os_.name], writes=[os_.name])
                    P.op("dve", lambda e, o_=o_, os_=os_, ob=ob: e.scalar_tensor_tensor(
                        out=ob[:], in0=o_[:], scalar=os_[:, 0:1], in1=subw[:], op0=ALU.mult, op1=ALU.mult),
                        reads=[o_.name, os_.name, "subw"], writes=[ob.name])
                    P.dma("sp", on_d[i * 128:(i + 1) * 128, h * 128:(h + 1) * 128], ob[:], reads=[ob.name])

        with P.stage():
            cst = P.sb("cst", [128, 5, 128], F32)
            identb = P.sb("identb", [128, 128], BF16)
            lng = P.sb("lng", [128, 1024], F32)
            lnb = P.sb("lnb", [128, 1024], F32)
            wo = P.sb("wo", [128, 8, 1024], BF16)
            ont = [P.sb("ont%d" % i, [128, 1024], BF16) for i in range(2)]
            hin = [P.sb("hin%d" % i, [128, 1024], F32) for i in range(2)]
            onT = P.sb("onT", [128, 8, 128], BF16)
            L = self.ln_tiles(None)
            tres = L["tres"]
            pT_ = P.ps("pT", [128, 512], F32)
            pTb = pT_[:].bitcast(BF16)
            pO = [P.ps("pO%d" % i, [128, 512], F32) for i in range(2)]
            P.dma("sp", cst[:], cst_d, writes=["cst"])
            P.dma("sp", lng[:], lng_d, writes=["lngb_g"])
            P.dma("sp", lnb[:], lnb_d, writes=["lngb_b"])
            for j in range(2):
                P.dma("pool", wo[:, :, j * 512:(j + 1) * 512], wo3[:, :, j * 512:(j + 1) * 512], writes=["wo%d" % j])
            P.op("dve", lambda e: e.tensor_copy(out=identb[:], in_=cst[:, 0, :]), reads=["cst"], writes=["identb"])
            for i in range(T):
                b = i % 2
                rows = slice(i * 128, (i + 1) * 128)
                P.dma("sp", ont[b][:], on_d[rows, :], writes=[ont[b].name])
                P.dma("sp", hin[b][:], h_d[rows, :], writes=[hin[b].name])

                def fto(pe, b=b):
                    for k in range(8):
                        ins = pe.transpose(pTb[:, k * 128:(k + 1) * 128], ont[b][:, k * 128:(k + 1) * 128], identb[:])
                    return ins
                P.op("pe", fto, reads=[ont[b].name, "identb"], writes=[pT_.name])
                P.op("act", lambda e: e.activation(out=onT[:], in_=pTb[:].rearrange("p (k c) -> p k c", c=128), func=AF.Copy),
                     reads=[pT_.name], writes=["onT"])
                for dh in range(2):
                    def fo(pe, dh=dh):
                        for cb in range(8):
                            ins = pe.matmul(pO[dh][:], lhsT=onT[:, cb, :], rhs=wo[:, cb, dh * 512:(dh + 1) * 512], start=(cb == 0), stop=(cb == 7))
                        return ins
                    P.op("pe", fo, reads=["onT", "wo%d" % dh], writes=[pO[dh].name])
                    P.op("dve", lambda e, dh=dh, b=b: e.scalar_tensor_tensor(
                        out=tres[:, dh * 512:(dh + 1) * 512], in0=hin[b][:, dh * 512:(dh + 1) * 512], scalar=ALPHA,
                        in1=pO[dh][:], op0=ALU.mult, op1=ALU.add), reads=[hin[b].name, pO[dh].name], writes=["tres"])
                self.ln_emit(L, i, lng, lnb, ho_d, hob_d, "lngb")


SEQ = 4096
MOE_STAGED_WEIGHTS = False
CAP0 = 512


def _consts():
    c = np.zeros((128, 5, 128), np.float32)
    j = np.arange(128)
    c[:, 0, :] = np.eye(128)
    c[:, 1, :] = (j[:, None] <= j[None, :])
    c[:, 2, :] = (j[:, None] > j[None, :])
    c[:, 3, :] = 1.0
    c[:, 4, :] = (j[:, None] < j[None, :])
    return c


def _rep(v):
    return np.ascontiguousarray(np.broadcast_to(np.asarray(v, np.float32).reshape(1, -1), (128, np.asarray(v).size)))


def build_full(S, C, dbg=()):
    b = Builder(S, dbg=dbg)
    i = b.inp
    x_d = i("x", [S, 1024])
    pos_d = i("pos", [S, 1], I32)
    cst_d = i("cst", [128, 5, 128])
    w_in_d = i("w_in", [1024, INDIM])
    convw_d = i("convw", [128, 32, 4])
    convb_d = i("convb", [128, 32])
    hp_d = i("hp", [128, 3, 32])
    w_out_d = i("w_out", [2048, 1024])
    normw_d = i("normw", [128, 2048])
    ln_d = i("ln", [8, 128, 1024])
    erow_d = i("erow", [128, 32])
    wr_d = [i("wr%d" % l, [1024, 36]) for l in range(2)]
    wg_d = [i("wg%d" % l, [32, 1024, 512]) for l in range(2)]
    wu_d = [i("wu%d" % l, [32, 1024, 512]) for l in range(2)]
    wd_d = [i("wd%d" % l, [32, 512, 1024]) for l in range(2)]
    wqkv_d = i("wqkv", [1024, 3072])
    wo_d = i("wo", [1024, 1024])
    invr_d = i("invr", [128, 8])
    lamv_d = i("lamv", [128, 4, 64])
    subw_d = i("subw", [128, 128])
    s = b.scratch
    zs_d = s("zs_d", [S, 2048], BF16)
    xbcT_d = s("xbcT_d", [4096, S], BF16)
    dtd_d = s("dtd_d", [S, 64], F32)
    hs = [s("h%d_d" % k, [S, 1024], F32) for k in range(1, 4)]
    hbs = [s("h%db_d" % k, [S, 1024], BF16) for k in range(1, 4)]
    xpad_d = s("xpad_d", [32 * C, 1024], BF16)
    ypad_d = s("ypad_d", [32 * C, 1024], BF16)
    qT_d = s("qT_d", [1024, S], BF16)
    kT_d = s("kT_d", [1024, S], BF16)
    v_d = s("v_d", [S, 1024], BF16)
    on_d = s("on_d", [S, 1024], BF16)
    out_d = b.nc.dram_tensor("out", [S, 1024], F32, kind="ExternalOutput").ap()
    lambda_init = 0.8 - 0.6 * math.exp(-0.3 * 1)
    b.l0_in(x_d, w_in_d, convw_d, convb_d, hp_d, cst_d, zs_d, xbcT_d, dtd_d)
    b.l0_ssd(x_d, zs_d, xbcT_d, dtd_d, w_out_d, normw_d, hp_d, cst_d, ln_d[0], ln_d[1], hs[0], hbs[0])
    b.moe(hs[0], hbs[0], wr_d[0], wg_d[0], wu_d[0], wd_d[0], cst_d, erow_d, ln_d[2], ln_d[3], xpad_d, ypad_d, hs[1], hbs[1], C)
    b.attn(hs[1], hbs[1], wqkv_d, wo_d, pos_d, invr_d, lamv_d, subw_d, cst_d, ln_d[4], ln_d[5],
           qT_d, kT_d, v_d, on_d, hs[2], hbs[2], lambda_init)
    b.moe(hs[2], hbs[2], wr_d[1], wg_d[1], wu_d[1], wd_d[1], cst_d, erow_d, ln_d[6], ln_d[7], xpad_d, ypad_d, out_d, None, C)
    b.P.finish()
    return b


def make_in_maps(inputs, S, C, n_cores=8):
    f = lambda k: np.asarray(inputs[k])
    conv_w = f("ssm_conv_w")[0]
    conv_b = f("ssm_conv_b")[0]
    shared = {
        "cst": _consts(),
        "w_in": np.ascontiguousarray(f("ssm_w_in")[0]),
        "convw": np.ascontiguousarray(conv_w.T.reshape(32, 128, 4).transpose(1, 0, 2)),
        "convb": np.ascontiguousarray(conv_b.reshape(32, 128).T),
        "hp": np.ascontiguousarray(np.stack([_rep(f("ssm_dt_bias")[0]), _rep(f("ssm_a_log")[0]), _rep(f("ssm_d")[0])], axis=1)),
        "w_out": np.ascontiguousarray(f("ssm_w_out")[0]),
        "normw": _rep(f("ssm_norm_w")[0]),
        "ln": np.ascontiguousarray(np.stack([_rep(f("ln_mix_g")[0]), _rep(f("ln_mix_b")[0]), _rep(f("ln_ffn_g")[0]), _rep(f("ln_ffn_b")[0]),
                                             _rep(f("ln_mix_g")[1]), _rep(f("ln_mix_b")[1]), _rep(f("ln_ffn_g")[1]), _rep(f("ln_ffn_b")[1])])),
        "erow": _rep(np.arange(32, dtype=np.float32) * C),
        "wqkv": np.ascontiguousarray(f("attn_w_qkv")[0]),
        "wo": np.ascontiguousarray(f("attn_w_o")[0]),
        "invr": _rep((500000.0 ** (-np.arange(0, 16, 2, dtype=np.float32) / 16)).astype(np.float32)),
        "lamv": np.ascontiguousarray(np.broadcast_to(
            np.stack([f("attn_lam_q1")[0], f("attn_lam_k1")[0], f("attn_lam_q2")[0], f("attn_lam_k2")[0]])[None], (128, 4, 64))).astype(np.float32),
        "subw": _rep(f("attn_subln_w")[0]),
    }
    for l in range(2):
        shared["wr%d" % l] = np.ascontiguousarray(np.concatenate([f("moe_w_group")[l], f("moe_w_expert")[l]], axis=1))
        shared["wg%d" % l] = np.ascontiguousarray(f("moe_w_gate")[l])
        shared["wu%d" % l] = np.ascontiguousarray(f("moe_w_up")[l])
        shared["wd%d" % l] = np.ascontiguousarray(f("moe_w_down")[l])
    maps = []
    for c in range(n_cores):
        bi = c // 2
        m = dict(shared)
        m["x"] = np.ascontiguousarray(f("x")[bi, :S])
        m["pos"] = np.ascontiguousarray(f("positions")[bi, :S].reshape(S, 1).astype(np.int32))
        maps.append(m)
    return maps


def kernel(**inputs):
    S, C = SEQ, CAP0
    b = build_full(S, C)
    maps = make_in_maps(inputs, S, C)
    res = run_bass_kernel_spmd(b.nc, maps, core_ids=list(range(8)))
    out = np.stack([np.asarray(res.results[2 * bi]["out"]) for bi in range(4)], axis=0)
    return out.astype(np.float32)
```

```python
import math
from contextlib import ExitStack

import numpy as np
import concourse.bass as bass
import concourse.mybir as mybir
from concourse.bass_utils import run_bass_kernel_spmd

F32 = mybir.dt.float32
BF16 = mybir.dt.bfloat16
I32 = mybir.dt.int32
U32 = mybir.dt.uint32
AF = mybir.ActivationFunctionType
ALU = mybir.AluOpType
AX = mybir.AxisListType


class _Op:
    __slots__ = ("eng", "fn", "reads", "writes", "kind", "sem", "val", "final", "deps", "slot")

    def __init__(self, eng, fn, reads, writes, kind, final=False):
        self.eng, self.fn, self.reads, self.writes, self.kind, self.final = eng, fn, reads, writes, kind, final
        self.sem = None
        self.val = 0
        self.deps = ()
        self.slot = -1


class Prog:
    ENGS = ("pe", "act", "dve", "pool", "sp")
    NDMA = 24

    def __init__(self, nc):
        self.nc = nc
        self.ops = []
        self.stack = ExitStack()
        self._init_sems()

    sid = 0

    def sb(self, name, shape, dt):
        return self.stack.enter_context(self.nc.sbuf_tensor("%s_s%d" % (name, self.sid), list(shape), dt))

    def ps(self, name, shape, dt):
        return self.stack.enter_context(self.nc.psum_tensor("%s_p%d" % (name, self.sid), list(shape), dt))

    def dram(self, name, shape, dt, kind="Internal"):
        return self.nc.dram_tensor(name, list(shape), dt, kind=kind).ap()

    def op(self, eng, fn, reads=(), writes=()):
        o = _Op(eng, fn, tuple(reads), tuple(writes), "c")
        self.ops.append(o)
        return o

    def dma(self, q, out=None, in_=None, reads=(), writes=(), final=False, indirect=None, **kw):
        if indirect is None:
            fn = lambda e: e.dma_start(out=out, in_=in_, **kw)
        else:
            fn = indirect
        o = _Op(q, fn, tuple(reads), tuple(writes), "d", final)
        self.ops.append(o)
        return o

    def make_identity(self, t, dt):
        nc = self.nc
        n = t.shape[0]

        def f(e):
            e.memset(t[:], 0.0)
            return e.affine_select(out=t[:], in_=t[:], pattern=[[-1, n]], compare_op=ALU.not_equal,
                                   fill=1.0, base=0, channel_multiplier=1)
        self.op("pool", f, writes=[t.name])

    def _init_sems(self):
        nc, st = self.nc, self.stack
        self.sems = {e: st.enter_context(nc.semaphore("s_" + e)) for e in ("pe", "act", "dve", "pool")}
        self.dsems = {q: [st.enter_context(nc.semaphore("d_%s%d" % (q, i))) for i in range(self.NDMA)]
                      for q in ("sp", "pool", "act")}
        self.cnt = {e: 0 for e in self.sems}
        self.dcnt = {q: [0] * self.NDMA for q in self.dsems}
        self.dnext = {q: 0 for q in self.dsems}
        self.waited = {e: {} for e in self.ENGS}

    def stage(self):
        prog = self

        class _S:
            def __enter__(s):
                s.outer = prog.stack
                prog.stack = ExitStack()
                prog.sid += 1
                return prog

            def __exit__(s, *a):
                if a[0] is None:
                    prog.flush()
                prog.stack.close()
                prog.stack = s.outer
                return False
        return _S()

    def reg(self, eng, value):
        r = self._regs.get(value)
        if r is None:
            r = self._regs[value] = eng.to_reg(value)
        return r

    def flush(self):
        nc = self.nc
        self._regs = {}
        sems, dsems, cnt, dcnt, dnext = self.sems, self.dsems, self.cnt, self.dcnt, self.dnext
        last_w = {}
        readers = {}
        per_eng = {e: [] for e in self.ENGS}
        for o in self.ops:
            ps_r = [k for k in o.reads if "_p" in k]
            if ps_r:
                o.reads = tuple(k for k in o.reads if "_p" not in k)
                o.writes = tuple(o.writes) + tuple(ps_r)
            deps = []
            for k in o.reads:
                w = last_w.get(k)
                if w is not None:
                    deps.append(w)
            for k in o.writes:
                w = last_w.get(k)
                if w is not None:
                    deps.append(w)
                deps.extend(readers.get(k, ()))
            o.deps = [d for d in dict.fromkeys(deps) if d is not o]
            for k in o.reads:
                readers.setdefault(k, []).append(o)
            for k in o.writes:
                last_w[k] = o
                readers[k] = []
            if o.kind == "c":
                cnt[o.eng] += 1
                o.sem, o.val = sems[o.eng], cnt[o.eng]
            else:
                q = o.eng
                s = dnext[q]
                dnext[q] = (s + 1) % self.NDMA
                dcnt[q][s] += 16
                o.sem, o.val = dsems[q][s], dcnt[q][s]
            per_eng[o.eng].append(o)
        self.ops = []

        def emit(eng_name, e):
            waited = self.waited[eng_name]

            def wait(sem, val):
                key = id(sem)
                if waited.get(key, 0) >= val:
                    return
                waited[key] = val
                e.wait_ge(sem, val)

            for o in per_eng[eng_name]:
                for d in o.deps:
                    if d.kind == "c" and d.eng == eng_name and eng_name == "pe":
                        continue
                    wait(d.sem, d.val)
                if o.kind == "d" and o.val > 16:
                    wait(o.sem, o.val - 16)
                ins = o.fn(e)
                ins.then_inc(o.sem, 16 if o.kind == "d" else 1)
            if eng_name == "sp":
                for q in dsems:
                    for i, s in enumerate(dsems[q]):
                        if dcnt[q][i]:
                            wait(s, dcnt[q][i])

        with nc.Block() as block:
            @block.tensor
            def _(e):
                emit("pe", e)

            @block.scalar
            def _(e):
                emit("act", e)

            @block.vector
            def _(e):
                emit("dve", e)

            @block.gpsimd
            def _(e):
                emit("pool", e)

            @block.sync
            def _(e):
                emit("sp", e)

    def finish(self):
        if self.ops:
            self.flush()
        self.stack.close()


D = 1024
DI = 2048
NH = 32
HP = 64
NG = 8
NS = 128
INDIM = 6176
ALPHA = 4.0 ** 0.25
EPS = 1e-5


class Builder:
    def __init__(self, S, dbg=()):
        self.S = S
        self.T = S // 128
        self.nc = bass.Bass("TRN2", target_bir_lowering=False)
        self.P = Prog(self.nc)
        self.dbg = set(dbg)

    def inp(self, name, shape, dt=F32):
        return self.nc.dram_tensor(name, list(shape), dt, kind="ExternalInput").ap()

    def scratch(self, name, shape, dt):
        kind = "ExternalOutput" if name in self.dbg else "Internal"
        return self.nc.dram_tensor(name, list(shape), dt, kind=kind).ap()

    def l0_in(self, x_d, w_in_d, convw_d, convb_d, hp_d, cst_d, zs_d, xbcT_d, dtd_d):
        P, S, T = self.P, self.S, self.T
        w3 = w_in_d.rearrange("(c p) n -> p c n", p=128)
        with P.stage():
            cst = P.sb("cst", [128, 5, 128], F32)
            identb = P.sb("identb", [128, 128], BF16)
            hp = P.sb("hp", [128, 3, 32], F32)
            abc = P.sb("abc", [128, 32], F32)
            convw = P.sb("convw", [128, 32, 4], F32)
            convb = P.sb("convb", [128, 32], F32)
            xT = P.sb("xT", [128, 8, S], BF16)
            wz = P.sb("wz", [128, 8, 2048], BF16)
            wdt = P.sb("wdt", [128, 8, 32], BF16)
            xin = [P.sb("xin%d" % i, [128, 1024], F32) for i in range(2)]
            zsb = [P.sb("zsb%d" % i, [128, 2048], BF16) for i in range(2)]
            dts = [P.sb("dts%d" % i, [128, 64], F32) for i in range(2)]
            t0 = P.sb("t0", [128, 32], F32)
            ab = P.sb("ab", [128, 32], F32)
            e1 = P.sb("e1", [128, 32], F32)
            l1 = P.sb("l1", [128, 32], F32)
            wblk = [P.sb("wblk%d" % i, [128, 8, 512], BF16) for i in range(2)]
            ub = [P.sb("ub%d" % i, [128, 3 + S], BF16) for i in range(2)]
            dg = [P.sb("dg%d" % i, [128, 4, 128], BF16) for i in range(2)]
            xo = [P.sb("xo%d" % i, [128, 512], BF16) for i in range(2)]
            ptr = [P.ps("ptr%d" % i, [128, 512], F32) for i in range(2)]
            pz = [P.ps("pz%d" % i, [128, 512], F32) for i in range(2)]
            pu = [P.ps("pu%d" % i, [128, 512], F32) for i in range(2)]
            pc = [P.ps("pc%d" % i, [128, 512], F32) for i in range(2)]

            P.dma("sp", cst[:], cst_d, writes=["cst"])
            P.dma("sp", hp[:], hp_d, writes=["hp"])
            P.dma("sp", convw[:], convw_d, writes=["convw"])
            P.dma("sp", convb[:], convb_d, writes=["convb"])
            P.op("dve", lambda e: e.tensor_copy(out=identb[:], in_=cst[:, 0, :]), reads=["cst"], writes=["identb"])
            P.op("act", lambda e: e.activation(out=abc[:], in_=hp[:, 1, :], func=AF.Exp), reads=["hp"], writes=["abc"])
            P.op("dve", lambda e: e.tensor_scalar(out=abc[:], in0=abc[:], scalar1=-1.0, scalar2=None, op0=ALU.mult),
                 reads=["abc"], writes=["abc"])
            for j in range(4):
                P.dma("pool", wz[:, :, j * 512:(j + 1) * 512], w3[:, :, j * 512:(j + 1) * 512], writes=["wz%d" % j])
            P.dma("pool", wdt[:], w3[:, :, 6144:6176], writes=["wdt"])
            for i in range(2):
                P.op("pool", lambda e, i=i: e.memset(ub[i][:, 0:3], 0.0), writes=["ubpad%d" % i])

            for i in range(T):
                xi = xin[i % 2]
                P.dma("sp", xi[:], x_d[i * 128:(i + 1) * 128, :], writes=[xi.name])
                for hf in range(2):
                    def ftr(pe, xi=xi, hf=hf):
                        for k in range(4):
                            kk = hf * 4 + k
                            ins = pe.transpose(ptr[hf][:, k * 128:(k + 1) * 128], xi[:, kk * 128:(kk + 1) * 128], cst[:, 0, :])
                        return ins
                    P.op("pe", ftr, reads=[xi.name, "cst"], writes=[ptr[hf].name])
                    P.op("act", lambda e, i=i, hf=hf: e.activation(
                        out=xT[:, hf * 4:(hf + 1) * 4, i * 128:(i + 1) * 128],
                        in_=ptr[hf][:].rearrange("p (k c) -> p k c", c=128), func=AF.Copy),
                        reads=[ptr[hf].name], writes=["xT%d" % i])
                zb = zsb[i % 2]
                for cbk in range(4):
                    pzz = pz[cbk % 2]

                    def fz(pe, i=i, cbk=cbk, pzz=pzz):
                        for k in range(8):
                            ins = pe.matmul(pzz[:], lhsT=xT[:, k, i * 128:(i + 1) * 128], rhs=wz[:, k, cbk * 512:(cbk + 1) * 512],
                                            start=(k == 0), stop=(k == 7))
                        return ins
                    P.op("pe", fz, reads=["xT%d" % i, "wz%d" % cbk], writes=[pzz.name])
                    P.op("act", lambda e, zb=zb, cbk=cbk, pzz=pzz: e.activation(
                        out=zb[:, cbk * 512:(cbk + 1) * 512], in_=pzz[:], func=AF.Silu),
                        reads=[pzz.name], writes=[zb.name])
                P.dma("sp", zs_d[i * 128:(i + 1) * 128, :], zb[:], reads=[zb.name], writes=["zs_d%d" % i])
                pzz = pz[0]

                def fdt(pe, i=i, pzz=pzz):
                    for k in range(8):
                        ins = pe.matmul(pzz[:, 0:32], lhsT=xT[:, k, i * 128:(i + 1) * 128], rhs=wdt[:, k, :],
                                        start=(k == 0), stop=(k == 7))
                    return ins
                P.op("pe", fdt, reads=["xT%d" % i, "wdt"], writes=[pzz.name])
                dd = dts[i % 2]
                P.op("dve", lambda e, pzz=pzz: e.tensor_tensor(out=t0[:], in0=pzz[:, 0:32], in1=hp[:, 0, :], op=ALU.add),
                     reads=[pzz.name, "hp"], writes=["t0"])
                P.op("dve", lambda e: e.scalar_tensor_tensor(out=ab[:], in0=t0[:], scalar=-1.0, in1=t0[:], op0=ALU.mult, op1=ALU.max),
                     reads=["t0"], writes=["ab"])
                P.op("act", lambda e: e.activation(out=e1[:], in_=ab[:], func=AF.Exp, scale=-1.0), reads=["ab"], writes=["e1"])
                P.op("act", lambda e: e.activation(out=l1[:], in_=e1[:], func=AF.Ln, bias=1.0, scale=1.0),
                     reads=["e1"], writes=["l1"])
                P.op("dve", lambda e, dd=dd: e.scalar_tensor_tensor(out=dd[:, 0:32], in0=t0[:], scalar=0.0, in1=l1[:],
                                                                    op0=ALU.max, op1=ALU.add),
                     reads=["t0", "l1"], writes=[dd.name])
                P.op("dve", lambda e, dd=dd: e.tensor_tensor(out=dd[:, 32:64], in0=dd[:, 0:32], in1=abc[:], op=ALU.mult),
                     reads=[dd.name, "abc"], writes=[dd.name])
                P.dma("sp", dtd_d[i * 128:(i + 1) * 128, :], dd[:], reads=[dd.name], writes=["dtd_d%d" % i])

            NTG = S // 512
            for sb4 in range(8):
                wb = wblk[sb4 % 2]
                c0 = 2048 + sb4 * 512
                P.dma("pool", wb[:], w3[:, :, c0:c0 + 512], writes=[wb.name])
                for j in range(4):
                    cb = sb4 * 4 + j
                    dgc = dg[cb % 2]
                    u = ub[cb % 2]

                    def fdg(e, dgc=dgc, cb=cb):
                        for k in range(4):
                            ins = e.tensor_scalar(out=dgc[:, k, :], in0=identb[:], scalar1=convw[:, cb, k:k + 1],
                                                  scalar2=None, op0=ALU.mult)
                        return ins
                    P.op("dve", fdg, reads=["identb", "convw"], writes=[dgc.name])
                    for tg in range(NTG):
                        puu = pu[tg % 2]
                        pcc = pc[tg % 2]
                        xoo = xo[tg % 2]

                        def fu(pe, wb=wb, j=j, tg=tg, puu=puu):
                            for k in range(8):
                                ins = pe.matmul(puu[:], lhsT=wb[:, k, j * 128:(j + 1) * 128],
                                                rhs=xT[:, k, tg * 512:(tg + 1) * 512], start=(k == 0), stop=(k == 7))
                            return ins
                        P.op("pe", fu, reads=[wb.name] + ["xT%d" % t for t in range(tg * 4, tg * 4 + 4)], writes=[puu.name])
                        P.op("act", lambda e, u=u, tg=tg, puu=puu: e.activation(
                            out=u[:, 3 + tg * 512:3 + (tg + 1) * 512], in_=puu[:], func=AF.Copy),
                            reads=[puu.name], writes=["%s_%d" % (u.name, tg)])

                        def fcv(pe, u=u, tg=tg, pcc=pcc, dgc=dgc):
                            for k in range(4):
                                ins = pe.matmul(pcc[:], lhsT=dgc[:, k, :], rhs=u[:, tg * 512 + k:tg * 512 + k + 512],
                                                start=(k == 0), stop=(k == 3))
                            return ins
                        rk = ["%s_%d" % (u.name, tg), dgc.name, "ubpad%d" % (cb % 2)]
                        if tg > 0:
                            rk.append("%s_%d" % (u.name, tg - 1))
                        P.op("pe", fcv, reads=rk, writes=[pcc.name])
                        P.op("act", lambda e, xoo=xoo, pcc=pcc, cb=cb: e.activation(
                            out=xoo[:], in_=pcc[:], func=AF.Silu, bias=convb[:, cb:cb + 1], scale=1.0),
                            reads=[pcc.name, "convb"], writes=[xoo.name])
                        P.dma("sp", xbcT_d[cb * 128:(cb + 1) * 128, tg * 512:(tg + 1) * 512], xoo[:],
                              reads=[xoo.name])


    def ln_tiles(self, names):
        P = self.P
        d = {}
        d["tres"] = P.sb("tres", [128, 1024], F32)
        d["sq"] = P.sb("lnsq", [128, 1024], F32)
        d["s12"] = P.sb("s12", [128, 2], F32)
        d["m2"] = P.sb("m2", [128, 1], F32)
        d["mv"] = P.sb("mv", [128, 2], F32)
        d["rstd"] = P.sb("rstd", [128, 1], F32)
        d["hn"] = P.sb("hn", [128, 1024], F32)
        d["ho"] = [P.sb("ho%d" % i, [128, 1024], F32) for i in range(2)]
        d["hob"] = [P.sb("hob%d" % i, [128, 1024], BF16) for i in range(2)]
        return d

    def ln_emit(self, L, i, lng, lnb, h_d, hb_d, gkey):
        P = self.P
        tres, mv, rstd, hn = L["tres"], L["mv"], L["rstd"], L["hn"]
        ho, hob = L["ho"][i % 2], L["hob"][i % 2]

        sq, s12, m2 = L["sq"], L["s12"], L["m2"]
        P.op("pool", lambda e: e.tensor_tensor(out=sq[:], in0=tres[:], in1=tres[:], op=ALU.mult), reads=["tres"], writes=["lnsq"])
        P.op("dve", lambda e: e.tensor_reduce(out=s12[:, 0:1], in_=tres[:], axis=AX.X, op=ALU.add), reads=["tres"], writes=["s1"])
        P.op("dve", lambda e: e.tensor_reduce(out=s12[:, 1:2], in_=sq[:], axis=AX.X, op=ALU.add), reads=["lnsq"], writes=["s2"])
        P.op("dve", lambda e: e.tensor_scalar(out=mv[:, 0:1], in0=s12[:, 0:1], scalar1=1.0 / 1024, scalar2=None, op0=ALU.mult),
             reads=["s1"], writes=["mv"])
        P.op("dve", lambda e: e.tensor_tensor(out=m2[:], in0=mv[:, 0:1], in1=mv[:, 0:1], op=ALU.mult), reads=["mv"], writes=["m2"])
        P.op("dve", lambda e: e.scalar_tensor_tensor(out=mv[:, 1:2], in0=s12[:, 1:2], scalar=1.0 / 1024, in1=m2[:],
                                                     op0=ALU.mult, op1=ALU.subtract), reads=["s2", "m2", "mv"], writes=["mv"])
        P.op("act", lambda e: e.activation(out=rstd[:], in_=mv[:, 1:2], func=AF.Sqrt, bias=EPS, scale=1.0),
             reads=["mv"], writes=["rstd"])
        P.op("dve", lambda e: e.reciprocal(out=rstd[:], in_=rstd[:]), reads=["rstd"], writes=["rstd"])
        P.op("dve", lambda e: e.tensor_scalar(out=hn[:], in0=tres[:], scalar1=mv[:, 0:1], scalar2=rstd[:, 0:1],
                                              op0=ALU.subtract, op1=ALU.mult), reads=["tres", "mv", "rstd"], writes=["hn"])
        P.op("pool", lambda e: e.tensor_tensor(out=ho[:], in0=hn[:], in1=lng[:], op=ALU.mult),
             reads=["hn", gkey + "_g"], writes=[ho.name])
        P.op("dve", lambda e: e.tensor_tensor(out=ho[:], in0=ho[:], in1=lnb[:], op=ALU.add),
             reads=[ho.name, gkey + "_b"], writes=[ho.name])
        P.dma("sp", h_d[i * 128:(i + 1) * 128, :], ho[:], reads=[ho.name])
        if hb_d is not None:
            P.op("act", lambda e: e.activation(out=hob[:], in_=ho[:], func=AF.Copy), reads=[ho.name], writes=[hob.name])
            P.dma("sp", hb_d[i * 128:(i + 1) * 128, :], hob[:], reads=[hob.name])

    def l0_ssd(self, x_d, zs_d, xbcT_d, dtd_d, w_out_d, normw_d, hp_d, cst_d, lng_d, lnb_d, h_d, hb_d):
        P, S, T = self.P, self.S, self.T
        wo3 = w_out_d.rearrange("(c p) n -> p c n", p=128)
        xbc3 = xbcT_d.rearrange("(b p) t -> p b t", p=128)
        with P.stage():
            cst = P.sb("cst", [128, 5, 128], F32)
            identb = P.sb("identb", [128, 128], BF16)
            hp = P.sb("hp", [128, 3, 32], F32)
            normw = P.sb("normw", [128, 2048], F32)
            lng = P.sb("lng", [128, 1024], F32)
            lnb = P.sb("lnb", [128, 1024], F32)
            wout = P.sb("wout", [128, 16, 1024], BF16)
            St = P.sb("St", [128, 8, 256], F32)
            prevb = P.sb("prevb", [128, 8, 256], BF16)
            xTc = [P.sb("xTc%d" % i, [128, 16, 128], BF16) for i in range(2)]
            bcT = [P.sb("bcT%d" % i, [128, 16, 128], BF16) for i in range(2)]
            dtc = [P.sb("dtc%d" % i, [128, 64], F32) for i in range(2)]
            zc = [P.sb("zc%d" % i, [128, 2048], BF16) for i in range(2)]
            xres = [P.sb("xres%d" % i, [128, 1024], F32) for i in range(2)]
            ex = [P.sb("ex%d" % i, [128, 96], F32) for i in range(2)]
            dte = [P.sb("dte%d" % i, [128, 32], F32) for i in range(2)]
            R = P.sb("R", [128, 32, 128], F32)
            xraw = P.sb("xraw", [128, 2048], BF16)
            xdt = P.sb("xdt", [128, 2048], BF16)
            xde = P.sb("xde", [128, 2048], BF16)
            xD = P.sb("xD", [128, 2048], BF16)
            Btm = P.sb("Btm", [128, 8, 128], BF16)
            dec = [P.sb("dec%d" % i, [128, 512], F32) for i in range(2)]
            cbm = [P.sb("cbm%d" % i, [128, 128], F32) for i in range(2)]
            MT = [P.sb("MT%d" % i, [128, 4, 128], BF16) for i in range(2)]
            t1 = [P.sb("t1%d" % i, [128, 256], F32) for i in range(2)]
            yg = [P.sb("yg%d" % i, [128, 256], F32) for i in range(2)]
            sqj = P.sb("sqj", [128, 256], F32)
            ss = [P.sb("ss%d" % i, [128, 1], F32) for i in range(2)]
            ygn = [P.sb("ygn%d" % i, [128, 256], BF16) for i in range(2)]
            ygT = [P.sb("ygT%d" % i, [128, 16, 128], BF16) for i in range(2)]
            L = self.ln_tiles(None)
            tres = L["tres"]
            pX = [P.ps("pX%d" % i, [128, 512], F32) for i in range(2)]
            pG = [P.ps("pG%d" % i, [128, 512], F32) for i in range(2)]
            pM = [P.ps("pM%d" % i, [128, 512], F32) for i in range(2)]
            pO = [P.ps("pO%d" % i, [128, 512], F32) for i in range(2)]
            pXb = [p[:].bitcast(BF16) for p in pX]
            tri, su, ones = cst[:, 1, :], cst[:, 2, :], cst[:, 3, :]

            P.dma("sp", cst[:], cst_d, writes=["cst"])
            P.dma("sp", hp[:], hp_d, writes=["hp"])
            P.dma("sp", normw[:], normw_d, writes=["normw"])
            P.dma("sp", lng[:], lng_d, writes=["lngb_g"])
            P.dma("sp", lnb[:], lnb_d, writes=["lngb_b"])
            for j in range(2):
                P.dma("pool", wout[:, :, j * 512:(j + 1) * 512], wo3[:, :, j * 512:(j + 1) * 512], writes=["wout%d" % j])
            P.op("dve", lambda e: e.tensor_copy(out=identb[:], in_=cst[:, 0, :]), reads=["cst"], writes=["identb"])
            P.op("pool", lambda e: e.memset(St[:], 0.0), writes=["St%d" % q for q in range(8)])

            for c in range(T):
                b = c % 2
                rows = slice(c * 128, (c + 1) * 128)
                P.dma("sp", dtc[b][:], dtd_d[rows, :], writes=[dtc[b].name])
                P.dma("sp", xTc[b][:], xbc3[:, 0:16, rows], writes=[xTc[b].name])
                P.dma("sp", bcT[b][:], xbc3[:, 16:32, rows], writes=[bcT[b].name])
                P.dma("sp", zc[b][:], zs_d[rows, :], writes=[zc[b].name])
                P.dma("sp", xres[b][:], x_d[rows, :], writes=[xres[b].name])
                dtk, xk, bk, zk, exk, dtek = dtc[b], xTc[b], bcT[b], zc[b], ex[b], dte[b]
                P.op("pool", lambda e, dtk=dtk: e.tensor_tensor(
                    out=R[:], in0=dtk[:, 32:64].unsqueeze(2).to_broadcast([128, 32, 128]),
                    in1=tri.unsqueeze(1).to_broadcast([128, 32, 128]), op=ALU.mult),
                    reads=[dtk.name, "cst"], writes=["R"])
                pa = pG[0]

                def fpa(pe, dtk=dtk, pa=pa):
                    pe.matmul(pa[:, 0:32], lhsT=tri, rhs=dtk[:, 32:64], start=True, stop=True)
                    pe.matmul(pa[:, 32:64], lhsT=su, rhs=dtk[:, 32:64], start=True, stop=True)
                    return pe.matmul(pa[:, 64:96], lhsT=ones, rhs=dtk[:, 32:64], start=True, stop=True)
                P.op("pe", fpa, reads=[dtk.name, "cst"], writes=[pa.name])
                P.op("act", lambda e, exk=exk, pa=pa: e.activation(out=exk[:], in_=pa[:, 0:96], func=AF.Exp),
                     reads=[pa.name], writes=[exk.name])
                P.op("dve", lambda e, dtek=dtek, dtk=dtk, exk=exk: e.tensor_tensor(
                    out=dtek[:], in0=dtk[:, 0:32], in1=exk[:, 32:64], op=ALU.mult),
                    reads=[dtk.name, exk.name], writes=[dtek.name])
                for hf in range(2):
                    def ftx(pe, xk=xk, hf=hf):
                        for k in range(8):
                            ins = pe.transpose(pXb[hf][:, k * 128:(k + 1) * 128], xk[:, hf * 8 + k, :], identb[:])
                        return ins
                    P.op("pe", ftx, reads=[xk.name, "identb"], writes=[pX[hf].name])
                    sl = slice(hf * 1024, (hf + 1) * 1024)
                    P.op("act", lambda e, hf=hf, sl=sl: e.activation(out=xraw[:, sl], in_=pXb[hf][:], func=AF.Copy),
                         reads=[pX[hf].name], writes=["xraw%d" % hf])
                    P.op("dve", lambda e, hf=hf, sl=sl, dtk=dtk: e.tensor_tensor(
                        out=xdt[:, sl].rearrange("p (h q) -> p h q", q=64),
                        in0=pXb[hf][:].rearrange("p (h q) -> p h q", q=64),
                        in1=dtk[:, hf * 16:(hf + 1) * 16].unsqueeze(2).to_broadcast([128, 16, 64]), op=ALU.mult),
                        reads=[pX[hf].name, dtk.name], writes=["xdt%d" % hf])
                    P.op("dve", lambda e, hf=hf, sl=sl, dtek=dtek: e.tensor_tensor(
                        out=xde[:, sl].rearrange("p (h q) -> p h q", q=64),
                        in0=pXb[hf][:].rearrange("p (h q) -> p h q", q=64),
                        in1=dtek[:, hf * 16:(hf + 1) * 16].unsqueeze(2).to_broadcast([128, 16, 64]), op=ALU.mult),
                        reads=[pX[hf].name, dtek.name], writes=["xde%d" % hf])
                    P.op("pool", lambda e, hf=hf, sl=sl: e.tensor_tensor(
                        out=xD[:, sl].rearrange("p (h q) -> p h q", q=64),
                        in0=xraw[:, sl].rearrange("p (h q) -> p h q", q=64),
                        in1=hp[:, 2, hf * 16:(hf + 1) * 16].unsqueeze(2).to_broadcast([128, 16, 64]), op=ALU.mult),
                        reads=["xraw%d" % hf, "hp"], writes=["xD%d" % hf])

                def ftb(pe, bk=bk):
                    for k in range(8):
                        ins = pe.transpose(pXb[0][:, k * 128:(k + 1) * 128], bk[:, k, :], identb[:])
                    return ins
                P.op("pe", ftb, reads=[bk.name, "identb"], writes=[pX[0].name])
                P.op("act", lambda e: e.activation(out=Btm[:], in_=pXb[0][:].rearrange("p (k c) -> p k c", c=128), func=AF.Copy),
                     reads=[pX[0].name], writes=["Btm"])
                P.op("act", lambda e: e.activation(out=prevb[:], in_=St[:], func=AF.Copy), reads=["St%d" % q for q in range(8)], writes=["prevb"])

                for g in range(NG):
                    gb = g % 2
                    pg, decg, cbmg, MTg, t1g, ygg, ssg, ygng = pG[gb], dec[gb], cbm[gb], MT[gb], t1[gb], yg[gb], ss[gb], ygn[gb]
                    hf = g // 4
                    P.op("pe", lambda pe, gb=gb, g=g, pg=pg: pe.matmul(
                        pg[:], lhsT=su, rhs=R[:, 4 * g:4 * g + 4, :], start=True, stop=True),
                        reads=["R", "cst"], writes=[pg.name])
                    P.op("act", lambda e, gb=gb, pg=pg, decg=decg: e.activation(out=decg[:], in_=pg[:], func=AF.Exp),
                         reads=[pg.name], writes=[decg.name])
                    P.op("pe", lambda pe, gb=gb, g=g, bk=bk: pe.matmul(pM[gb][:, 0:128], lhsT=bk[:, g, :], rhs=bk[:, 8 + g, :],
                                                              start=True, stop=True),
                         reads=[bk.name], writes=[pM[gb].name])
                    P.op("dve", lambda e, gb=gb, cbmg=cbmg: e.tensor_tensor(out=cbmg[:], in0=pM[gb][:, 0:128], in1=tri, op=ALU.mult),
                         reads=[pM[gb].name, "cst"], writes=[cbmg.name])
                    P.op("dve", lambda e, gb=gb, MTg=MTg, decg=decg, cbmg=cbmg: e.tensor_tensor(
                        out=MTg[:], in0=decg[:].rearrange("p (r l) -> p r l", l=128),
                        in1=cbmg[:].unsqueeze(1).to_broadcast([128, 4, 128]), op=ALU.mult),
                        reads=[decg.name, cbmg.name], writes=[MTg.name])

                    def fy(pe, gb=gb, g=g, MTg=MTg):
                        for r in range(4):
                            h = 4 * g + r
                            pe.matmul(pO[gb][:, r * 64:(r + 1) * 64], lhsT=MTg[:, r, :], rhs=xdt[:, h * 64:(h + 1) * 64],
                                      start=True, stop=False)
                            ins = pe.matmul(pO[gb][:, r * 64:(r + 1) * 64], lhsT=identb[:], rhs=xD[:, h * 64:(h + 1) * 64],
                                            start=False, stop=True)
                        return ins
                    P.op("pe", fy, reads=[MTg.name, "xdt%d" % hf, "xD%d" % hf, "identb"], writes=[pO[gb].name])
                    P.op("pe", lambda pe, gb=gb, g=g, bk=bk: pe.matmul(pM[gb][:, 128:384], lhsT=bk[:, 8 + g, :], rhs=prevb[:, g, :],
                                                              start=True, stop=True),
                         reads=[bk.name, "prevb"], writes=[pM[gb].name])
                    P.op("pe", lambda pe, gb=gb, g=g: pe.matmul(pO[gb][:, 256:512], lhsT=Btm[:, g, :], rhs=xde[:, g * 256:(g + 1) * 256],
                                                       start=True, stop=True),
                         reads=["Btm", "xde%d" % hf], writes=[pO[gb].name])
                    P.op("dve", lambda e, gb=gb, g=g, t1g=t1g, exk=exk: e.tensor_tensor(
                        out=t1g[:].rearrange("p (r q) -> p r q", q=64),
                        in0=pM[gb][:, 128:384].rearrange("p (r q) -> p r q", q=64),
                        in1=exk[:, 4 * g:4 * g + 4].unsqueeze(2).to_broadcast([128, 4, 64]), op=ALU.mult),
                        reads=[pM[gb].name, exk.name], writes=[t1g.name])
                    P.op("dve", lambda e, gb=gb, t1g=t1g: e.tensor_tensor(out=t1g[:], in0=pO[gb][:, 0:256], in1=t1g[:], op=ALU.add),
                         reads=[pO[gb].name, t1g.name], writes=[t1g.name])
                    if "ydbg" in self.dbg:
                        P.dma("sp", self.ydbg[rows, g * 256:(g + 1) * 256], t1g[:], reads=[t1g.name])
                    P.op("dve", lambda e, gb=gb, g=g, t1g=t1g, ygg=ygg, zk=zk: e.tensor_tensor(
                        out=ygg[:], in0=t1g[:], in1=zk[:, g * 256:(g + 1) * 256], op=ALU.mult),
                        reads=[t1g.name, zk.name], writes=[ygg.name])
                    P.op("pool", lambda e, gb=gb, ygg=ygg: e.tensor_tensor(out=sqj[:], in0=ygg[:], in1=ygg[:], op=ALU.mult),
                         reads=[ygg.name], writes=["sqj"])
                    P.op("dve", lambda e, gb=gb, ssg=ssg: e.tensor_reduce(out=ssg[:], in_=sqj[:], axis=AX.X, op=ALU.add),
                         reads=["sqj"], writes=[ssg.name])
                    P.op("act", lambda e, gb=gb, ssg=ssg: e.activation(out=ssg[:], in_=ssg[:], func=AF.Sqrt, bias=EPS, scale=1.0 / 256),
                         reads=[ssg.name], writes=[ssg.name])
                    P.op("dve", lambda e, gb=gb, ssg=ssg: e.reciprocal(out=ssg[:], in_=ssg[:]), reads=[ssg.name], writes=[ssg.name])
                    P.op("dve", lambda e, gb=gb, g=g, ygg=ygg, ssg=ssg, ygng=ygng: e.scalar_tensor_tensor(
                        out=ygng[:], in0=ygg[:], scalar=ssg[:, 0:1], in1=normw[:, g * 256:(g + 1) * 256],
                        op0=ALU.mult, op1=ALU.mult), reads=[ygg.name, ssg.name, "normw"], writes=[ygng.name])
                    P.op("dve", lambda e, gb=gb, g=g, exk=exk: e.tensor_tensor(
                        out=St[:, g, :].rearrange("p (r q) -> p r q", q=64),
                        in0=St[:, g, :].rearrange("p (r q) -> p r q", q=64),
                        in1=exk[:, 64 + 4 * g:64 + 4 * g + 4].unsqueeze(2).to_broadcast([128, 4, 64]), op=ALU.mult),
                        reads=["St%d" % g, exk.name], writes=["St%d" % g])
                    P.op("dve", lambda e, gb=gb, g=g: e.tensor_tensor(out=St[:, g, :], in0=St[:, g, :], in1=pO[gb][:, 256:512], op=ALU.add),
                         reads=["St%d" % g, pO[gb].name], writes=["St%d" % g])

                    def fty(pe, gb=gb, ygng=ygng):
                        for j in range(2):
                            ins = pe.transpose(pXb[1][:, j * 128:(j + 1) * 128], ygng[:, j * 128:(j + 1) * 128], identb[:])
                        return ins
                    P.op("pe", fty, reads=[ygng.name, "identb"], writes=[pX[1].name])
                    P.op("act", lambda e, gb=gb, g=g, b=b: e.activation(
                        out=ygT[b][:, 2 * g:2 * g + 2, :], in_=pXb[1][:, 0:256].rearrange("p (j c) -> p j c", c=128), func=AF.Copy),
                        reads=[pX[1].name], writes=[ygT[b].name])

                for dh in range(2):
                    def fo(pe, b=b, dh=dh):
                        for cb in range(16):
                            ins = pe.matmul(pG[dh][:], lhsT=ygT[b][:, cb, :], rhs=wout[:, cb, dh * 512:(dh + 1) * 512],
                                            start=(cb == 0), stop=(cb == 15))
                        return ins
                    P.op("pe", fo, reads=[ygT[b].name, "wout%d" % dh], writes=[pG[dh].name])
                    P.op("dve", lambda e, dh=dh, b=b: e.scalar_tensor_tensor(
                        out=tres[:, dh * 512:(dh + 1) * 512], in0=xres[b][:, dh * 512:(dh + 1) * 512], scalar=ALPHA,
                        in1=pG[dh][:], op0=ALU.mult, op1=ALU.add), reads=[xres[b].name, pG[dh].name], writes=["tres"])
                if "tdbg" in self.dbg:
                    P.dma("sp", self.tdbg[rows, :], tres[:], reads=["tres"])
                self.ln_emit(L, c, lng, lnb, h_d, hb_d, "lngb")


    def moe(self, h_d, hb_d, wr_d, wg_d, wu_d, wd_d, cst_d, erow_d, lng_d, lnb_d, xpad_d, ypad_d, ho_d, hob_d, C):
        P, S, T = self.P, self.S, self.T
        NSL = 32 * C
        wr3 = wr_d.rearrange("(c p) n -> p c n", p=128)
        with P.stage():
            cst = P.sb("cst", [128, 5, 128], F32)
            identb = P.sb("identb", [128, 128], BF16)
            onesb = P.sb("onesb", [128, 128], BF16)
            slb = P.sb("slb", [128, 128], BF16)
            erow = P.sb("erow", [128, 32], F32)
            lng = P.sb("lng", [128, 1024], F32)
            lnb = P.sb("lnb", [128, 1024], F32)
            wr = P.sb("wr", [128, 8, 36], F32)
            cnt = P.sb("cnt", [128, 32], F32)
            slots = P.sb("slots", [128, T, 2], I32)
            wts = P.sb("wts", [128, T, 2], F32)
            hin = [P.sb("hin%d" % i, [128, 1024], F32) for i in range(2)]
            hbt = [P.sb("hbt%d" % i, [128, 1024], BF16) for i in range(2)]
            hT32 = P.sb("hT32", [128, 8, 128], F32)
            lg = P.sb("lg", [128, 36], F32)
            sm = P.sb("sm", [128, 16], F32)
            ohg = P.sb("ohg", [128, 4], F32)
            ge = P.sb("ge", [128, 4], F32)
            pen = P.sb("pen", [128, 4], F32)
            msk = P.sb("msk", [128, 32], F32)
            top8 = P.sb("top8", [128, 8], F32)
            sel = P.sb("sel", [128, 32], F32)
            selb = P.sb("selb", [128, 32], BF16)
            is1 = P.sb("is1", [128, 32], F32)
            is2 = P.sb("is2", [128, 32], F32)
            rk = P.sb("rk", [128, 32], F32)
            val = P.sb("val", [128, 32], F32)
            vld = P.sb("vld", [128, 32], F32)
            tmp32 = P.sb("tmp32", [128, 32], F32)
            slf = P.sb("slf", [128, 2], F32)
            wgt = [P.sb("wgt%d" % i, [128, 8, 512], BF16) for i in range(2)]
            wut = [P.sb("wut%d" % i, [128, 8, 512], BF16) for i in range(2)]
            wdt = [P.sb("wdt%d" % i, [128, 4, 1024], BF16) for i in range(2)]
            wstage = [P.sb("wstage%d" % i, [128, 4, 512], F32) for i in range(3)] if MOE_STAGED_WEIGHTS else None
            xs = [P.sb("xs%d" % i, [128, 1024], BF16) for i in range(2)]
            xsTs = [P.sb("xsT%d" % i, [128, 8, C], BF16) for i in range(2)]
            sg = [P.sb("sg%d" % i, [128, C], F32) for i in range(2)]
            hhs = [P.sb("hh%d" % i, [128, 4, C], BF16) for i in range(2)]
            yo = [P.sb("yo%d" % i, [128, 1024], BF16) for i in range(2)]
            yA = [P.sb("yA%d" % i, [128, 1024], BF16) for i in range(2)]
            yB = [P.sb("yB%d" % i, [128, 1024], BF16) for i in range(2)]
            L = self.ln_tiles(None)
            tres = L["tres"]
            pA = [P.ps("pA%d" % i, [128, 512], F32) for i in range(2)]
            pB = [P.ps("pB%d" % i, [128, 512], F32) for i in range(2)]
            pC = [P.ps("pC%d" % i, [128, 512], F32) for i in range(2)]
            pD = [P.ps("pD%d" % i, [128, 512], F32) for i in range(2)]
            pDb = pD[0][:].bitcast(BF16)

            P.dma("sp", cst[:], cst_d, writes=["cst"])
            P.dma("sp", erow[:], erow_d, writes=["erow"])
            P.dma("sp", lng[:], lng_d, writes=["lngb_g"])
            P.dma("sp", lnb[:], lnb_d, writes=["lngb_b"])
            P.dma("sp", wr[:], wr3, writes=["wr"])
            P.op("dve", lambda e: e.tensor_copy(out=identb[:], in_=cst[:, 0, :]), reads=["cst"], writes=["identb"])
            P.op("dve", lambda e: e.tensor_copy(out=onesb[:], in_=cst[:, 3, :]), reads=["cst"], writes=["onesb"])
            P.op("dve", lambda e: e.tensor_copy(out=slb[:], in_=cst[:, 4, :]), reads=["cst"], writes=["slb"])
            P.op("pool", lambda e: e.memset(cnt[:], 0.0), writes=["cnt"])
            for i in range(2):
                P.op("pool", lambda e, i=i: e.memset(yA[i][:], 0.0), writes=[yA[i].name])
                P.op("pool", lambda e, i=i: e.memset(yB[i][:], 0.0), writes=[yB[i].name])

            for i in range(T):
                b = i % 2
                rows = slice(i * 128, (i + 1) * 128)
                hi, hb = hin[b], hbt[b]
                P.dma("sp", hi[:], h_d[rows, :], writes=[hi.name])
                P.dma("sp", hb[:], hb_d[rows, :], writes=[hb.name])
                for hf in range(2):
                    def ftr(pe, hi=hi, hf=hf):
                        for k in range(4):
                            kk = hf * 4 + k
                            ins = pe.transpose(pA[hf][:, k * 128:(k + 1) * 128], hi[:, kk * 128:(kk + 1) * 128], cst[:, 0, :])
                        return ins
                    P.op("pe", ftr, reads=[hi.name, "cst"], writes=[pA[hf].name])
                    P.op("act", lambda e, hf=hf: e.activation(out=hT32[:, hf * 4:(hf + 1) * 4, :],
                                                               in_=pA[hf][:].rearrange("p (k c) -> p k c", c=128), func=AF.Copy),
                         reads=[pA[hf].name], writes=["hT32_%d" % hf])

                def frt(pe):
                    for k in range(8):
                        ins = pe.matmul(pB[0][:, 0:36], lhsT=hT32[:, k, :], rhs=wr[:, k, :], start=(k == 0), stop=(k == 7))
                    return ins
                P.op("pe", frt, reads=["hT32_0", "hT32_1", "wr"], writes=[pB[0].name])
                P.op("act", lambda e: e.activation(out=lg[:], in_=pB[0][:, 0:36], func=AF.Copy), reads=[pB[0].name], writes=["lg"])
                P.op("dve", lambda e: e.tensor_reduce(out=sm[:, 0:1], in_=lg[:, 0:4], axis=AX.X, op=ALU.max), reads=["lg"], writes=["sm0"])
                P.op("dve", lambda e: e.tensor_scalar(out=ohg[:], in0=lg[:, 0:4], scalar1=sm[:, 0:1], scalar2=None, op0=ALU.is_equal),
                     reads=["lg", "sm0"], writes=["ohg"])
                P.op("dve", lambda e: e.tensor_scalar(out=sm[:, 1:2], in0=sm[:, 0:1], scalar1=-1.0, scalar2=None, op0=ALU.mult),
                     reads=["sm0"], writes=["sm1"])
                P.op("act", lambda e: e.activation(out=ge[:], in_=lg[:, 0:4], func=AF.Exp, bias=sm[:, 1:2], scale=1.0),
                     reads=["lg", "sm1"], writes=["ge"])
                P.op("dve", lambda e: e.tensor_reduce(out=sm[:, 2:3], in_=ge[:], axis=AX.X, op=ALU.add), reads=["ge"], writes=["sm2"])
                P.op("dve", lambda e: e.reciprocal(out=sm[:, 3:4], in_=sm[:, 2:3]), reads=["sm2"], writes=["sm3"])
                P.op("dve", lambda e: e.tensor_scalar(out=pen[:], in0=ohg[:], scalar1=1.0, scalar2=1e30, op0=ALU.subtract, op1=ALU.mult),
                     reads=["ohg"], writes=["pen"])
                P.op("dve", lambda e: e.tensor_tensor(out=msk[:].rearrange("p (g j) -> p g j", j=8),
                                                      in0=lg[:, 4:36].rearrange("p (g j) -> p g j", j=8),
                                                      in1=pen[:].unsqueeze(2).to_broadcast([128, 4, 8]), op=ALU.add),
                     reads=["lg", "pen"], writes=["msk"])
                P.op("dve", lambda e: e.max(out=top8[:], in_=msk[:]), reads=["msk"], writes=["top8"])
                P.op("dve", lambda e: e.tensor_scalar(out=sel[:], in0=msk[:], scalar1=top8[:, 1:2], scalar2=None, op0=ALU.is_ge),
                     reads=["msk", "top8"], writes=["sel"])
                P.op("dve", lambda e: e.tensor_copy(out=selb[:], in_=sel[:]), reads=["sel"], writes=["selb"])
                P.op("dve", lambda e: e.tensor_scalar(out=is1[:], in0=msk[:], scalar1=top8[:, 0:1], scalar2=None, op0=ALU.is_equal),
                     reads=["msk", "top8"], writes=["is1"])
                P.op("dve", lambda e: e.tensor_tensor(out=is2[:], in0=sel[:], in1=is1[:], op=ALU.subtract), reads=["sel", "is1"], writes=["is2"])
                P.op("dve", lambda e: e.tensor_tensor(out=sm[:, 4:5], in0=top8[:, 0:1], in1=top8[:, 1:2], op=ALU.subtract),
                     reads=["top8"], writes=["sm4"])
                P.op("act", lambda e: e.activation(out=sm[:, 5:6], in_=sm[:, 4:5], func=AF.Sigmoid), reads=["sm4"], writes=["sm5"])
                def frk(pe):
                    pe.matmul(pB[1][:, 0:32], lhsT=slb[:], rhs=selb[:], start=True, stop=True)
                    return pe.matmul(pB[1][:, 32:64], lhsT=onesb[:], rhs=selb[:], start=True, stop=True)
                P.op("pe", frk, reads=["slb", "onesb", "selb"], writes=[pB[1].name])
                P.op("dve", lambda e: e.tensor_tensor(out=rk[:], in0=pB[1][:, 0:32], in1=cnt[:], op=ALU.add),
                     reads=[pB[1].name, "cnt"], writes=["rk"])
                P.op("dve", lambda e: e.tensor_tensor(out=cnt[:], in0=pB[1][:, 32:64], in1=cnt[:], op=ALU.add),
                     reads=[pB[1].name, "cnt", "rk"], writes=["cnt"])
                P.op("dve", lambda e: e.tensor_scalar(out=vld[:], in0=rk[:], scalar1=float(C), scalar2=None, op0=ALU.is_lt),
                     reads=["rk"], writes=["vld"])
                P.op("dve", lambda e: e.tensor_tensor(out=val[:], in0=rk[:], in1=erow[:], op=ALU.add), reads=["rk", "erow"], writes=["val"])
                BIG = float(4 * NSL)
                P.op("dve", lambda e: e.scalar_tensor_tensor(out=val[:], in0=val[:], scalar=-BIG, in1=vld[:], op0=ALU.add, op1=ALU.mult),
                     reads=["val", "vld"], writes=["val"])
                P.op("dve", lambda e: e.tensor_scalar(out=val[:], in0=val[:], scalar1=BIG, scalar2=None, op0=ALU.add),
                     reads=["val"], writes=["val"])
                for q, isq in ((0, is1), (1, is2)):
                    P.op("dve", lambda e, isq=isq: e.tensor_tensor(out=tmp32[:], in0=isq[:], in1=val[:], op=ALU.mult),
                         reads=["is1", "is2", "val"], writes=["tmp32"])
                    P.op("dve", lambda e, q=q: e.tensor_reduce(out=slf[:, q:q + 1], in_=tmp32[:], axis=AX.X, op=ALU.add),
                         reads=["tmp32"], writes=["slf%d" % q])
                    P.op("dve", lambda e, isq=isq: e.tensor_tensor(out=tmp32[:], in0=isq[:], in1=vld[:], op=ALU.mult),
                         reads=["is1", "is2", "vld", "slf%d" % q], writes=["tmp32"])
                    P.op("dve", lambda e, q=q: e.tensor_reduce(out=sm[:, 8 + q:9 + q], in_=tmp32[:], axis=AX.X, op=ALU.add),
                         reads=["tmp32"], writes=["sm%d" % (8 + q)])
                P.op("dve", lambda e, i=i: e.tensor_copy(out=slots[:, i, :], in_=slf[:]), reads=["slf0", "slf1"], writes=["slots%d" % i])
                P.op("dve", lambda e: e.tensor_tensor(out=sm[:, 6:7], in0=sm[:, 3:4], in1=sm[:, 5:6], op=ALU.mult),
                     reads=["sm3", "sm5"], writes=["sm6"])
                P.op("dve", lambda e: e.tensor_tensor(out=sm[:, 7:8], in0=sm[:, 3:4], in1=sm[:, 6:7], op=ALU.subtract),
                     reads=["sm3", "sm6"], writes=["sm7"])
                P.op("dve", lambda e, i=i: e.tensor_tensor(out=wts[:, i, :], in0=sm[:, 6:8], in1=sm[:, 8:10], op=ALU.mult),
                     reads=["sm6", "sm7", "sm8", "sm9"], writes=["wts%d" % i])
                for q in range(2):
                    P.dma("pool", None, None, reads=["slots%d" % i, hb.name], writes=["xsc_%d_%d" % (i, q)],
                          indirect=lambda g, i=i, q=q, hb=hb: g.indirect_dma_start(
                        out=xpad_d[:, :], out_offset=bass.IndirectOffsetOnAxis(ap=slots[:, i, q:q + 1], axis=0),
                        in_=hb[:], in_offset=None, bounds_check=P.reg(g, NSL - 1), oob_is_err=False))

            P.op("pool", lambda e: e.memset(tmp32[:], 0.0), reads=["xsc_%d_%d" % (i, q) for i in range(T) for q in range(2)],
                 writes=["xpad_ready", "tmp32"])

            wg4 = wg_d.rearrange("e (c p) f -> e p c f", p=128)
            wu4 = wu_d.rearrange("e (c p) f -> e p c f", p=128)
            wd4 = wd_d.rearrange("e (c p) d -> e p c d", p=128)
            NST = C // 128
            npiece = [0]

            def load_w(ex):
                b = ex % 2
                pieces = []
                for hk in range(2):
                    pieces.append((wgt[b][:, hk * 4:(hk + 1) * 4, :], wg4[ex][:, hk * 4:(hk + 1) * 4, :], "%s_%d" % (wgt[b].name, hk)))
                for hk in range(2):
                    pieces.append((wut[b][:, hk * 4:(hk + 1) * 4, :], wu4[ex][:, hk * 4:(hk + 1) * 4, :], "%s_%d" % (wut[b].name, hk)))
                for hk in range(2):
                    pieces.append((wdt[b][:, hk * 2:(hk + 1) * 2, :], wd4[ex][:, hk * 2:(hk + 1) * 2, :], "%s_%d" % (wdt[b].name, hk)))
                for pi, (dst, srcap, key) in enumerate(pieces):
                    n = npiece[0]
                    npiece[0] += 1
                    stg = wstage[n % 3]
                    sv = stg[:] if pi < 4 else stg[:].rearrange("p a b -> p (a b)").rearrange("p (a b) -> p a b", b=1024)
                    P.dma("sp", sv, srcap, writes=[stg.name])
                    eng = "pool" if n % 2 == 0 else "dve"
                    P.op(eng, lambda e, dst=dst, sv=sv: e.tensor_copy(out=dst, in_=sv), reads=[stg.name], writes=[key])

            for ex in range(32):
                b = ex % 2
                wg, wu, wd = wgt[b], wut[b], wdt[b]
                xsT, hh = xsTs[b], hhs[b]
                if MOE_STAGED_WEIGHTS:
                    if ex == 0:
                        load_w(0)
                    if ex + 1 < 32:
                        load_w(ex + 1)
                else:
                    P.dma("pool", wg[:], wg4[ex], writes=[wg.name + "_0", wg.name + "_1"])
                    P.dma("pool", wu[:], wu4[ex], writes=[wu.name + "_0", wu.name + "_1"])
                    P.dma("pool", wd[:], wd4[ex], writes=[wd.name + "_0", wd.name + "_1"])
                for st in range(NST):
                    xx = xs[st % 2]
                    r0 = ex * C + st * 128
                    P.dma("sp", xx[:], xpad_d[r0:r0 + 128, :], reads=["xpad_ready"], writes=[xx.name])

                    def ftx(pe, xx=xx):
                        for k in range(8):
                            ins = pe.transpose(pDb[:, k * 128:(k + 1) * 128], xx[:, k * 128:(k + 1) * 128], identb[:])
                        return ins
                    P.op("pe", ftx, reads=[xx.name, "identb"], writes=[pD[0].name])
                    P.op("act", lambda e, st=st, xsT=xsT: e.activation(out=xsT[:, :, st * 128:(st + 1) * 128],
                                                               in_=pDb[:].rearrange("p (k c) -> p k c", c=128), func=AF.Copy),
                         reads=[pD[0].name], writes=["%s_%d" % (xsT.name, st)])
                xk = ["%s_%d" % (xsT.name, st) for st in range(NST)]
                for fb in range(4):
                    pg, pu, sgg = pA[fb % 2], pB[fb % 2], sg[fb % 2]

                    def fgu(pe, wg=wg, wu=wu, fb=fb, pg=pg, pu=pu, xsT=xsT):
                        for k in range(8):
                            pe.matmul(pg[:, 0:C], lhsT=wg[:, k, fb * 128:(fb + 1) * 128], rhs=xsT[:, k, :], start=(k == 0), stop=(k == 7))
                        for k in range(8):
                            ins = pe.matmul(pu[:, 0:C], lhsT=wu[:, k, fb * 128:(fb + 1) * 128], rhs=xsT[:, k, :], start=(k == 0), stop=(k == 7))
                        return ins
                    P.op("pe", fgu, reads=[wg.name + "_0", wg.name + "_1", wu.name + "_0", wu.name + "_1"] + xk, writes=[pg.name, pu.name])
                    P.op("act", lambda e, pg=pg, sgg=sgg: e.activation(out=sgg[:], in_=pg[:, 0:C], func=AF.Silu),
                         reads=[pg.name], writes=[sgg.name])
                    P.op("dve", lambda e, fb=fb, pu=pu, sgg=sgg, hh=hh: e.tensor_tensor(out=hh[:, fb, :], in0=sgg[:], in1=pu[:, 0:C], op=ALU.mult),
                         reads=[sgg.name, pu.name], writes=["%s_%d" % (hh.name, fb)])
                for st in range(NST):
                    yy = yo[st % 2]
                    for dh in range(2):
                        def fdn(pe, st=st, dh=dh, wd=wd, hh=hh):
                            for fb in range(4):
                                ins = pe.matmul(pC[dh][:], lhsT=hh[:, fb, st * 128:(st + 1) * 128], rhs=wd[:, fb, dh * 512:(dh + 1) * 512],
                                                start=(fb == 0), stop=(fb == 3))
                            return ins
                        P.op("pe", fdn, reads=[wd.name + "_0", wd.name + "_1"] + ["%s_%d" % (hh.name, fb) for fb in range(4)], writes=[pC[dh].name])
                        P.op("act", lambda e, yy=yy, dh=dh: e.activation(out=yy[:, dh * 512:(dh + 1) * 512], in_=pC[dh][:], func=AF.Copy),
                             reads=[pC[dh].name], writes=[yy.name])
                    r0 = ex * C + st * 128
                    P.dma("sp", ypad_d[r0:r0 + 128, :], yy[:], reads=[yy.name], writes=["ypad_%d_%d" % (ex, st)])

            P.op("pool", lambda e: e.memset(tmp32[:], 0.0), reads=["ypad_%d_%d" % (ex, st) for ex in range(32) for st in range(NST)],
                 writes=["ypad_ready", "tmp32"])

            for i in range(T):
                b = i % 2
                rows = slice(i * 128, (i + 1) * 128)
                hi = hin[b]
                P.dma("sp", hi[:], h_d[rows, :], writes=[hi.name])
                for q, yq in ((0, yA[b]), (1, yB[b])):
                    P.dma("pool", None, None, reads=["ypad_ready", "slots%d" % i], writes=[yq.name],
                          indirect=lambda g, i=i, q=q, yq=yq: g.indirect_dma_start(
                              out=yq[:], out_offset=None, in_=ypad_d[:, :],
                              in_offset=bass.IndirectOffsetOnAxis(ap=slots[:, i, q:q + 1], axis=0),
                              bounds_check=P.reg(g, NSL - 1), oob_is_err=False))
                P.op("act", lambda e, hi=hi: e.activation(out=tres[:], in_=hi[:], func=AF.Copy, scale=ALPHA), reads=[hi.name], writes=["tres"])
                for q, yq in ((0, yA[b]), (1, yB[b])):
                    P.op("dve", lambda e, i=i, q=q, yq=yq: e.scalar_tensor_tensor(
                        out=tres[:], in0=yq[:], scalar=wts[:, i, q:q + 1], in1=tres[:], op0=ALU.mult, op1=ALU.add),
                        reads=[yq.name, "wts%d" % i, "tres"], writes=["tres"])
                self.ln_emit(L, i, lng, lnb, ho_d, hob_d, "lngb")


    def attn(self, h_d, hb_d, wqkv_d, wo_d, pos_d, invr_d, lamv_d, subw_d, cst_d, lng_d, lnb_d,
             qT_d, kT_d, v_d, on_d, ho_d, hob_d, lambda_init):
        P, S, T = self.P, self.S, self.T
        TWO_PI = 2.0 * math.pi
        MAGIC = 12582912.0
        wq3 = wqkv_d.rearrange("(c p) n -> p c n", p=128)
        wo3 = wo_d.rearrange("(c p) n -> p c n", p=128)
        qT3 = qT_d.rearrange("(j p) t -> p j t", p=128)
        kT3 = kT_d.rearrange("(j p) t -> p j t", p=128)
        v3 = v_d.rearrange("(t p) c -> p t c", p=128)

        with P.stage():
            cst = P.sb("cst", [128, 5, 128], F32)
            identb = P.sb("identb", [128, 128], BF16)
            invr = P.sb("invr", [128, 8], F32)
            wqkv = P.sb("wqkv", [128, 8, 3072], BF16)
            hbt = [P.sb("hbt%d" % i, [128, 1024], BF16) for i in range(2)]
            hTt = P.sb("hTt", [128, 8, 128], BF16)
            qkv = P.sb("qkv", [128, 3072], F32)
            posi = [P.sb("posi%d" % i, [128, 1], I32) for i in range(2)]
            posf = P.sb("posf", [128, 1], F32)
            a16 = P.sb("a16", [128, 16], F32)
            kk = P.sb("kk", [128, 16], F32)
            sc16 = P.sb("sc16", [128, 16], F32)
            tt4 = [P.sb("tt%d" % i, [128, 32, 8], F32) for i in range(4)]
            qkb = P.sb("qkb", [128, 2048], BF16)
            vb = [P.sb("vb%d" % i, [128, 1024], BF16) for i in range(2)]
            qkT = [P.sb("qkT%d" % i, [128, 16, 128], BF16) for i in range(2)]
            pT_ = [P.ps("pT%d" % i, [128, 512], F32) for i in range(2)]
            pQ = [P.ps("pQ%d" % i, [128, 512], F32) for i in range(2)]
            pTb = [p[:].bitcast(BF16) for p in pT_]

            P.dma("sp", cst[:], cst_d, writes=["cst"])
            P.dma("sp", invr[:], invr_d, writes=["invr"])
            P.op("dve", lambda e: e.tensor_copy(out=identb[:], in_=cst[:, 0, :]), reads=["cst"], writes=["identb"])
            for j in range(6):
                P.dma("pool", wqkv[:, :, j * 512:(j + 1) * 512], wq3[:, :, j * 512:(j + 1) * 512], writes=["wqkv%d" % j])
            for i in range(T):
                b = i % 2
                rows = slice(i * 128, (i + 1) * 128)
                hb = hbt[b]
                P.dma("sp", hb[:], hb_d[rows, :], writes=[hb.name])
                P.dma("sp", posi[b][:], pos_d[rows, :], writes=[posi[b].name])

                def fth(pe, hb=hb):
                    for k in range(8):
                        ins = pe.transpose(pTb[0][:, k * 128:(k + 1) * 128], hb[:, k * 128:(k + 1) * 128], identb[:])
                    return ins
                P.op("pe", fth, reads=[hb.name, "identb"], writes=[pT_[0].name])
                P.op("act", lambda e: e.activation(out=hTt[:], in_=pTb[0][:].rearrange("p (k c) -> p k c", c=128), func=AF.Copy),
                     reads=[pT_[0].name], writes=["hTt"])
                for cbk in range(6):
                    pq = pQ[cbk % 2]

                    def fq(pe, cbk=cbk, pq=pq):
                        for k in range(8):
                            ins = pe.matmul(pq[:], lhsT=hTt[:, k, :], rhs=wqkv[:, k, cbk * 512:(cbk + 1) * 512], start=(k == 0), stop=(k == 7))
                        return ins
                    P.op("pe", fq, reads=["hTt", "wqkv%d" % cbk], writes=[pq.name])
                    P.op("act", lambda e, cbk=cbk, pq=pq: e.activation(out=qkv[:, cbk * 512:(cbk + 1) * 512], in_=pq[:], func=AF.Copy),
                         reads=[pq.name], writes=["qkv%d" % cbk])
                P.op("dve", lambda e, b=b: e.tensor_copy(out=posf[:], in_=posi[b][:]), reads=[posi[b].name], writes=["posf"])
                P.op("dve", lambda e: e.tensor_scalar(out=a16[:, 0:8], in0=invr[:], scalar1=posf[:, 0:1], scalar2=None, op0=ALU.mult),
                     reads=["invr", "posf"], writes=["a16a"])
                P.op("dve", lambda e: e.tensor_scalar(out=a16[:, 8:16], in0=a16[:, 0:8], scalar1=0.5 * math.pi, scalar2=None, op0=ALU.add),
                     reads=["a16a"], writes=["a16b"])
                P.op("dve", lambda e: e.tensor_scalar(out=kk[:], in0=a16[:], scalar1=1.0 / TWO_PI, scalar2=MAGIC, op0=ALU.mult, op1=ALU.add),
                     reads=["a16a", "a16b"], writes=["kk"])
                P.op("dve", lambda e: e.tensor_scalar(out=kk[:], in0=kk[:], scalar1=-MAGIC, scalar2=None, op0=ALU.add), reads=["kk"], writes=["kk"])
                P.op("dve", lambda e: e.scalar_tensor_tensor(out=kk[:], in0=kk[:], scalar=-TWO_PI, in1=a16[:], op0=ALU.mult, op1=ALU.add),
                     reads=["kk", "a16a", "a16b"], writes=["kk"])
                P.op("dve", lambda e: e.tensor_scalar(out=kk[:], in0=kk[:], scalar1=-math.pi, scalar2=math.pi, op0=ALU.max, op1=ALU.min),
                     reads=["kk"], writes=["kk"])
                P.op("act", lambda e: e.activation(out=sc16[:], in_=kk[:], func=AF.Sin), reads=["kk"], writes=["sc16"])
                qk3 = qkv[:, 0:2048].rearrange("p (g d) -> p g d", d=64)
                r1, r2 = qk3[:, :, 0:8], qk3[:, :, 8:16]
                sinb = sc16[:, 0:8].unsqueeze(1).to_broadcast([128, 32, 8])
                cosb = sc16[:, 8:16].unsqueeze(1).to_broadcast([128, 32, 8])
                qkeys = ["qkv%d" % j for j in range(4)]
                for n, (aa, bb) in enumerate(((r1, cosb), (r2, sinb), (r2, cosb), (r1, sinb))):
                    P.op("dve", lambda e, n=n, aa=aa, bb=bb: e.tensor_tensor(out=tt4[n][:], in0=aa, in1=bb, op=ALU.mult),
                         reads=qkeys + ["sc16"], writes=[tt4[n].name])
                P.op("dve", lambda e: e.tensor_tensor(out=r1, in0=tt4[0][:], in1=tt4[1][:], op=ALU.subtract),
                     reads=[tt4[0].name, tt4[1].name, tt4[2].name, tt4[3].name], writes=qkeys)
                P.op("dve", lambda e: e.tensor_tensor(out=r2, in0=tt4[2][:], in1=tt4[3][:], op=ALU.add),
                     reads=[tt4[2].name, tt4[3].name], writes=qkeys)
                P.op("act", lambda e: e.activation(out=qkb[:], in_=qkv[:, 0:2048], func=AF.Copy), reads=qkeys, writes=["qkb"])
                P.op("act", lambda e, b=b: e.activation(out=vb[b][:], in_=qkv[:, 2048:3072], func=AF.Copy),
                     reads=["qkv4", "qkv5"], writes=[vb[b].name])
                P.dma("sp", v_d[rows, :], vb[b][:], reads=[vb[b].name])
                for hf in range(2):
                    def ftq(pe, hf=hf):
                        for k in range(8):
                            j = hf * 8 + k
                            ins = pe.transpose(pTb[hf][:, k * 128:(k + 1) * 128], qkb[:, j * 128:(j + 1) * 128], identb[:])
                        return ins
                    P.op("pe", ftq, reads=["qkb", "identb"], writes=[pT_[hf].name])
                    P.op("act", lambda e, hf=hf, b=b: e.activation(out=qkT[b][:, hf * 8:(hf + 1) * 8, :],
                                                                   in_=pTb[hf][:].rearrange("p (k c) -> p k c", c=128), func=AF.Copy),
                         reads=[pT_[hf].name], writes=["%s_%d" % (qkT[b].name, hf)])
                P.dma("sp", qT3[:, :, rows], qkT[b][:, 0:8, :], reads=["%s_0" % qkT[b].name])
                P.dma("sp", kT3[:, :, rows], qkT[b][:, 8:16, :], reads=["%s_1" % qkT[b].name])

        with P.stage():
            cst = P.sb("cst", [128, 5, 128], F32)
            trib = P.sb("trib", [128, 128], BF16)
            lamv = P.sb("lamv", [128, 4, 64], F32)
            lpr = P.sb("lpr", [128, 2, 64], F32)
            ls = P.sb("ls", [128, 4], F32)
            subw = P.sb("subw", [128, 128], F32)
            KT = [P.sb("KT%d" % i, [128, S], BF16) for i in range(2)]
            QT = [P.sb("QT%d" % i, [128, S], BF16) for i in range(2)]
            Vx = [P.sb("Vx%d" % i, [128, T, 129], BF16) for i in range(2)]
            pTt = [P.sb("pTt%d" % i, [128, 512], BF16) for i in range(4)]
            rr = [P.sb("rr%d" % i, [128, 2], F32) for i in range(2)]
            oA = [P.sb("oA%d" % i, [128, 128], F32) for i in range(2)]
            oo = [P.sb("oo%d" % i, [128, 128], F32) for i in range(2)]
            osq = P.sb("osq", [128, 128], F32)
            oss = [P.sb("oss%d" % i, [128, 1], F32) for i in range(2)]
            onb = [P.sb("onb%d" % i, [128, 128], BF16) for i in range(2)]
            pS = [P.ps("pS%d" % i, [128, 512], F32) for i in range(4)]
            pO = [P.ps("pO%d" % i, [128, 512], F32) for i in range(4)]
            scale = 64 ** -0.5

            P.dma("sp", cst[:], cst_d, writes=["cst"])
            P.dma("sp", lamv[:], lamv_d, writes=["lamv"])
            P.dma("sp", subw[:], subw_d, writes=["subw"])
            P.op("dve", lambda e: e.tensor_copy(out=trib[:], in_=cst[:, 1, :]), reads=["cst"], writes=["trib"])
            P.op("dve", lambda e: e.tensor_scalar(out=subw[:], in0=subw[:], scalar1=1.0 - lambda_init, scalar2=None, op0=ALU.mult),
                 reads=["subw"], writes=["subw"])
            P.op("dve", lambda e: e.tensor_tensor(out=lpr[:, 0, :], in0=lamv[:, 0, :], in1=lamv[:, 1, :], op=ALU.mult), reads=["lamv"], writes=["lpr0"])
            P.op("dve", lambda e: e.tensor_tensor(out=lpr[:, 1, :], in0=lamv[:, 2, :], in1=lamv[:, 3, :], op=ALU.mult), reads=["lamv"], writes=["lpr1"])
            P.op("dve", lambda e: e.tensor_reduce(out=ls[:, 0:2], in_=lpr[:], axis=AX.X, op=ALU.add), reads=["lpr0", "lpr1"], writes=["ls"])
            P.op("act", lambda e: e.activation(out=ls[:, 0:2], in_=ls[:, 0:2], func=AF.Exp), reads=["ls"], writes=["ls"])
            P.op("dve", lambda e: e.tensor_tensor(out=ls[:, 2:3], in0=ls[:, 0:1], in1=ls[:, 1:2], op=ALU.subtract), reads=["ls"], writes=["ls"])
            P.op("dve", lambda e: e.tensor_scalar(out=ls[:, 3:4], in0=ls[:, 2:3], scalar1=lambda_init, scalar2=-1.0, op0=ALU.add, op1=ALU.mult),
                 reads=["ls"], writes=["ls"])
            for i in range(2):
                P.op("pool", lambda e, i=i: e.memset(Vx[i][:, :, 128:129], 1.0), writes=["%s_one" % Vx[i].name])
            gcount = 0
            for h in range(8):
                hb_ = h % 2
                kt, qt, vx = KT[hb_], QT[hb_], Vx[hb_]
                P.dma("sp", kt[:], kT_d[h * 128:(h + 1) * 128, :], writes=[kt.name])
                P.dma("sp", qt[:], qT_d[h * 128:(h + 1) * 128, :], writes=[qt.name])
                P.dma("sp", vx[:, :, 0:128], v3[:, :, h * 128:(h + 1) * 128], writes=[vx.name])
                for i in range(T):
                    ib = i % 2
                    for g0 in range(0, i + 1, 4):
                        kbs = list(range(g0, min(g0 + 4, i + 1)))
                        n = len(kbs)
                        gb = gcount % 2
                        gcount += 1
                        psc = [pS[gb * 2], pS[gb * 2 + 1]]
                        ptc = [pTt[gb * 2], pTt[gb * 2 + 1]]

                        def fs(pe, kbs=kbs, psc=psc, kt=kt, qt=qt, i=i):
                            for j, kb in enumerate(kbs):
                                for c in range(2):
                                    cs = slice(c * 64, (c + 1) * 64)
                                    ins = pe.matmul(psc[c][:, j * 128:(j + 1) * 128], lhsT=kt[cs, kb * 128:(kb + 1) * 128],
                                                    rhs=qt[cs, i * 128:(i + 1) * 128], start=True, stop=True)
                            return ins
                        P.op("pe", fs, reads=[kt.name, qt.name], writes=[psc[0].name, psc[1].name])
                        for c in range(2):
                            ps_, pt = psc[c], ptc[c]
                            po = pO[ib * 2 + c]
                            P.op("act", lambda e, n=n, ps_=ps_, pt=pt: e.activation(out=pt[:, 0:n * 128], in_=ps_[:, 0:n * 128],
                                                                                   func=AF.Exp, scale=scale),
                                 reads=[ps_.name], writes=[pt.name])
                            if kbs[-1] == i:
                                jd = n - 1
                                P.op("dve", lambda e, jd=jd, pt=pt: e.tensor_tensor(out=pt[:, jd * 128:(jd + 1) * 128],
                                                                                    in0=pt[:, jd * 128:(jd + 1) * 128], in1=trib[:], op=ALU.mult),
                                     reads=[pt.name, "trib"], writes=[pt.name])

                            def fav(pe, kbs=kbs, pt=pt, vx=vx, po=po, i=i):
                                for j, kb in enumerate(kbs):
                                    ins = pe.matmul(po[:, 0:129], lhsT=pt[:, j * 128:(j + 1) * 128], rhs=vx[:, kb, :],
                                                    start=(kb == 0), stop=(kb == i))
                                return ins
                            P.op("pe", fav, reads=[pt.name, vx.name, "%s_one" % vx.name], writes=[po.name])
                    p0, p1 = pO[ib * 2], pO[ib * 2 + 1]
                    rq, oa, o_, os_, ob = rr[ib], oA[ib], oo[ib], oss[ib], onb[ib]
                    P.op("dve", lambda e, rq=rq, p0=p0: e.reciprocal(out=rq[:, 0:1], in_=p0[:, 128:129]), reads=[p0.name], writes=[rq.name + "a"])
                    P.op("dve", lambda e, rq=rq, p1=p1: e.reciprocal(out=rq[:, 1:2], in_=p1[:, 128:129]), reads=[p1.name], writes=[rq.name + "b"])
                    P.op("dve", lambda e, rq=rq: e.tensor_tensor(out=rq[:, 1:2], in0=rq[:, 1:2], in1=ls[:, 3:4], op=ALU.mult),
                         reads=[rq.name + "b", "ls"], writes=[rq.name + "b"])
                    P.op("act", lambda e, rq=rq, oa=oa, p0=p0: e.activation(out=oa[:], in_=p0[:, 0:128], func=AF.Copy, scale=rq[:, 0:1]),
                         reads=[p0.name, rq.name + "a"], writes=[oa.name])
                    P.op("dve", lambda e, rq=rq, oa=oa, o_=o_, p1=p1: e.scalar_tensor_tensor(
                        out=o_[:], in0=p1[:, 0:128], scalar=rq[:, 1:2], in1=oa[:], op0=ALU.mult, op1=ALU.add),
                        reads=[p1.name, rq.name + "b", oa.name], writes=[o_.name])
                    P.op("pool", lambda e, o_=o_: e.tensor_tensor(out=osq[:], in0=o_[:], in1=o_[:], op=ALU.mult), reads=[o_.name], writes=["osq"])
                    P.op("dve", lambda e, os_=os_: e.tensor_reduce(out=os_[:], in_=osq[:], axis=AX.X, op=ALU.add), reads=["osq"], writes=[os_.name])
                    P.op("act", lambda e, os_=os_: e.activation(out=os_[:], in_=os_[:], func=AF.Sqrt, bias=EPS, scale=1.0 / 128),
                         reads=[os_.name], writes=[os_.name])
                    P.op("dve", lambda e, os_=os_: e.reciprocal(out=os_[:], in_=os_[:]), reads=[os_.name], writes=[os_.name])
                    P.op("dve", lambda e, o_=o_, os_=os_, ob=ob: e.scalar_tensor_tensor(
                        out=ob[:], in0=o_[:], scalar=os_[:, 0:1], in1=subw[:], op0=ALU.mult, op1=ALU.mult),
                        reads=[o_.name, os_.name, "subw"], writes=[ob.name])
                    P.dma("sp", on_d[i * 128:(i + 1) * 128, h * 128:(h + 1) * 128], ob[:], reads=[ob.name])

        with P.stage():
            cst = P.sb("cst", [128, 5, 128], F32)
            identb = P.sb("identb", [128, 128], BF16)
            lng = P.sb("lng", [128, 1024], F32)
            lnb = P.sb("lnb", [128, 1024], F32)
            wo = P.sb("wo", [128, 8, 1024], BF16)
            ont = [P.sb("ont%d" % i, [128, 1024], BF16) for i in range(2)]
            hin = [P.sb("hin%d" % i, [128, 1024], F32) for i in range(2)]
            onT = P.sb("onT", [128, 8, 128], BF16)
            L = self.ln_tiles(None)
            tres = L["tres"]
            pT_ = P.ps("pT", [128, 512], F32)
            pTb = pT_[:].bitcast(BF16)
            pO = [P.ps("pO%d" % i, [128, 512], F32) for i in range(2)]
            P.dma("sp", cst[:], cst_d, writes=["cst"])
            P.dma("sp", lng[:], lng_d, writes=["lngb_g"])
            P.dma("sp", lnb[:], lnb_d, writes=["lngb_b"])
            for j in range(2):
                P.dma("pool", wo[:, :, j * 512:(j + 1) * 512], wo3[:, :, j * 512:(j + 1) * 512], writes=["wo%d" % j])
            P.op("dve", lambda e: e.tensor_copy(out=identb[:], in_=cst[:, 0, :]), reads=["cst"], writes=["identb"])
            for i in range(T):
                b = i % 2
                rows = slice(i * 128, (i + 1) * 128)
                P.dma("sp", ont[b][:], on_d[rows, :], writes=[ont[b].name])
                P.dma("sp", hin[b][:], h_d[rows, :], writes=[hin[b].name])

                def fto(pe, b=b):
                    for k in range(8):
                        ins = pe.transpose(pTb[:, k * 128:(k + 1) * 128], ont[b][:, k * 128:(k + 1) * 128], identb[:])
                    return ins
                P.op("pe", fto, reads=[ont[b].name, "identb"], writes=[pT_.name])
                P.op("act", lambda e: e.activation(out=onT[:], in_=pTb[:].rearrange("p (k c) -> p k c", c=128), func=AF.Copy),
                     reads=[pT_.name], writes=["onT"])
                for dh in range(2):
                    def fo(pe, dh=dh):
                        for cb in range(8):
                            ins = pe.matmul(pO[dh][:], lhsT=onT[:, cb, :], rhs=wo[:, cb, dh * 512:(dh + 1) * 512], start=(cb == 0), stop=(cb == 7))
                        return ins
                    P.op("pe", fo, reads=["onT", "wo%d" % dh], writes=[pO[dh].name])
                    P.op("dve", lambda e, dh=dh, b=b: e.scalar_tensor_tensor(
                        out=tres[:, dh * 512:(dh + 1) * 512], in0=hin[b][:, dh * 512:(dh + 1) * 512], scalar=ALPHA,
                        in1=pO[dh][:], op0=ALU.mult, op1=ALU.add), reads=[hin[b].name, pO[dh].name], writes=["tres"])
                self.ln_emit(L, i, lng, lnb, ho_d, hob_d, "lngb")


SEQ = 4096
MOE_STAGED_WEIGHTS = False
CAP0 = 512


def _consts():
    c = np.zeros((128, 5, 128), np.float32)
    j = np.arange(128)
    c[:, 0, :] = np.eye(128)
    c[:, 1, :] = (j[:, None] <= j[None, :])
    c[:, 2, :] = (j[:, None] > j[None, :])
    c[:, 3, :] = 1.0
    c[:, 4, :] = (j[:, None] < j[None, :])
    return c


def _rep(v):
    return np.ascontiguousarray(np.broadcast_to(np.asarray(v, np.float32).reshape(1, -1), (128, np.asarray(v).size)))


def build_full(S, C, dbg=()):
    b = Builder(S, dbg=dbg)
    i = b.inp
    x_d = i("x", [S, 1024])
    pos_d = i("pos", [S, 1], I32)
    cst_d = i("cst", [128, 5, 128])
    w_in_d = i("w_in", [1024, INDIM])
    convw_d = i("convw", [128, 32, 4])
    convb_d = i("convb", [128, 32])
    hp_d = i("hp", [128, 3, 32])
    w_out_d = i("w_out", [2048, 1024])
    normw_d = i("normw", [128, 2048])
    ln_d = i("ln", [8, 128, 1024])
    erow_d = i("erow", [128, 32])
    wr_d = [i("wr%d" % l, [1024, 36]) for l in range(2)]
    wg_d = [i("wg%d" % l, [32, 1024, 512]) for l in range(2)]
    wu_d = [i("wu%d" % l, [32, 1024, 512]) for l in range(2)]
    wd_d = [i("wd%d" % l, [32, 512, 1024]) for l in range(2)]
    wqkv_d = i("wqkv", [1024, 3072])
    wo_d = i("wo", [1024, 1024])
    invr_d = i("invr", [128, 8])
    lamv_d = i("lamv", [128, 4, 64])
    subw_d = i("subw", [128, 128])
    s = b.scratch
    zs_d = s("zs_d", [S, 2048], BF16)
    xbcT_d = s("xbcT_d", [4096, S], BF16)
    dtd_d = s("dtd_d", [S, 64], F32)
    hs = [s("h%d_d" % k, [S, 1024], F32) for k in range(1, 4)]
    hbs = [s("h%db_d" % k, [S, 1024], BF16) for k in range(1, 4)]
    xpad_d = s("xpad_d", [32 * C, 1024], BF16)
    ypad_d = s("ypad_d", [32 * C, 1024], BF16)
    qT_d = s("qT_d", [1024, S], BF16)
    kT_d = s("kT_d", [1024, S], BF16)
    v_d = s("v_d", [S, 1024], BF16)
    on_d = s("on_d", [S, 1024], BF16)
    out_d = b.nc.dram_tensor("out", [S, 1024], F32, kind="ExternalOutput").ap()
    lambda_init = 0.8 - 0.6 * math.exp(-0.3 * 1)
    b.l0_in(x_d, w_in_d, convw_d, convb_d, hp_d, cst_d, zs_d, xbcT_d, dtd_d)
    b.l0_ssd(x_d, zs_d, xbcT_d, dtd_d, w_out_d, normw_d, hp_d, cst_d, ln_d[0], ln_d[1], hs[0], hbs[0])
    b.moe(hs[0], hbs[0], wr_d[0], wg_d[0], wu_d[0], wd_d[0], cst_d, erow_d, ln_d[2], ln_d[3], xpad_d, ypad_d, hs[1], hbs[1], C)
    b.attn(hs[1], hbs[1], wqkv_d, wo_d, pos_d, invr_d, lamv_d, subw_d, cst_d, ln_d[4], ln_d[5],
           qT_d, kT_d, v_d, on_d, hs[2], hbs[2], lambda_init)
    b.moe(hs[2], hbs[2], wr_d[1], wg_d[1], wu_d[1], wd_d[1], cst_d, erow_d, ln_d[6], ln_d[7], xpad_d, ypad_d, out_d, None, C)
    b.P.finish()
    return b


def make_in_maps(inputs, S, C, n_cores=8):
    f = lambda k: np.asarray(inputs[k])
    conv_w = f("ssm_conv_w")[0]
    conv_b = f("ssm_conv_b")[0]
    shared = {
        "cst": _consts(),
        "w_in": np.ascontiguousarray(f("ssm_w_in")[0]),
        "convw": np.ascontiguousarray(conv_w.T.reshape(32, 128, 4).transpose(1, 0, 2)),
        "convb": np.ascontiguousarray(conv_b.reshape(32, 128).T),
        "hp": np.ascontiguousarray(np.stack([_rep(f("ssm_dt_bias")[0]), _rep(f("ssm_a_log")[0]), _rep(f("ssm_d")[0])], axis=1)),
        "w_out": np.ascontiguousarray(f("ssm_w_out")[0]),
        "normw": _rep(f("ssm_norm_w")[0]),
        "ln": np.ascontiguousarray(np.stack([_rep(f("ln_mix_g")[0]), _rep(f("ln_mix_b")[0]), _rep(f("ln_ffn_g")[0]), _rep(f("ln_ffn_b")[0]),
                                             _rep(f("ln_mix_g")[1]), _rep(f("ln_mix_b")[1]), _rep(f("ln_ffn_g")[1]), _rep(f("ln_ffn_b")[1])])),
        "erow": _rep(np.arange(32, dtype=np.float32) * C),
        "wqkv": np.ascontiguousarray(f("attn_w_qkv")[0]),
        "wo": np.ascontiguousarray(f("attn_w_o")[0]),
        "invr": _rep((500000.0 ** (-np.arange(0, 16, 2, dtype=np.float32) / 16)).astype(np.float32)),
        "lamv": np.ascontiguousarray(np.broadcast_to(
            np.stack([f("attn_lam_q1")[0], f("attn_lam_k1")[0], f("attn_lam_q2")[0], f("attn_lam_k2")[0]])[None], (128, 4, 64))).astype(np.float32),
        "subw": _rep(f("attn_subln_w")[0]),
    }
    for l in range(2):
        shared["wr%d" % l] = np.ascontiguousarray(np.concatenate([f("moe_w_group")[l], f("moe_w_expert")[l]], axis=1))
        shared["wg%d" % l] = np.ascontiguousarray(f("moe_w_gate")[l])
        shared["wu%d" % l] = np.ascontiguousarray(f("moe_w_up")[l])
        shared["wd%d" % l] = np.ascontiguousarray(f("moe_w_down")[l])
    maps = []
    for c in range(n_cores):
        bi = c // 2
        m = dict(shared)
        m["x"] = np.ascontiguousarray(f("x")[bi, :S])
        m["pos"] = np.ascontiguousarray(f("positions")[bi, :S].reshape(S, 1).astype(np.int32))
        maps.append(m)
    return maps


def kernel(**inputs):
    S, C = SEQ, CAP0
    b = build_full(S, C)
    maps = make_in_maps(inputs, S, C)
    res = run_bass_kernel_spmd(b.nc, maps, core_ids=list(range(8)))
    out = np.stack([np.asarray(res.results[2 * bi]["out"]) for bi in range(4)], axis=0)
    return out.astype(np.float32)
```
